# Optimizing a Trainium2 kernel written in Bass

```python
import math
import jax, jax.numpy as jnp
from jax import lax
import numpy as np

D_MODEL = 1024
BATCH = 8
SEQ = 4096
DEPTH = 2

GRID_W = 64
CTX_LEN = 256
BRANCH_W = 256
HEAD_DIM = 64
N_BRANCHES = 4
N_MOD = 6
RWKV_HEADS = BRANCH_W // HEAD_DIM
RWKV_DECAY_LORA = 64
RWKV_A_LORA = 64
RWKV_G_LORA = 128
RWKV_COLS = 3 * BRANCH_W + RWKV_DECAY_LORA + RWKV_A_LORA + RWKV_G_LORA
RWKV_SPLITS = (BRANCH_W, 2 * BRANCH_W, 3 * BRANCH_W,
               3 * BRANCH_W + RWKV_DECAY_LORA, 3 * BRANCH_W + RWKV_DECAY_LORA + RWKV_A_LORA)
RWKV_GN_EPS = 64e-5
HYENA_COLS = 3 * BRANCH_W
HYENA_EMB = 33
HYENA_FILTER_HIDDEN = 64
HYENA_FAST_DECAY = 0.3
HYENA_SLOW_DECAY = 1.5
HYENA_TARGET = 1e-2
SCONV_COLS = 3 * BRANCH_W
ATTN_Q_HEADS = BRANCH_W // HEAD_DIM
ATTN_KV_HEADS = 2
Q_W = ATTN_Q_HEADS * HEAD_DIM
KV_W = ATTN_KV_HEADS * HEAD_DIM
ATTN_COLS = Q_W + 2 * KV_W
Q_BLOCK = 128
ROPE_THETA = 10000.0
RMS_EPS = 1e-6
GATE_COLS = N_BRANCHES * D_MODEL
OFF_HYENA = RWKV_COLS
OFF_SCONV = OFF_HYENA + HYENA_COLS
OFF_ATTN = OFF_SCONV + SCONV_COLS
OFF_GATE = OFF_ATTN + ATTN_COLS
IN_COLS = OFF_GATE + GATE_COLS
N_EXPERTS = 16
N_GROUPS = 4
EXPERTS_PER_GROUP = N_EXPERTS // N_GROUPS
TOP_K = 2
D_EXPERT = 512
ALPHA = (2 * DEPTH) ** 0.25
BETA = (8 * DEPTH) ** -0.25
LN_EPS = 1e-6

kernel_name = "hybrid_rwkv_hyena_conv_gqa_moe_dit"

F32 = jnp.float32


def ln_plain(x):
    xf = x.astype(F32)
    mu = xf.mean(-1, keepdims=True)
    var = jnp.mean(jnp.square(xf - mu), -1, keepdims=True)
    return ((xf - mu) * lax.rsqrt(var + LN_EPS)).astype(x.dtype)


def layer_norm(x, g, b):
    return ln_plain(x) * g + b


def modulate(x, shift, scale):
    return ln_plain(x) * (1.0 + scale) + shift


def rms_norm(x, g):
    xf = x.astype(F32)
    return (xf * lax.rsqrt(jnp.mean(xf * xf, -1, keepdims=True) + RMS_EPS) * g).astype(x.dtype)


def conv3(x, w):
    xp = jnp.pad(x, ((0, 0), (1, 1), (0, 0)))
    return w[0] * xp[:, :-2] + w[1] * xp[:, 1:-1] + w[2] * xp[:, 2:]


def centred_shift_mix(p, mu):
    xp = jnp.pad(p, ((0, 0), (1, 1), (0, 0)))
    return p + mu * (0.5 * (xp[:, :-2] + xp[:, 2:]) - p)


def to_heads(t):
    return t.reshape(t.shape[:-1] + (RWKV_HEADS, HEAD_DIM))


def rwkv_inputs(p, mu, w0, w_up, a0, a_up, k_k, k_a):
    p = centred_shift_mix(p, mu).astype(F32)
    r, k, v, wd, ad, gd = jnp.split(p, RWKV_SPLITS, axis=-1)
    wlog = w0[:, None, None, :] + jnp.einsum('btr,drc->dbtc', jnp.tanh(wd), w_up)
    decay = jnp.exp(-jnp.exp(-jax.nn.softplus(-wlog) - 0.5))
    a = jax.nn.sigmoid(a0[:, None, None, :] + jnp.einsum('btr,drc->dbtc', ad, a_up))
    kk = to_heads(k * k_k)
    kk = kk * lax.rsqrt(jnp.maximum(jnp.sum(kk * kk, -1, keepdims=True), 1e-24))
    k_dir = k[None] * (1.0 + (a - 1.0) * k_a)
    return to_heads(r), to_heads(k_dir), to_heads(v), kk, to_heads(a), to_heads(decay), gd


def wkv_scan(S0, r, decay, k, v, kk, a, reverse, emit):
    def step(S, inp):
        r_t, w_t, k_t, v_t, kk_t, a_t = inp
        S = (S * w_t[:, :, None, :]
             - jnp.einsum('bhvk,bhk->bhv', S, kk_t)[..., None] * (kk_t * a_t)[:, :, None, :]
             + v_t[..., :, None] * k_t[..., None, :])
        return S, (jnp.einsum('bhvk,bhk->bhv', S, r_t) if emit else None)
    xs = tuple(jnp.moveaxis(t, 1, 0) for t in (r, decay, k, v, kk, a))
    S, ys = lax.scan(step, S0, xs, reverse=reverse)
    return S, (jnp.moveaxis(ys, 0, 1) if emit else None)


def direction_scan(S0, ins, d, reverse, emit):
    r, k_dir, v, kk, a, decay, _ = ins
    return wkv_scan(S0, r, decay[d], k_dir[d], v, kk, a[d], reverse, emit)


def rwkv_output(y, ins, g_up, r_k, lnx_g, lnx_b):
    r, k_dir, v, _, _, _, gd = ins
    B, T = y.shape[:2]
    mu = y.mean(-1, keepdims=True)
    var = jnp.mean(jnp.square(y - mu), -1, keepdims=True)
    yn = ((y - mu) * lax.rsqrt(var + RWKV_GN_EPS)).reshape(B, T, BRANCH_W) * lnx_g + lnx_b
    bonus = jnp.sum(r[None] * k_dir * to_heads(r_k), axis=-1, keepdims=True).sum(0) * v
    g = jax.nn.sigmoid(gd) @ g_up
    return (yn + bonus.reshape(B, T, BRANCH_W)) * g


def rwkv_mixer(p_ctx, p_lat, mu, w0, w_up, a0, a_up, g_up, k_k, k_a, r_k, lnx_g, lnx_b, ctx_out):
    ins_c = rwkv_inputs(p_ctx, mu, w0, w_up, a0, a_up, k_k, k_a)
    ins_l = rwkv_inputs(p_lat, mu, w0, w_up, a0, a_up, k_k, k_a)
    S0 = jnp.zeros((p_lat.shape[0], RWKV_HEADS, HEAD_DIM, HEAD_DIM), F32)
    y_lat, y_ctx = [], []
    for d, rev in enumerate((False, True)):
        S_ctx, yc = direction_scan(S0, ins_c, d, rev, ctx_out)
        _, yl = direction_scan(S_ctx, ins_l, d, rev, True)
        y_lat.append(yl)
        y_ctx.append(yc)
    out_l = rwkv_output(y_lat[0] + y_lat[1], ins_l, g_up, r_k, lnx_g, lnx_b).astype(p_lat.dtype)
    out_c = (rwkv_output(y_ctx[0] + y_ctx[1], ins_c, g_up, r_k, lnx_g, lnx_b).astype(p_ctx.dtype)
             if ctx_out else None)
    return out_c, out_l


def hyena_filter(L, w1, b1, f1, w2, b2, f2, w3):
    bands = (HYENA_EMB - 1) // 2
    t = jnp.linspace(0.0, 1.0, L, dtype=F32)[:, None]
    wpos = 2.0 * math.pi * jnp.arange(L, dtype=F32)[:, None] / L
    f = jnp.linspace(1e-4, bands - 1, bands, dtype=F32)[None, :]
    z = jnp.concatenate([t, jnp.cos(f * wpos), -jnp.sin(f * wpos)], axis=-1)
    hdn = jnp.sin(f1 * (z @ w1 + b1))
    hdn = jnp.sin(f2 * (hdn @ w2 + b2))
    filt = (hdn @ w3).astype(F32)
    C = filt.shape[-1] // 2
    max_decay = math.log(HYENA_TARGET) / HYENA_FAST_DECAY
    min_decay = math.log(HYENA_TARGET) / HYENA_SLOW_DECAY
    deltas = jnp.abs(jnp.linspace(min_decay, max_decay, C, dtype=F32))
    window = jnp.exp(-t * deltas[None, :])
    return filt[:, :C] * window, filt[:, C:] * window


def bidir_fft_conv(u, h_fwd, h_bwd):
    B, L, C = u.shape
    k = jnp.concatenate([h_fwd, jnp.zeros((1, C), F32), h_bwd[:0:-1]], axis=0)
    uf = jnp.fft.rfft(u.astype(F32), n=2 * L, axis=1)
    kf = jnp.fft.rfft(k, n=2 * L, axis=0)
    return jnp.fft.irfft(uf * kf[None], n=2 * L, axis=1)[:, :L]


def hyena_mixer(p, conv_w, skip, w1, b1, f1, w2, b2, f2, w3):
    p = conv3(p, conv_w)
    x0, x1, v = jnp.split(p, 3, axis=-1)
    u = x1 * v
    h_fwd, h_bwd = hyena_filter(p.shape[1], w1, b1, f1, w2, b2, f2, w3)
    y = bidir_fft_conv(u, h_fwd, h_bwd) + u.astype(F32) * skip
    return (x0 * y).astype(p.dtype)


def short_conv_mixer(p, w):
    b_gate, c_gate, xin = jnp.split(p, 3, axis=-1)
    return b_gate * conv3(c_gate * xin, w)


def rope_2d(x, rows, cols):
    half = x.shape[-1] // 2
    inv = ROPE_THETA ** (-jnp.arange(0, half, 2, dtype=F32) / half)
    ang = jnp.concatenate([rows[:, None].astype(F32) * inv, cols[:, None].astype(F32) * inv], -1)
    cos = jnp.cos(ang)[None, :, None, :]
    sin = jnp.sin(ang)[None, :, None, :]
    xp = x.astype(F32).reshape(x.shape[:-1] + (half, 2))
    x1, x2 = xp[..., 0], xp[..., 1]
    out = jnp.stack([x1 * cos - x2 * sin, x1 * sin + x2 * cos], -1).reshape(x.shape)
    return out.astype(x.dtype)


def attn_q(p_q, q_norm):
    B, T, _ = p_q.shape
    return rms_norm(p_q.reshape(B, T, ATTN_Q_HEADS, HEAD_DIM), q_norm)


def attn_kv(p_kv, k_norm):
    B, T, _ = p_kv.shape
    k = rms_norm(p_kv[..., :KV_W].reshape(B, T, ATTN_KV_HEADS, HEAD_DIM), k_norm)
    v = p_kv[..., KV_W:].reshape(B, T, ATTN_KV_HEADS, HEAD_DIM)
    return k, v


def blocked_attention(q, k, v):
    B, T, H, Dh = q.shape
    G = k.shape[2]
    R = H // G
    nb = T // Q_BLOCK
    qb = q.reshape(B, nb, Q_BLOCK, G, R, Dh).transpose(1, 0, 2, 3, 4, 5)
    scale = Dh ** -0.5

    def one_block(qblk):
        s = jnp.einsum('bqgrd,bkgd->bgrqk', qblk, k).astype(F32) * scale
        pr = jax.nn.softmax(s, axis=-1).astype(v.dtype)
        return jnp.einsum('bgrqk,bkgd->bqgrd', pr, v)

    o = lax.map(one_block, qb)
    return o.transpose(1, 0, 2, 3, 4, 5).reshape(B, T, H * Dh)


def attention_mixer(p_ctx_kv, p_ctx_q, p_lat, q_norm, k_norm, rows, cols, ctx_out):
    kc, vc = attn_kv(p_ctx_kv, k_norm)
    ql = rope_2d(attn_q(p_lat[..., :Q_W], q_norm), rows, cols)
    kl, vl = attn_kv(p_lat[..., Q_W:], k_norm)
    kl = rope_2d(kl, rows, cols)
    y_lat = blocked_attention(ql, jnp.concatenate([kc, kl], 1), jnp.concatenate([vc, vl], 1))
    y_ctx = blocked_attention(attn_q(p_ctx_q, q_norm), kc, vc) if ctx_out else None
    return y_ctx, y_lat


def merge_branches(ys, gate_proj, w_branch, w_out):
    B, T, _ = gate_proj.shape
    gates = jax.nn.sigmoid(gate_proj.astype(F32)).reshape(B, T, N_BRANCHES, D_MODEL).astype(gate_proj.dtype)
    merged = gates[:, :, 0] * (ys[0] @ w_branch[0])
    for n in range(1, N_BRANCHES):
        merged = merged + gates[:, :, n] * (ys[n] @ w_branch[n])
    return merged @ w_out


def moe(h, router_w, router_bias, w1, w3, w2):
    B, T, D = h.shape
    hf = h.reshape(-1, D)
    N = hf.shape[0]
    s = jax.nn.sigmoid((hf @ router_w).astype(F32))
    sel = s + router_bias.astype(F32)
    grp_score = lax.top_k(sel.reshape(N, N_GROUPS, EXPERTS_PER_GROUP), 2)[0].sum(-1)
    g_idx = jnp.argmax(grp_score, axis=-1)
    in_group = jnp.repeat(jax.nn.one_hot(g_idx, N_GROUPS, dtype=F32) > 0, EXPERTS_PER_GROUP, axis=-1)
    _, idx = lax.top_k(jnp.where(in_group, sel, -jnp.inf), TOP_K)
    wts = jnp.take_along_axis(s, idx, axis=-1)
    wts = wts / jnp.sum(wts, -1, keepdims=True)
    gates = jnp.einsum('nk,nke->ne', wts, jax.nn.one_hot(idx, N_EXPERTS, dtype=F32)).astype(h.dtype)
    out = jnp.zeros_like(hf)
    for e in range(N_EXPERTS):
        act = jax.nn.silu(hf @ w1[e]) * (hf @ w3[e])
        out = out + gates[:, e:e + 1] * (act @ w2[e])
    return out.reshape(B, T, D)


def setup_inputs(seed: int = 0) -> dict:
    key = jax.random.key(seed)
    ks = iter(jax.random.split(key, 64))

    def nrm(shape, s):
        return jax.random.normal(next(ks), shape, F32) * s

    def uni(shape, lo, hi):
        return jax.random.uniform(next(ks), shape, F32, lo, hi)

    C = BRANCH_W
    Hd = HYENA_FILTER_HIDDEN
    return {
        "x": nrm((BATCH, SEQ, D_MODEL), 1.0),
        "c": nrm((BATCH, D_MODEL), 1.0),
        "ctx": nrm((BATCH, CTX_LEN, D_MODEL), 1.0),
        "c_ctx": nrm((D_MODEL,), 1.0),
        "ada_w": nrm((DEPTH, D_MODEL, N_MOD * D_MODEL), 0.5 * D_MODEL ** -0.5),
        "ada_b": nrm((DEPTH, N_MOD * D_MODEL), 0.02),
        "w_in": nrm((DEPTH, D_MODEL, IN_COLS), D_MODEL ** -0.5),
        "rwkv_mu": uni((DEPTH, RWKV_COLS), 0.0, 1.0),
        "rwkv_w0": uni((DEPTH, 2, C), -6.0, -1.0),
        "rwkv_w_up": nrm((DEPTH, 2, RWKV_DECAY_LORA, C), RWKV_DECAY_LORA ** -0.5),
        "rwkv_a0": nrm((DEPTH, 2, C), 0.1),
        "rwkv_a_up": nrm((DEPTH, 2, RWKV_A_LORA, C), 0.5 * RWKV_A_LORA ** -0.5),
        "rwkv_g_up": nrm((DEPTH, RWKV_G_LORA, C), RWKV_G_LORA ** -0.5),
        "rwkv_k_k": 0.85 + nrm((DEPTH, C), 0.05),
        "rwkv_k_a": 1.0 + nrm((DEPTH, C), 0.05),
        "rwkv_r_k": nrm((DEPTH, C), 0.1),
        "rwkv_lnx_g": 1.0 + nrm((DEPTH, C), 0.05),
        "rwkv_lnx_b": nrm((DEPTH, C), 0.02),
        "hyena_conv": nrm((DEPTH, 3, HYENA_COLS), 3 ** -0.5),
        "hyena_w1": nrm((DEPTH, HYENA_EMB, Hd), HYENA_EMB ** -0.5),
        "hyena_b1": nrm((DEPTH, Hd), 0.1),
        "hyena_freq1": 1.0 + nrm((DEPTH, Hd), 0.05),
        "hyena_w2": nrm((DEPTH, Hd, Hd), Hd ** -0.5),
        "hyena_b2": nrm((DEPTH, Hd), 0.1),
        "hyena_freq2": 1.0 + nrm((DEPTH, Hd), 0.05),
        "hyena_w3": nrm((DEPTH, Hd, 2 * C), 0.1 * Hd ** -0.5),
        "hyena_skip": nrm((DEPTH, C), 0.5),
        "sconv_w": nrm((DEPTH, 3, C), 3 ** -0.5),
        "attn_q_norm": 1.0 + nrm((DEPTH, HEAD_DIM), 0.05),
        "attn_k_norm": 1.0 + nrm((DEPTH, HEAD_DIM), 0.05),
        "w_branch": nrm((DEPTH, N_BRANCHES, C, D_MODEL), C ** -0.5),
        "w_out": nrm((DEPTH, D_MODEL, D_MODEL), BETA * D_MODEL ** -0.5),
        "ln1_g": 1.0 + nrm((DEPTH, D_MODEL), 0.05),
        "ln1_b": nrm((DEPTH, D_MODEL), 0.02),
        "ln2_g": 1.0 + nrm((DEPTH, D_MODEL), 0.05),
        "ln2_b": nrm((DEPTH, D_MODEL), 0.02),
        "router_w": nrm((D_MODEL, N_EXPERTS), D_MODEL ** -0.5),
        "router_bias": nrm((N_EXPERTS,), 0.01),
        "exp_w1": nrm((DEPTH, N_EXPERTS, D_MODEL, D_EXPERT), D_MODEL ** -0.5),
        "exp_w3": nrm((DEPTH, N_EXPERTS, D_MODEL, D_EXPERT), D_MODEL ** -0.5),
        "exp_w2": nrm((DEPTH, N_EXPERTS, D_EXPERT, D_MODEL), BETA * D_EXPERT ** -0.5),
    }


def reference(x, c, ctx, c_ctx, ada_w, ada_b, w_in, rwkv_mu, rwkv_w0, rwkv_w_up, rwkv_a0, rwkv_a_up,
              rwkv_g_up, rwkv_k_k, rwkv_k_a, rwkv_r_k, rwkv_lnx_g, rwkv_lnx_b, hyena_conv, hyena_w1,
              hyena_b1, hyena_freq1, hyena_w2, hyena_b2, hyena_freq2, hyena_w3, hyena_skip, sconv_w,
              attn_q_norm, attn_k_norm, w_branch, w_out, ln1_g, ln1_b, ln2_g, ln2_b, router_w,
              router_bias, exp_w1, exp_w3, exp_w2):
    L = x.shape[1]
    Lc = ctx.shape[1]
    n_rows = L // GRID_W
    rows = jnp.repeat(jnp.arange(n_rows), GRID_W)
    cols = jnp.tile(jnp.arange(GRID_W), n_rows)
    splits = (OFF_HYENA, OFF_SCONV, OFF_ATTN, OFF_GATE)
    for l in range(DEPTH):
        ctx_out = l < DEPTH - 1
        mod = jax.nn.silu(c) @ ada_w[l] + ada_b[l]
        mod_c = jax.nn.silu(c_ctx) @ ada_w[l] + ada_b[l]
        sh_a, sc_a, g_a, sh_f, sc_f, g_f = jnp.split(mod[:, None, :], N_MOD, axis=-1)
        sh_ac, sc_ac, g_ac, sh_fc, sc_fc, g_fc = jnp.split(mod_c[None, None, :], N_MOD, axis=-1)
        h = modulate(x, sh_a, sc_a)
        hc = modulate(ctx, sh_ac, sc_ac)
        wl = w_in[l]
        pa, pb, pcv, pd, pg = jnp.split(h @ wl, splits, axis=-1)
        if ctx_out:
            pa_c, pb_c, pcv_c, pd_c, pg_c = jnp.split(hc @ wl, splits, axis=-1)
            pd_c_q, pd_c_kv = pd_c[..., :Q_W], pd_c[..., Q_W:]
        else:
            pa_c = hc @ wl[:, :OFF_HYENA]
            pd_c_kv = hc @ wl[:, OFF_ATTN + Q_W:OFF_GATE]
            pd_c_q = None
        ya_c, ya = rwkv_mixer(pa_c, pa, rwkv_mu[l], rwkv_w0[l], rwkv_w_up[l], rwkv_a0[l], rwkv_a_up[l],
                              rwkv_g_up[l], rwkv_k_k[l], rwkv_k_a[l], rwkv_r_k[l], rwkv_lnx_g[l],
                              rwkv_lnx_b[l], ctx_out)
        yd_c, yd = attention_mixer(pd_c_kv, pd_c_q, pd, attn_q_norm[l], attn_k_norm[l], rows, cols, ctx_out)
        filt = (hyena_w1[l], hyena_b1[l], hyena_freq1[l], hyena_w2[l], hyena_b2[l], hyena_freq2[l], hyena_w3[l])
        yb = hyena_mixer(pb, hyena_conv[l], hyena_skip[l], *filt)
        ycv = short_conv_mixer(pcv, sconv_w[l])
        y = merge_branches((ya, yb, ycv, yd), pg, w_branch[l], w_out[l])
        x = layer_norm(ALPHA * x + g_a * y, ln1_g[l], ln1_b[l])
        hf = modulate(x, sh_f, sc_f)
        if ctx_out:
            yb_c = hyena_mixer(pb_c, hyena_conv[l], hyena_skip[l], *filt)
            ycv_c = short_conv_mixer(pcv_c, sconv_w[l])
            y_c = merge_branches((ya_c, yb_c, ycv_c, yd_c), pg_c, w_branch[l], w_out[l])
            ctx = layer_norm(ALPHA * ctx + g_ac * y_c, ln1_g[l], ln1_b[l])
            hfc = modulate(ctx, sh_fc, sc_fc)
            f_all = moe(jnp.concatenate([hfc, hf], axis=1), router_w, router_bias, exp_w1[l], exp_w3[l], exp_w2[l])
            ctx = layer_norm(ALPHA * ctx + g_fc * f_all[:, :Lc], ln2_g[l], ln2_b[l])
            x = layer_norm(ALPHA * x + g_f * f_all[:, Lc:], ln2_g[l], ln2_b[l])
        else:
            f_lat = moe(hf, router_w, router_bias, exp_w1[l], exp_w3[l], exp_w2[l])
            x = layer_norm(ALPHA * x + g_f * f_lat, ln2_g[l], ln2_b[l])
    return x
```

```python
from contextlib import ExitStack
import math
import numpy as np
import concourse.bass as bass
import concourse.mybir as mybir
from concourse.bass_utils import run_bass_kernel_spmd

F32 = mybir.dt.float32
BF16 = mybir.dt.bfloat16
I32 = mybir.dt.int32
AF = mybir.ActivationFunctionType
ALU = mybir.AluOpType
AX = mybir.AxisListType


class Buf:
    __slots__ = ("name", "w", "r", "excl")

    def __init__(self, name="", excl=False):
        self.name = name
        self.w = {}
        self.r = {}
        self.excl = excl


class Prog:
    CE = ["pe", "dve", "act", "pool", "sp"]
    SAME = {"pe": False, "dve": True, "act": True, "pool": True, "sp": False}
    NDQ = 6

    def __init__(self, nc):
        self.nc = nc
        self.ops = {e: [] for e in self.CE}
        self.cnt = {}
        self.sem = {}
        self.seen = {e: {} for e in self.CE}
        for e in self.CE:
            self.sem[e] = nc.alloc_semaphore("sem_" + e)
            self.cnt[e] = 0
        self.dq = {}
        self.dq_next = {}
        for q in ("sp", "pool", "act"):
            names = []
            for i in range(self.NDQ):
                n = "dq_%s_%d" % (q, i)
                self.sem[n] = nc.alloc_semaphore("sem_" + n)
                self.cnt[n] = 0
                names.append(n)
            self.dq[q] = names
            self.dq_next[q] = 0
        self.nins = 0

    def _mult(self, e):
        return 16 if e.startswith("dq_") else 1

    def _waits(self, eng, reads, writes, extra=()):
        need = {}

        def add(e2, c):
            if c > need.get(e2, 0):
                need[e2] = c
        for b in reads:
            for e2, c in b.w.items():
                add(e2, c)
            if b.excl:
                for e2, c in b.r.items():
                    if e2 != eng:
                        add(e2, c)
        for b in writes:
            for e2, c in b.w.items():
                add(e2, c)
            for e2, c in b.r.items():
                add(e2, c)
        for e2, c in extra:
            add(e2, c)
        out = []
        seen = self.seen[eng]
        for e2, c in need.items():
            if e2 == eng and not self.SAME[eng]:
                continue
            if c > seen.get(e2, 0):
                seen[e2] = c
                out.append((self.sem[e2], c * self._mult(e2)))
        return out

    def op(self, eng, fn, reads=(), writes=(), inc=True):
        waits = self._waits(eng, reads, writes)
        if inc:
            self.cnt[eng] += 1
            my = self.cnt[eng]
        else:
            my = self.cnt[eng] + 1
        self.ops[eng].append((waits, fn, self.sem[eng] if inc else None, 1))
        for b in reads:
            b.r[eng] = max(b.r.get(eng, 0), my)
        for b in writes:
            b.w[eng] = my
            b.r = {}
        self.nins += 1

    def dma(self, q, fn, reads=(), writes=()):
        names = self.dq[q]
        n = names[self.dq_next[q] % len(names)]
        self.dq_next[q] += 1
        extra = [(n, self.cnt[n])] if self.cnt[n] > 0 else []
        waits = self._waits(q, reads, writes, extra)
        self.cnt[n] += 1
        my = self.cnt[n]
        self.ops[q].append((waits, fn, self.sem[n], 16))
        for b in reads:
            b.r[n] = max(b.r.get(n, 0), my)
        for b in writes:
            b.w[n] = my
            b.r = {}
        self.nins += 1

    def finish(self, final_bufs):
        waits = self._waits("sp", final_bufs, ())
        self.ops["sp"].append((waits, None, None, 0))
        nc = self.nc
        ops = self.ops
        with nc.Block() as block:
            def emit(engobj, lst):
                for waits, fn, sem, k in lst:
                    for s, v in waits:
                        engobj.wait_ge(s, v)
                    if fn is not None:
                        ins = fn(engobj)
                        if sem is not None:
                            ins.then_inc(sem, k)

            @block.tensor
            def _(e):
                emit(e, ops["pe"])

            @block.vector
            def _(e):
                emit(e, ops["dve"])

            @block.scalar
            def _(e):
                emit(e, ops["act"])

            @block.gpsimd
            def _(e):
                emit(e, ops["pool"])

            @block.sync
            def _(e):
                emit(e, ops["sp"])


D = 1024
L = 4096
LC = 256
NTOK = L + LC
NTILE = NTOK // 128
NTP = NTOK + 4
CTX0 = 1
LAT0 = 259
DEPTH = 2
IN_COLS = 7168
OFF_HYENA = 1024
OFF_SCONV = 1792
OFF_ATTN = 2560
OFF_GATE = 3072
NE = 16
DE = 512
ALPHA = (2 * DEPTH) ** 0.25
LN_EPS = 1e-6
PI = math.pi
TWO_PI = 2.0 * math.pi
MAGIC = 12582912.0
CH = 64
SBUF_LIMIT = 208 * 1024


def tok_col(t):
    return CTX0 + t if t < LC else LAT0 + (t - LC)


class K:
    def __init__(self, nc, dbg=False):
        self.nc = nc
        self.P = Prog(nc)
        self.dbg = dbg
        self.dram = {}
        self.dbuf = {}

    def din(self, name, shape, dt=F32):
        t = self.nc.dram_tensor(name, list(shape), dt, kind="ExternalInput")
        self.dram[name] = t
        self.dbuf[name] = Buf(name)
        return t.ap()

    def dscr(self, name, shape, dt=F32, out=False):
        kind = "ExternalOutput" if (out or self.dbg) else "Internal"
        t = self.nc.dram_tensor(name, list(shape), dt, kind=kind)
        self.dram[name] = t
        self.dbuf[name] = Buf(name)
        return t.ap()

    def sb(self, es, name, shape, dt=F32):
        self._uid = getattr(self, "_uid", 0) + 1
        t = es.enter_context(self.nc.sbuf_tensor("%s_u%d" % (name, self._uid), list(shape), dt))
        nb = 1
        for d_ in shape[1:]:
            nb *= d_
        nb *= 2 if dt == BF16 else 4
        nb = (nb + 31) // 32 * 32
        self.sb_used = getattr(self, "sb_used", 17 * 1024) + nb
        self.sb_peak = max(getattr(self, "sb_peak", 0), self.sb_used)

        def _free(nb=nb):
            self.sb_used -= nb
        es.callback(_free)
        if self.sb_used > SBUF_LIMIT:
            raise RuntimeError("SBUF budget exceeded at %s: %d" % (name, self.sb_used))
        return t

    def op(self, eng, fn, r=(), w=(), inc=True):
        self.P.op(eng, fn, r, w, inc)

    def dma(self, q, out, in_, r=(), w=(), **kw):
        self.P.dma(q, lambda e: e.dma_start(out=out, in_=in_, **kw), r, w)

    def copy(self, eng, out, in_, r, w):
        if eng == "act":
            self.op("act", lambda e: e.activation(out=out, in_=in_, func=AF.Copy), r, w)
        else:
            self.op(eng, lambda e: e.tensor_copy(out=out, in_=in_), r, w)

    def act(self, out, in_, func, r, w, bias=0.0, scale=1.0):
        self.op("act", lambda e: e.activation(out=out, in_=in_, func=func, bias=bias, scale=scale), r, w)

    def ts(self, eng, out, in0, s1, s2, op0, op1, r, w):
        if op1 is None:
            self.op(eng, lambda e: e.tensor_scalar(out=out, in0=in0, scalar1=s1, scalar2=None, op0=op0), r, w)
        else:
            self.op(eng, lambda e: e.tensor_scalar(out=out, in0=in0, scalar1=s1, scalar2=s2, op0=op0, op1=op1), r, w)

    def tt(self, eng, out, in0, in1, op, r, w):
        self.op(eng, lambda e: e.tensor_tensor(out=out, in0=in0, in1=in1, op=op), r, w)

    def stt(self, out, in0, scalar, in1, op0, op1, r, w):
        self.op("dve", lambda e: e.scalar_tensor_tensor(out=out, in0=in0, scalar=scalar, in1=in1, op0=op0, op1=op1), r, w)

    def mm(self, out, lhsT, rhs, start, stop, r, w, inc=None):
        if inc is None:
            inc = stop
        self.op("pe", lambda e: e.matmul(out, lhsT=lhsT, rhs=rhs, start=start, stop=stop), r, w, inc)

    def tr(self, out, in_, ident, r, w, inc=True):
        self.op("pe", lambda e: e.transpose(out=out, in_=in_, identity=ident), r, w, inc)

    def memset(self, eng, ap, val, w):
        self.op(eng, lambda e: e.memset(ap, val), (), w)

    def barrier(self):
        P = self.P
        allc = [(e, c) for e, c in P.cnt.items() if c > 0]
        for eng in P.CE:
            waits = []
            for e2, c in allc:
                if e2 == eng and not P.SAME[eng]:
                    continue
                if c > P.seen[eng].get(e2, 0):
                    P.seen[eng][e2] = c
                    waits.append((P.sem[e2], c * P._mult(e2)))
            if waits:
                P.ops[eng].append((waits, None, None, 0))

    def range_reduce(self, x, tmp, r, w):
        bufs = list(set(list(r) + list(w)))
        self.ts("dve", tmp, x, 1.0 / TWO_PI, MAGIC, ALU.mult, ALU.add, bufs, bufs)
        self.ts("dve", tmp, tmp, -MAGIC, None, ALU.add, None, bufs, bufs)
        self.stt(x, tmp, -TWO_PI, x, ALU.mult, ALU.add, bufs, bufs)
        self.ts("dve", tmp, x, PI, -TWO_PI, ALU.is_gt, ALU.mult, bufs, bufs)
        self.tt("dve", x, x, tmp, ALU.add, bufs, bufs)
        self.ts("dve", tmp, x, -PI, TWO_PI, ALU.is_lt, ALU.mult, bufs, bufs)
        self.tt("dve", x, x, tmp, ALU.add, bufs, bufs)
        self.ts("dve", x, x, -PI, PI, ALU.max, ALU.min, bufs, bufs)

    def setup(self):
        nc = self.nc
        A = {}
        self.A = A
        A["x"] = self.din("x", [L, D])
        A["c"] = self.din("c", [1, D])
        A["ctx"] = self.din("ctx", [LC, D])
        A["c_ctx"] = self.din("c_ctx", [1, D])
        A["ada_w"] = self.din("ada_w", [DEPTH, D, 6 * D])
        A["ada_b"] = self.din("ada_b", [DEPTH, 6 * D])
        A["w_in"] = self.din("w_in", [DEPTH, D, IN_COLS])
        A["rwkv_mu"] = self.din("rwkv_mu", [DEPTH, 1024])
        A["rwkv_w0"] = self.din("rwkv_w0", [DEPTH, 2, 256])
        A["rwkv_w_up"] = self.din("rwkv_w_up", [DEPTH, 2, 64, 256])
        A["rwkv_a0"] = self.din("rwkv_a0", [DEPTH, 2, 256])
        A["rwkv_a_up"] = self.din("rwkv_a_up", [DEPTH, 2, 64, 256])
        A["rwkv_g_up"] = self.din("rwkv_g_up", [DEPTH, 128, 256])
        for n in ("rwkv_k_k", "rwkv_k_a", "rwkv_r_k", "rwkv_lnx_g", "rwkv_lnx_b", "hyena_skip"):
            A[n] = self.din(n, [DEPTH, 256])
        A["hyena_conv"] = self.din("hyena_conv", [DEPTH, 3, 768])
        A["hyena_w1"] = self.din("hyena_w1", [DEPTH, 33, 64])
        A["hyena_b1"] = self.din("hyena_b1", [DEPTH, 64])
        A["hyena_freq1"] = self.din("hyena_freq1", [DEPTH, 64])
        A["hyena_w2"] = self.din("hyena_w2", [DEPTH, 64, 64])
        A["hyena_b2"] = self.din("hyena_b2", [DEPTH, 64])
        A["hyena_freq2"] = self.din("hyena_freq2", [DEPTH, 64])
        A["hyena_w3"] = self.din("hyena_w3", [DEPTH, 64, 512])
        A["sconv_w"] = self.din("sconv_w", [DEPTH, 3, 256])
        A["attn_q_norm"] = self.din("attn_q_norm", [DEPTH, 64])
        A["attn_k_norm"] = self.din("attn_k_norm", [DEPTH, 64])
        A["w_branch"] = self.din("w_branch", [DEPTH, 4, 256, D])
        A["w_out"] = self.din("w_out", [DEPTH, D, D])
        for n in ("ln1_g", "ln1_b", "ln2_g", "ln2_b"):
            A[n] = self.din(n, [DEPTH, D])
        A["router_w"] = self.din("router_w", [D, NE])
        A["router_bias"] = self.din("router_bias", [1, NE])
        A["exp_w1"] = self.din("exp_w1", [DEPTH, NE, D, DE])
        A["exp_w3"] = self.din("exp_w3", [DEPTH, NE, D, DE])
        A["exp_w2"] = self.din("exp_w2", [DEPTH, NE, DE, D])
        A["rope_cs"] = self.din("rope_cs", [L, 64])
        A["hy_z"] = self.din("hy_z", [33, 2 * L])
        A["hy_zc"] = self.din("hy_zc", [33, 2 * LC])
        A["hy_t"] = self.din("hy_t", [1, 2 * L])
        A["hy_tc"] = self.din("hy_tc", [1, 2 * LC])
        A["hy_delta"] = self.din("hy_delta", [256, 1])
        A["out"] = self.dscr("out", [L, D], F32, out=True)
        A["xres"] = self.dscr("xres", [NTOK, D], F32)
        A["hT"] = self.dscr("hT", [128, 8 * NTP], BF16)
        A["hfT"] = self.dscr("hfT", [128, 8 * NTOK], BF16)
        A["ysT"] = self.dscr("ysT", [128, 8 * NTOK], BF16)
        A["rk_fm"] = self.dscr("rk_fm", [2 * 4 * 256, NTOK], F32)
        A["rk_tm"] = self.dscr("rk_tm", [2 * NTOK, 2 * 256], F32)
        A["rk_v"] = self.dscr("rk_v", [NTOK, 256], F32)
        A["rk_gc"] = self.dscr("rk_gc", [2 * 256, NTOK // 64], F32)
        A["rk_g"] = self.dscr("rk_g", [256, NTOK], F32)
        A["rk_bonus"] = self.dscr("rk_bonus", [256, NTOK], F32)
        A["rk_y"] = self.dscr("rk_y", [2 * NTOK, 256], F32)
        A["ewb1"] = self.dscr("ewb1", [NE * D, DE], BF16)
        A["ewb3"] = self.dscr("ewb3", [NE * D, DE], BF16)
        A["ewb2"] = self.dscr("ewb2", [NE * DE, D], BF16)
        A["hyG"] = self.dscr("hyG", [256, 2 * L], BF16)
        A["hyGc"] = self.dscr("hyGc", [256, 2 * LC], BF16)
        self.hT3 = A["hT"].rearrange("p (k n) -> p k n", k=8)
        self.hfT3 = A["hfT"].rearrange("p (k n) -> p k n", k=8)
        self.ysT3 = A["ysT"].rearrange("p (k n) -> p k n", k=8)

        self.ges = ExitStack()
        es = self.ges
        self.ident_b = self.sb(es, "ident_b", [128, 128], BF16); self.b_ident_b = Buf()
        self.ident_f = self.sb(es, "ident_f", [128, 128], F32); self.b_ident_f = Buf()
        self.flip_b = self.sb(es, "flip_b", [128, 128], BF16); self.b_flip_b = Buf()
        self.ones_f = self.sb(es, "ones_f", [128, 128], F32); self.b_ones_f = Buf()
        self.blk_f = self.sb(es, "blk_f", [128, 128], F32); self.b_blk_f = Buf()
        self.sel2 = self.sb(es, "sel2", [2, 2, 128], F32); self.b_sel2 = Buf()
        self.modT = self.sb(es, "modT", [128, 48, 2], F32); self.b_modT = Buf()
        self.gbc = self.sb(es, "gbc", [128, 4, D], F32); self.b_gbc = Buf()
        self.lnbc = self.sb(es, "lnbc", [128, 4, D], F32); self.b_lnbc = Buf()
        self.gates = self.sb(es, "gates", [128, NTILE, NE], F32); self.b_gates = Buf()
        self.rw32 = self.sb(es, "rw32", [128, 8, NE], F32); self.b_rw32 = Buf()
        self.rbias = self.sb(es, "rbias", [128, NE], F32); self.b_rbias = Buf()
        self.psf = []
        for i in range(6):
            t = nc.alloc_psum_tensor("psf%d" % i, [128, 512], F32)
            self.psf.append((t, Buf("psf%d" % i, excl=True)))
        self.psb = []
        for i in range(2):
            t = nc.alloc_psum_tensor("psb%d" % i, [128, 1024], BF16)
            self.psb.append((t, Buf("psb%d" % i, excl=True)))

        self.psb_f32 = [(t_[:].bitcast(F32), b_) for (t_, b_) in self.psb]
        G = "pool"
        tmpf = self.sb(es, "tmp_idf", [128, 128], F32); b_tmp = Buf()
        self.memset(G, self.ident_f[:], 0.0, [self.b_ident_f])
        self.op(G, lambda e: e.affine_select(out=self.ident_f[:], in_=self.ident_f[:], pattern=[[-1, 128]],
                                              compare_op=ALU.not_equal, fill=1.0, base=0, channel_multiplier=1),
                [self.b_ident_f], [self.b_ident_f])
        self.copy("dve", self.ident_b[:], self.ident_f[:], [self.b_ident_f], [self.b_ident_b])
        self.memset(G, tmpf[:], 0.0, [b_tmp])
        self.op(G, lambda e: e.affine_select(out=tmpf[:], in_=tmpf[:], pattern=[[1, 128]],
                                              compare_op=ALU.not_equal, fill=1.0, base=-127, channel_multiplier=1),
                [b_tmp], [b_tmp])
        self.copy("dve", self.flip_b[:], tmpf[:], [b_tmp], [self.b_flip_b])
        self.memset(G, self.ones_f[:], 1.0, [self.b_ones_f])
        self.memset(G, self.blk_f[:], 0.0, [self.b_blk_f])
        self.memset(G, self.blk_f[0:64, 0:64], 1.0, [self.b_blk_f])
        self.memset(G, self.blk_f[64:128, 64:128], 1.0, [self.b_blk_f])
        self.memset(G, self.sel2[:], 0.0, [self.b_sel2])
        self.op(G, lambda e: e.affine_select(out=self.sel2[:], in_=self.sel2[:], pattern=[[-1, 2], [0, 128]],
                                              compare_op=ALU.not_equal, fill=1.0, base=0, channel_multiplier=1),
                [self.b_sel2], [self.b_sel2])
        self.dma("sp", self.rw32[:], A["router_w"].rearrange("(k p) e -> p k e", p=128), [], [self.b_rw32])
        self.dma("sp", self.rbias[:], A["router_bias"][0:1, :].partition_broadcast(128), [], [self.b_rbias])
        bx = self.dbuf["xres"]
        self.dma("sp", A["xres"][0:LC, :], A["ctx"][:, :], [], [bx])
        self.dma("sp", A["xres"][LC:NTOK, :], A["x"][:, :], [], [bx])
        zt = self.sb(es, "zpad", [128, 8, 2], BF16); bz = Buf()
        self.memset(G, zt[:], 0.0, [bz])
        bh = self.dbuf["hT"]
        self.dma("sp", self.hT3[:, :, 0:1], zt[:, :, 0:1], [bz], [bh], allow_slow_non_contiguous=True)
        self.dma("sp", self.hT3[:, :, 257:259], zt[:, :, 0:2], [bz], [bh], allow_slow_non_contiguous=True)
        self.dma("sp", self.hT3[:, :, NTP - 1:NTP], zt[:, :, 0:1], [bz], [bh], allow_slow_non_contiguous=True)

    def phase_mod(self, l):
        A = self.A
        with ExitStack() as es:
            self.modrow = self.sb(es, "modrow", [2, 6 * D], F32); self.b_modrow = Buf()
            cT = self.sb(es, "cT", [128, 8, 2], F32); b_cT = Buf()
            sT = self.sb(es, "sT", [128, 8, 2], F32); b_sT = Buf()
            brow = self.sb(es, "brow", [1, 6 * D], F32); b_brow = Buf()
            wch = [self.sb(es, "adaw%d" % i, [128, 8, 512], F32) for i in range(2)]
            b_wch = [Buf(), Buf()]
            self.dma("sp", cT[:, :, 0], A["c"].rearrange("o (k p) -> p (o k)", p=128), [], [b_cT],
                     allow_slow_non_contiguous=True)
            self.dma("sp", cT[:, :, 1], A["c_ctx"].rearrange("o (k p) -> p (o k)", p=128), [], [b_cT],
                     allow_slow_non_contiguous=True)
            self.dma("sp", brow[:], A["ada_b"][l:l + 1, :], [], [b_brow])
            self.act(sT[:], cT[:], AF.Silu, [b_cT], [b_sT])
            for j in range(12):
                w, bw = wch[j % 2], b_wch[j % 2]
                self.dma("sp", w[:], A["ada_w"][l, :, j * 512:(j + 1) * 512].rearrange("(k p) n -> p k n", p=128),
                         [], [bw])
                ps, bp = self.psf[j % 2]
                for k in range(8):
                    self.mm(ps[0:2, :], sT[:, k, :], w[:, k, :], k == 0, False, [b_sT, bw], [bp], inc=False)
                self.mm(ps[0:2, :], self.ones_f[0:1, 0:2], brow[0:1, j * 512:(j + 1) * 512], False, True,
                        [self.b_ones_f, b_brow], [bp])
                self.copy("act", self.modrow[0:2, j * 512:(j + 1) * 512], ps[0:2, :], [bp], [self.b_modrow])
            for j in range(48):
                ps, bp = self.psf[2 + j % 2]
                self.tr(ps[:, 0:2], self.modrow[0:2, j * 128:(j + 1) * 128], self.ident_f[0:2, 0:2],
                        [self.b_modrow, self.b_ident_f], [bp])
                self.copy("dve", self.modT[:, j, :], ps[:, 0:2], [bp], [self.b_modT])
            for base in (8, 32):
                self.ts("dve", self.modT[:, base:base + 8, :], self.modT[:, base:base + 8, :], 1.0, None,
                        ALU.add, None, [self.b_modT], [self.b_modT])
            i = 0
            for gi, col0 in ((0, 2 * D), (2, 5 * D)):
                for r_ in range(2):
                    for hh in range(2):
                        ps, bp = self.psf[4 + i % 2]
                        i += 1
                        self.mm(ps[:, :], self.sel2[0:2, r_, :], self.modrow[0:2, col0 + hh * 512:col0 + (hh + 1) * 512],
                                True, True, [self.b_sel2, self.b_modrow], [bp])
                        self.copy("act", self.gbc[:, gi + r_, hh * 512:(hh + 1) * 512], ps[:, :], [bp], [self.b_gbc])
            for i_, n in enumerate(("ln1_g", "ln1_b", "ln2_g", "ln2_b")):
                self.dma("sp", self.lnbc[:, i_, :], A[n][l:l + 1, :].partition_broadcast(128), [], [self.b_lnbc])
            self.barrier()

    def ln_stats(self, xt, bx, st, mv, rs, bs):
        for i in range(2):
            self.op("dve", lambda e, i=i: e.bn_stats(out=st[:, i, :], in_=xt[:, i * 512:(i + 1) * 512]), [bx], [bs])
        self.op("dve", lambda e: e.bn_aggr(out=mv[:], in_=st[:].rearrange("p a b -> p (a b)")), [bs], [bs])
        self.act(rs[:], mv[:, 1:2], AF.Sqrt, [bs], [bs], bias=LN_EPS, scale=1.0)
        self.op("dve", lambda e: e.reciprocal(out=rs[:], in_=rs[:]), [bs], [bs])

    def phase_h(self, l, tiles):
        A = self.A
        bxres = self.dbuf["xres"]
        bhT = self.dbuf["hT"]
        with ExitStack() as es:
            NB = 3
            xt = [self.sb(es, "h_x%d" % i, [128, D], F32) for i in range(NB)]
            bxt = [Buf() for _ in range(NB)]
            xn = [self.sb(es, "h_xn%d" % i, [128, D], BF16) for i in range(NB)]
            bxn = [Buf() for _ in range(NB)]
            st = [self.sb(es, "h_st%d" % i, [128, 2, 6], F32) for i in range(NB)]
            mv = [self.sb(es, "h_mv%d" % i, [128, 2], F32) for i in range(NB)]
            rs = [self.sb(es, "h_rs%d" % i, [128, 1], F32) for i in range(NB)]
            bs = [Buf() for _ in range(NB)]
            ho = [self.sb(es, "h_o%d" % i, [128, 8, 128], BF16) for i in range(NB)]
            bho = [Buf() for _ in range(NB)]
            def tile_gen(n, ti):
                s = n % NB
                r_ = 1 if ti < 2 else 0
                col = tok_col(ti * 128)
                self.dma("sp", xt[s][:], A["xres"][ti * 128:(ti + 1) * 128, :], [bxres], [bxt[s]])
                yield
                self.ln_stats(xt[s], bxt[s], st[s], mv[s], rs[s], bs[s])
                yield
                self.ts("dve", xn[s][:], xt[s][:], mv[s][:, 0:1], rs[s][:, 0:1], ALU.subtract, ALU.mult,
                        [bxt[s], bs[s]], [bxn[s]])
                yield True
                pt, bpt = self.psb[n % 2]
                for k in range(8):
                    self.tr(pt[:, k * 128:(k + 1) * 128], xn[s][:, k * 128:(k + 1) * 128], self.ident_b[:],
                            [bxn[s], self.b_ident_b], [bpt], inc=(k == 7))
                yield
                for k in range(8):
                    self.act(ho[s][:, k, :], pt[:, k * 128:(k + 1) * 128], AF.Identity, [bpt, self.b_modT], [bho[s]],
                             bias=self.modT[:, k, r_:r_ + 1], scale=self.modT[:, 8 + k, r_:r_ + 1])
                yield
                self.dma("sp", self.hT3[:, :, col:col + 128], ho[s][:], [bho[s]], [bhT])
                yield

            run_pipeline((tile_gen(n, ti) for n, ti in enumerate(tiles)), depth=3)
            self.barrier()


def const_tables():
    f32 = np.float32
    t = np.arange(L)
    rows = (t // 64).astype(f32)
    cols = (t % 64).astype(f32)
    inv = (np.float32(10000.0) ** (-np.arange(0, 32, 2, dtype=f32) / np.float32(32))).astype(f32)
    ang = np.concatenate([rows[:, None] * inv, cols[:, None] * inv], -1).astype(f32)
    rope_cs = np.concatenate([np.cos(ang), np.sin(ang)], -1).astype(f32)

    def ztab(Ls):
        tt = np.linspace(0.0, 1.0, Ls, dtype=f32)
        wpos = (f32(2.0 * math.pi) * np.arange(Ls, dtype=f32) / f32(Ls)).astype(f32)
        f = np.linspace(1e-4, 15, 16, dtype=f32)[None, :]
        z = np.concatenate([tt[:, None], np.cos(f * wpos[:, None]), -np.sin(f * wpos[:, None])], -1).astype(f32)
        pos = np.abs(np.arange(2 * Ls) - Ls)
        pos = np.minimum(pos, Ls - 1)
        return np.ascontiguousarray(z[pos].T), np.ascontiguousarray(tt[pos][None, :])
    hy_z, hy_t = ztab(L)
    hy_zc, hy_tc = ztab(LC)
    max_decay = math.log(1e-2) / 0.3
    min_decay = math.log(1e-2) / 1.5
    delta = np.abs(np.linspace(min_decay, max_decay, 256, dtype=f32)).astype(f32)[:, None]
    return dict(rope_cs=rope_cs, hy_z=hy_z, hy_t=hy_t, hy_zc=hy_zc, hy_tc=hy_tc, hy_delta=delta)


def make_in_maps(inputs, cores):
    f32 = np.float32
    shared = {}
    for k, v in inputs.items():
        if k in ("x", "c", "ctx"):
            continue
        v = np.ascontiguousarray(np.asarray(v, dtype=f32))
        if k in ("c_ctx", "router_bias"):
            v = v.reshape(1, -1)
        shared[k] = v
    shared.update(const_tables())
    maps = []
    for b in cores:
        m = dict(shared)
        m["x"] = np.ascontiguousarray(np.asarray(inputs["x"][b], dtype=f32))
        m["c"] = np.ascontiguousarray(np.asarray(inputs["c"][b], dtype=f32)).reshape(1, -1)
        m["ctx"] = np.ascontiguousarray(np.asarray(inputs["ctx"][b], dtype=f32))
        maps.append(m)
    return maps


def run_pipeline(gens, depth=2):
    it = iter(gens)
    active = []
    ready = True
    done = False
    while True:
        if ready and not done and len(active) < depth:
            try:
                active.append(next(it))
                ready = False
            except StopIteration:
                done = True
        if not active:
            if done:
                break
            ready = True
            continue
        for g_ in list(active):
            try:
                v = next(g_)
                if v is True and g_ is active[-1]:
                    ready = True
            except StopIteration:
                if g_ is active[-1]:
                    ready = True
                active.remove(g_)


def _seqs(ctx_out):
    s = [(LAT0, LC, L)]
    if ctx_out:
        s = [(CTX0, 0, LC)] + s
    return s


def load_hT(self, es):
    hT = self.sb(es, "hT_sb", [128, 8, NTP], BF16)
    b = Buf("hT_sb")
    for k in range(8):
        self.dma("sp", hT[:, k, :], self.hT3[:, k, :], [self.dbuf["hT"]], [b])
    return hT, b


def load_w_in(self, es, l, name, col0, ncols):
    w = self.sb(es, name, [128, 8, ncols], BF16)
    b = Buf(name)
    src = self.A["w_in"][l, :, col0:col0 + ncols].rearrange("(k p) n -> p k n", p=128)
    step = 1024
    for k in range(8):
        for c0 in range(0, ncols, step):
            c1 = min(ncols, c0 + step)
            self.dma("pool", w[:, k, c0:c1], src[:, k, c0:c1], [], [b])
    return w, b


K.load_hT = load_hT
K.load_w_in = load_w_in


def phase_sconv(self, l, ctx_out):
    A = self.A
    with ExitStack() as es:
        hT, bhT = self.load_hT(es)
        w, bw = self.load_w_in(es, l, "sc_w", OFF_SCONV, 768)
        taps = self.sb(es, "sc_taps", [128, 2, 3], F32); btaps = Buf()
        for cc in range(2):
            self.dma("sp", taps[:, cc, :], A["sconv_w"][l, :, cc * 128:(cc + 1) * 128].rearrange("j p -> p j"),
                     [], [btaps], allow_slow_non_contiguous=True)
        m = self.sb(es, "sc_m", [128, L + 2], F32); bm = Buf()
        bg = self.sb(es, "sc_bg", [128, L], F32); bbg = Buf()
        o = self.sb(es, "sc_o", [128, L], F32); bo = Buf()
        res = self.sb(es, "sc_res", [128, L], BF16); bres = Buf()
        cg = [self.sb(es, "sc_cg%d" % i, [128, 512], F32) for i in range(2)]
        bcg = [Buf(), Buf()]
        it = 0
        for cc in range(2):
            for (col0, tok0, n) in _seqs(ctx_out):
                self.memset("pool", m[:, 0:1], 0.0, [bm])
                self.memset("pool", m[:, n + 1:n + 2], 0.0, [bm])
                for t0 in range(0, n, 512):
                    nn = min(512, n - t0)
                    pss = []
                    for pi, cb in enumerate((256, 512, 0)):
                        ps, bp = self.psf[(it * 3 + pi) % 6]
                        for k in range(8):
                            self.mm(ps[:, 0:nn], w[:, k, cb + cc * 128:cb + (cc + 1) * 128],
                                    hT[:, k, col0 + t0:col0 + t0 + nn], k == 0, k == 7, [bw, bhT], [bp])
                        pss.append((ps, bp))
                    c_, bc_ = cg[it % 2], bcg[it % 2]
                    self.copy("act", c_[:, 0:nn], pss[0][0][:, 0:nn], [pss[0][1]], [bc_])
                    self.tt("dve", m[:, 1 + t0:1 + t0 + nn], pss[1][0][:, 0:nn], c_[:, 0:nn], ALU.mult,
                            [pss[1][1], bc_], [bm])
                    self.copy("act", bg[:, t0:t0 + nn], pss[2][0][:, 0:nn], [pss[2][1]], [bbg])
                    it += 1
                self.ts("dve", o[:, 0:n], m[:, 0:n], taps[:, cc, 0:1], None, ALU.mult, None, [bm, btaps], [bo])
                self.stt(o[:, 0:n], m[:, 1:n + 1], taps[:, cc, 1:2], o[:, 0:n], ALU.mult, ALU.add, [bm, btaps, bo], [bo])
                self.stt(o[:, 0:n], m[:, 2:n + 2], taps[:, cc, 2:3], o[:, 0:n], ALU.mult, ALU.add, [bm, btaps, bo], [bo])
                self.tt("dve", res[:, 0:n], o[:, 0:n], bg[:, 0:n], ALU.mult, [bo, bbg], [bres])
                self.dma("sp", self.ysT3[:, 4 + cc, tok0:tok0 + n], res[:, 0:n], [bres], [self.dbuf["ysT"]])
        self.barrier()


K.phase_sconv = phase_sconv


def merge_load(self, es, l):
    A = self.A
    wg, bwg = self.load_w_in(es, l, "mg_wg", OFF_GATE, 4096)
    wb = self.sb(es, "mg_wb", [128, 8, D], BF16); bwb = Buf()
    wo = self.sb(es, "mg_wo", [128, 8, D], BF16); bwo = Buf()
    for n4 in range(4):
        for j in range(2):
            self.dma("pool", wb[:, 2 * n4 + j, :], A["w_branch"][l, n4, j * 128:(j + 1) * 128, :], [], [bwb])
    for k in range(8):
        self.dma("pool", wo[:, k, :], A["w_out"][l, k * 128:(k + 1) * 128, :], [], [bwo])
    return wg, bwg, wb, bwb, wo, bwo


K.merge_load = merge_load


def phase_merge(self, l, tiles, pre=None):
    A = self.A
    with ExitStack() as es:
        if pre is None:
            pre = self.merge_load(es, l)
        wg, bwg, wb, bwb, wo, bwo = pre
        NB = 2
        ys = [self.sb(es, "mg_ys%d" % i, [128, 8, 128], BF16) for i in range(NB)]; bys = [Buf() for _ in range(NB)]
        ht = [self.sb(es, "mg_ht%d" % i, [128, 8, 128], BF16) for i in range(NB)]; bht = [Buf() for _ in range(NB)]
        xt = [self.sb(es, "mg_x%d" % i, [128, D], F32) for i in range(NB)]; bxt = [Buf() for _ in range(NB)]
        gs = [self.sb(es, "mg_gs%d" % i, [128, D], F32) for i in range(2)]; bgs = [Buf(), Buf()]
        mg = self.sb(es, "mg_m", [128, D], F32); bmg = Buf()
        mgb = self.sb(es, "mg_mb", [128, D], BF16); bmgb = Buf()
        mT = self.sb(es, "mg_mT", [128, 8, 128], BF16); bmT = Buf()
        t1 = self.sb(es, "mg_t1", [128, D], F32); bt1 = Buf()
        x1 = self.sb(es, "mg_x1", [128, D], F32); bx1 = Buf()
        xn = self.sb(es, "mg_xn", [128, D], F32); bxn = Buf()
        hf32 = self.sb(es, "mg_hf32", [128, 8, 128], F32); bhf32 = Buf()
        hfb = self.sb(es, "mg_hfb", [128, 8, 128], BF16); bhfb = Buf()
        st = self.sb(es, "mg_st", [128, 2, 6], F32); mv = self.sb(es, "mg_mv", [128, 2], F32)
        rs = self.sb(es, "mg_rs", [128, 1], F32); bs = Buf()
        rt = self.sb(es, "mg_rt", [128, 8, NE], F32); brt = Buf()
        bxres = self.dbuf["xres"]
        def tile_gen(n, ti):
            s = n % NB
            r_ = 1 if ti < 2 else 0
            col = tok_col(ti * 128)
            tk = ti * 128
            self.dma("sp", ys[s][:], self.ysT3[:, :, tk:tk + 128], [self.dbuf["ysT"]], [bys[s]])
            self.dma("sp", ht[s][:], self.hT3[:, :, col:col + 128], [self.dbuf["hT"]], [bht[s]])
            self.dma("sp", xt[s][:], A["xres"][tk:tk + 128, :], [bxres], [bxt[s]])
            yield
            for n4 in range(4):
                g_, bg_ = gs[n4 % 2], bgs[n4 % 2]
                for hh in range(2):
                    pg, bpg = self.psf[hh]
                    for k in range(8):
                        self.mm(pg[:, :], ht[s][:, k, :], wg[:, k, n4 * 1024 + hh * 512:n4 * 1024 + (hh + 1) * 512],
                                k == 0, k == 7, [bht[s], bwg], [bpg])
                    self.act(g_[:, hh * 512:(hh + 1) * 512], pg[:, :], AF.Sigmoid, [bpg], [bg_])
                for hh in range(2):
                    pz, bpz = self.psf[2 + hh]
                    for j in range(2):
                        self.mm(pz[:, :], ys[s][:, 2 * n4 + j, :], wb[:, 2 * n4 + j, hh * 512:(hh + 1) * 512],
                                j == 0, j == 1, [bys[s], bwb], [bpz])
                    sl = slice(hh * 512, (hh + 1) * 512)
                    if n4 == 0:
                        self.tt("dve", mg[:, sl], pz[:, :], g_[:, sl], ALU.mult, [bpz, bg_], [bmg])
                    else:
                        self.tt("dve", g_[:, sl], pz[:, :], g_[:, sl], ALU.mult, [bpz, bg_], [bg_])
                        if n4 < 3:
                            self.tt("pool", mg[:, sl], mg[:, sl], g_[:, sl], ALU.add, [bmg, bg_], [bmg])
                        else:
                            self.tt("pool", mgb[:, sl], mg[:, sl], g_[:, sl], ALU.add, [bmg, bg_], [bmgb])
                yield
            pt, bpt = self.psb[0]
            for k in range(8):
                self.tr(pt[:, k * 128:(k + 1) * 128], mgb[:, k * 128:(k + 1) * 128], self.ident_b[:],
                        [bmgb, self.b_ident_b], [bpt], inc=(k == 7))
            yield True
            self.copy("dve", mT[:].rearrange("p a b -> p (a b)"), pt[:, :], [bpt], [bmT])
            yield
            for hh in range(2):
                py, bpy = self.psf[4 + hh]
                sl = slice(hh * 512, (hh + 1) * 512)
                for k in range(8):
                    self.mm(py[:, :], mT[:, k, :], wo[:, k, sl], k == 0, k == 7, [bmT, bwo], [bpy])
                self.tt("dve", t1[:, sl], py[:, :], self.gbc[:, 0 + r_, sl], ALU.mult, [bpy, self.b_gbc], [bt1])
            yield
            self.stt(t1[:], xt[s][:], ALPHA, t1[:], ALU.mult, ALU.add, [bxt[s], bt1], [bt1])
            self.ln_stats(t1, bt1, st, mv, rs, bs)
            yield
            self.ts("dve", xn[:], t1[:], mv[:, 0:1], rs[:, 0:1], ALU.subtract, ALU.mult, [bt1, bs], [bxn])
            self.tt("pool", xn[:], xn[:], self.lnbc[:, 0, :], ALU.mult, [bxn, self.b_lnbc], [bxn])
            self.tt("pool", x1[:], xn[:], self.lnbc[:, 1, :], ALU.add, [bxn, self.b_lnbc], [bx1])
            self.dma("sp", A["xres"][tk:tk + 128, :], x1[:], [bx1], [bxres])
            yield
            self.ln_stats(x1, bx1, st, mv, rs, bs)
            self.ts("dve", xn[:], x1[:], mv[:, 0:1], rs[:, 0:1], ALU.subtract, ALU.mult, [bx1, bs], [bxn])
            yield
            for half in range(2):
                pf, bpf = self.psf[4 + half]
                for kk in range(4):
                    k = half * 4 + kk
                    self.tr(pf[:, kk * 128:(kk + 1) * 128], xn[:, k * 128:(k + 1) * 128], self.ident_f[:],
                            [bxn, self.b_ident_f], [bpf], inc=(kk == 3))
                for kk in range(4):
                    k = half * 4 + kk
                    self.act(hf32[:, k, :], pf[:, kk * 128:(kk + 1) * 128], AF.Identity, [bpf, self.b_modT], [bhf32],
                             bias=self.modT[:, 24 + k, r_:r_ + 1], scale=self.modT[:, 32 + k, r_:r_ + 1])
            yield
            self.copy("dve", hfb[:], hf32[:], [bhf32], [bhfb])
            self.dma("sp", self.hfT3[:, :, tk:tk + 128], hfb[:], [bhfb], [self.dbuf["hfT"]])
            pr, bpr = self.psb[1][0][:].bitcast(F32), self.psb[1][1]
            for k in range(8):
                self.mm(pr[:, 0:NE], hf32[:, k, :], self.rw32[:, k, :], k == 0, k == 7, [bhf32, self.b_rw32], [bpr])
            s_ = rt[:, 0, :]; sel = rt[:, 1, :]; tmp = rt[:, 2, :]; sel2_ = rt[:, 3, :]; msk = rt[:, 4, :]
            m1 = rt[:, 5, 0:4]; m2 = rt[:, 5, 4:8]; grp = rt[:, 5, 8:12]; gmx = rt[:, 5, 12:13]; oh = rt[:, 6, 0:4]
            den = rt[:, 6, 4:5]
            B = [brt]
            v3 = lambda a: a.rearrange("p (g e) -> p g e", g=4)
            b4 = lambda a: a.unsqueeze(2).to_broadcast([128, 4, 4])
            yield
            self.act(s_, pr[:, 0:NE], AF.Sigmoid, [bpr], B)
            self.tt("dve", sel, s_, self.rbias[:], ALU.add, B + [self.b_rbias], B)
            self.op("dve", lambda e: e.tensor_reduce(out=m1, in_=v3(sel), axis=AX.X, op=ALU.max), B, B)
            self.tt("dve", v3(tmp), v3(sel), b4(m1), ALU.is_equal, B, B)
            self.stt(sel2_, tmp, -1e30, sel, ALU.mult, ALU.add, B, B)
            self.op("dve", lambda e: e.tensor_reduce(out=m2, in_=v3(sel2_), axis=AX.X, op=ALU.max), B, B)
            self.tt("dve", grp, m1, m2, ALU.add, B, B)
            self.op("dve", lambda e: e.tensor_reduce(out=gmx, in_=grp, axis=AX.X, op=ALU.max), B, B)
            self.ts("dve", oh, grp, gmx, None, ALU.is_equal, None, B, B)
            self.tt("dve", v3(msk), v3(sel), b4(m2), ALU.is_ge, B, B)
            self.tt("dve", v3(msk), v3(msk), b4(oh), ALU.mult, B, B)
            self.tt("dve", tmp, msk, s_, ALU.mult, B, B)
            self.op("dve", lambda e: e.tensor_reduce(out=den, in_=tmp, axis=AX.X, op=ALU.add), B, B)
            self.op("dve", lambda e: e.reciprocal(out=den, in_=den), B, B)
            self.ts("dve", self.gates[:, ti, :], tmp, den, None, ALU.mult, None, B, [self.b_gates])
            yield

        run_pipeline((tile_gen(n, ti) for n, ti in enumerate(tiles)), depth=2)
        self.barrier()


K.phase_merge = phase_merge


def phase_moe(self, l, tiles, last):
    A = self.A
    nparts = 3
    per = (len(tiles) + nparts - 1) // nparts
    parts = [tiles[i:i + per] for i in range(0, len(tiles), per)]
    bxres = self.dbuf["xres"]
    with ExitStack() as es:
        hf = self.sb(es, "moe_hf", [128, 8, per * 128], BF16); bhf = Buf()
        acc = self.sb(es, "moe_acc", [128, per, D], F32); bacc = Buf()
        w1s = [self.sb(es, "moe_w1_%d" % i, [128, 8, DE], BF16) for i in range(2)]
        w3s = [self.sb(es, "moe_w3_%d" % i, [128, 8, DE], BF16) for i in range(2)]
        w2s = [self.sb(es, "moe_w2_%d" % i, [128, 4, D], BF16) for i in range(2)]
        bw = [Buf(), Buf()]
        sa = [self.sb(es, "moe_sa%d" % i, [128, 512], F32) for i in range(2)]; bsa = [Buf(), Buf()]
        aT = [self.sb(es, "moe_aT%d" % i, [128, 4, 512], BF16) for i in range(2)]; baT = [Buf(), Buf()]
        xt = [self.sb(es, "moe_x%d" % i, [128, D], F32) for i in range(2)]; bxt = [Buf(), Buf()]
        t1 = self.sb(es, "moe_t1", [128, D], F32); bt1 = Buf()
        xo = [self.sb(es, "moe_xo%d" % i, [128, D], F32) for i in range(2)]; bxo = [Buf(), Buf()]
        st = self.sb(es, "moe_st", [128, 2, 6], F32); mv = self.sb(es, "moe_mv", [128, 2], F32)
        rs = self.sb(es, "moe_rs", [128, 1], F32); bs = Buf()
        wi = 0
        ci = 0
        oi = 0
        pobanks = [self.psf[4], self.psf[5], (self.psb[0][0][:].bitcast(F32), self.psb[0][1]),
                   (self.psb[1][0][:].bitcast(F32), self.psb[1][1])]
        for part in parts:
            nt = len(part)
            tok0 = part[0] * 128
            ntok = nt * 128
            for k in range(8):
                self.dma("sp", hf[:, k, 0:ntok], self.hfT3[:, k, tok0:tok0 + ntok], [self.dbuf["hfT"]], [bhf])
            for e_ in range(NE):
                sl_ = wi % 2
                wi += 1
                self.dma("sp", w1s[sl_][:], A["ewb1"][e_ * D:(e_ + 1) * D, :].rearrange("(k p) n -> p k n", p=128),
                         [self.dbuf["ewb1"]], [bw[sl_]])
                self.dma("sp", w3s[sl_][:], A["ewb3"][e_ * D:(e_ + 1) * D, :].rearrange("(k p) n -> p k n", p=128),
                         [self.dbuf["ewb3"]], [bw[sl_]])
                self.dma("sp", w2s[sl_][:], A["ewb2"][e_ * DE:(e_ + 1) * DE, :].rearrange("(k p) n -> p k n", p=128),
                         [self.dbuf["ewb2"]], [bw[sl_]])
                for t0 in range(0, ntok, 512):
                    nn = min(512, ntok - t0)
                    a_, ba_ = aT[ci % 2], baT[ci % 2]
                    ci += 1
                    for f in range(4):
                        pa, bpa = self.psf[(2 * f) % 4]
                        pb, bpb = self.psf[(2 * f + 1) % 4]
                        for k in range(8):
                            self.mm(pa[:, 0:nn], w1s[sl_][:, k, f * 128:(f + 1) * 128], hf[:, k, t0:t0 + nn],
                                    k == 0, k == 7, [bw[sl_], bhf], [bpa])
                        for k in range(8):
                            self.mm(pb[:, 0:nn], w3s[sl_][:, k, f * 128:(f + 1) * 128], hf[:, k, t0:t0 + nn],
                                    k == 0, k == 7, [bw[sl_], bhf], [bpb])
                        s_, bs_ = sa[f % 2], bsa[f % 2]
                        self.act(s_[:, 0:nn], pa[:, 0:nn], AF.Silu, [bpa], [bs_])
                        self.tt("dve", a_[:, f, 0:nn], pb[:, 0:nn], s_[:, 0:nn], ALU.mult, [bpb, bs_], [ba_])
                    for tt_ in range(nn // 128):
                        j = (t0 + tt_ * 128) // 128
                        ti = part[j]
                        for hh in range(2):
                            oi += 1
                            po, bpo = pobanks[oi % 4]
                            sl = slice(hh * 512, (hh + 1) * 512)
                            for f in range(4):
                                self.mm(po[:, :], a_[:, f, tt_ * 128:(tt_ + 1) * 128], w2s[sl_][:, f, sl],
                                        f == 0, f == 3, [ba_, bw[sl_]], [bpo])
                            gcol = self.gates[:, ti, e_:e_ + 1]
                            if e_ == 0:
                                self.ts("dve", acc[:, j, sl], po[:, :], gcol, None, ALU.mult, None,
                                        [bpo, self.b_gates], [bacc])
                            else:
                                self.stt(acc[:, j, sl], po[:, :], gcol, acc[:, j, sl], ALU.mult, ALU.add,
                                         [bpo, self.b_gates, bacc], [bacc])
            for j, ti in enumerate(part):
                s = j % 2
                r_ = 1 if ti < 2 else 0
                tk = ti * 128
                self.dma("sp", xt[s][:], A["xres"][tk:tk + 128, :], [bxres], [bxt[s]])
                self.tt("pool", t1[:], acc[:, j, :], self.gbc[:, 2 + r_, :], ALU.mult, [bacc, self.b_gbc], [bt1])
                self.stt(t1[:], xt[s][:], ALPHA, t1[:], ALU.mult, ALU.add, [bxt[s], bt1], [bt1])
                self.ln_stats(t1, bt1, st, mv, rs, bs)
                self.ts("dve", t1[:], t1[:], mv[:, 0:1], rs[:, 0:1], ALU.subtract, ALU.mult, [bt1, bs], [bt1])
                self.tt("pool", t1[:], t1[:], self.lnbc[:, 2, :], ALU.mult, [bt1, self.b_lnbc], [bt1])
                self.tt("pool", xo[s][:], t1[:], self.lnbc[:, 3, :], ALU.add, [bt1, self.b_lnbc], [bxo[s]])
                if last:
                    self.dma("sp", A["out"][tk - LC:tk - LC + 128, :], xo[s][:], [bxo[s]], [self.dbuf["out"]])
                else:
                    self.dma("sp", A["xres"][tk:tk + 128, :], xo[s][:], [bxo[s]], [bxres])
        self.barrier()


K.phase_moe = phase_moe


def precast_experts(self, l):
    A = self.A
    for e_ in range(NE):
        self.dma("pool", A["ewb1"][e_ * D:(e_ + 1) * D, :], A["exp_w1"][l, e_, :, :], [], [self.dbuf["ewb1"]])
        self.dma("pool", A["ewb3"][e_ * D:(e_ + 1) * D, :], A["exp_w3"][l, e_, :, :], [], [self.dbuf["ewb3"]])
        self.dma("pool", A["ewb2"][e_ * DE:(e_ + 1) * DE, :], A["exp_w2"][l, e_, :, :], [], [self.dbuf["ewb2"]])


K.precast_experts = precast_experts


def phase_attn(self, l, ctx_out):
    A = self.A
    with ExitStack() as es:
        w, bw = self.load_w_in(es, l, "at_w", OFF_ATTN, 512)
        hts = [self.sb(es, "at_ht%d" % i, [128, 8, 128], BF16) for i in range(3)]
        bhts = [Buf() for _ in range(3)]
        qT = self.sb(es, "at_qT", [64, 4, NTOK], BF16); bqT = Buf()
        kT = self.sb(es, "at_kT", [64, 2, NTOK], BF16); bkT = Buf()
        va = self.sb(es, "at_va", [128, NTILE, 2, 66], BF16); bva = Buf()
        gqk = self.sb(es, "at_g", [128, 6, 64], F32); bg = Buf()
        cs = self.sb(es, "at_cs", [128, 32, 64], F32); bcs = Buf()
        for h in range(6):
            src = A["attn_q_norm"] if h < 4 else A["attn_k_norm"]
            self.dma("sp", gqk[:, h, :], src[l:l + 1, :].partition_broadcast(128), [], [bg])
        self.dma("sp", cs[:], A["rope_cs"].rearrange("(j p) c -> p j c", p=128), [], [bcs])
        self.memset("pool", va[:, :, :, 64:66], 1.0, [bva])
        NB = 2
        sq = [self.sb(es, "at_sq%d" % i, [128, 6, 64], F32) for i in range(NB)]
        qn = [self.sb(es, "at_qn%d" % i, [128, 6, 64], F32) for i in range(NB)]
        t_a = [self.sb(es, "at_ta%d" % i, [128, 6, 32], F32) for i in range(NB)]
        t_b = [self.sb(es, "at_tb%d" % i, [128, 6, 32], F32) for i in range(NB)]
        qr = [self.sb(es, "at_qr%d" % i, [128, 6, 64], BF16) for i in range(NB)]
        ss = [self.sb(es, "at_ss%d" % i, [128, 8], F32) for i in range(NB)]
        bt = [Buf() for _ in range(NB)]
        bqr = [Buf() for _ in range(NB)]
        tiles = list(range(NTILE))

        def tile_gen(n, ti):
            s = n % NB
            col = tok_col(ti * 128)
            tk = ti * 128
            ps, bp = self.psf[n % 2]
            ht_, bht_ = hts[n % 3], bhts[n % 3]
            self.dma("sp", ht_[:], self.hT3[:, :, col:col + 128], [self.dbuf["hT"]], [bht_])
            for k in range(8):
                self.mm(ps[:, :], ht_[:, k, :], w[:, k, :], k == 0, k == 7, [bht_, bw], [bp])
            yield
            B = [bt[s]]
            p3 = ps[:, 0:384].rearrange("p (h d) -> p h d", h=6)
            self.copy("dve", va[:, ti, :, 0:64], ps[:, 384:512].rearrange("p (g d) -> p g d", g=2), [bp], [bva])
            self.copy("act", qn[s][:].rearrange("p h d -> p (h d)"), ps[:, 0:384], [bp], B)
            self.tt("dve", sq[s][:], qn[s][:], qn[s][:], ALU.mult, B, B)
            yield
            self.op("dve", lambda e, s=s: e.tensor_reduce(out=ss[s][:, 0:6], in_=sq[s][:], axis=AX.X, op=ALU.add), B, B)
            self.act(ss[s][:, 0:6], ss[s][:, 0:6], AF.Sqrt, B, B, bias=1e-6, scale=1.0 / 64.0)
            self.op("dve", lambda e, s=s: e.reciprocal(out=ss[s][:, 0:6], in_=ss[s][:, 0:6]), B, B)
            self.tt("dve", qn[s][:], qn[s][:], ss[s][:, 0:6].unsqueeze(2).to_broadcast([128, 6, 64]), ALU.mult, B, B)
            self.tt("pool", qn[s][:], qn[s][:], gqk[:], ALU.mult, B + [bg], B)
            yield True
            if ti >= 2:
                j = ti - 2
                cosb = cs[:, j, 0:32].unsqueeze(1).to_broadcast([128, 6, 32])
                sinb = cs[:, j, 32:64].unsqueeze(1).to_broadcast([128, 6, 32])
                x1 = qn[s][:, :, 0:64:2]
                x2 = qn[s][:, :, 1:64:2]
                self.tt("dve", t_a[s][:], x1, cosb, ALU.mult, B + [bcs], B)
                self.tt("dve", t_b[s][:], x2, sinb, ALU.mult, B + [bcs], B)
                self.tt("dve", qr[s][:, :, 0:64:2], t_a[s][:], t_b[s][:], ALU.subtract, B + [bqr[s]], [bqr[s]])
                self.tt("dve", t_a[s][:], x1, sinb, ALU.mult, B + [bcs, bqr[s]], B)
                self.tt("dve", t_b[s][:], x2, cosb, ALU.mult, B + [bcs, bqr[s]], B)
                self.tt("dve", qr[s][:, :, 1:64:2], t_a[s][:], t_b[s][:], ALU.add, B + [bqr[s]], [bqr[s]])
            else:
                self.copy("dve", qr[s][:], qn[s][:], B, [bqr[s]])
            yield
            pt, bpt = self.psb[n % 2]
            for h in range(6):
                self.tr(pt[0:64, h * 128:(h + 1) * 128], qr[s][:, h, :], self.ident_b[:], [bqr[s], self.b_ident_b],
                        [bpt], inc=(h == 5))
            yield
            self.copy("act", qT[:, :, tk:tk + 128], pt[0:64, 0:512].rearrange("p (h t) -> p h t", h=4), [bpt], [bqT])
            self.copy("dve", kT[:, :, tk:tk + 128], pt[0:64, 512:768].rearrange("p (h t) -> p h t", h=2), [bpt], [bkT])
            yield

        run_pipeline((tile_gen(n, ti) for n, ti in enumerate(tiles)), depth=2)
        self.precast_experts(l)
        pT = [self.sb(es, "at_pT%d" % i, [128, 512], BF16) for i in range(3)]; bpT = [Buf() for _ in range(3)]
        osb = [self.sb(es, "at_o%d" % i, [65, 512], F32) for i in range(2)]; bosb = [Buf(), Buf()]
        yo = [self.sb(es, "at_y%d" % i, [64, 512], BF16) for i in range(2)]; byo = [Buf(), Buf()]
        qsets = [(LC + c * 512, 512, list(range(NTILE))) for c in range(8)]
        if ctx_out:
            qsets = [(0, LC, [0, 1])] + qsets
        it = 0
        ei = 0
        for (q0, nq, kts) in qsets:
            for h in range(4):
                g = h // 2
                po, bpo = self.psf[4 + it % 2]
                o_, bo_ = osb[it % 2], bosb[it % 2]
                y_, by_ = yo[it % 2], byo[it % 2]
                it += 1

                def S(i):
                    kt = kts[i]
                    ps_, bp_ = self.psf[i % 3]
                    self.mm(ps_[:, 0:nq], kT[0:64, g, kt * 128:(kt + 1) * 128], qT[0:64, h, q0:q0 + nq], True, True,
                            [bkT, bqT], [bp_])
                S(0)
                for i, kt in enumerate(kts):
                    if i + 1 < len(kts):
                        S(i + 1)
                    ps_, bp_ = self.psf[i % 3]
                    p_, bp2 = pT[ei % 3], bpT[ei % 3]
                    ei += 1
                    self.act(p_[:, 0:nq], ps_[:, 0:nq], AF.Exp, [bp_], [bp2], scale=0.125)
                    self.mm(po[0:65, 0:nq], va[:, kt, g, 0:65], p_[:, 0:nq], i == 0, i == len(kts) - 1, [bva, bp2], [bpo])
                self.copy("dve", o_[0:65, 0:nq], po[0:65, 0:nq], [bpo], [bo_])
                self.op("dve", lambda e, o_=o_, nq=nq: e.reciprocal(out=o_[64:65, 0:nq], in_=o_[64:65, 0:nq]), [bo_], [bo_])
                pb_, bpb_ = self.psf[3]
                self.mm(pb_[0:64, 0:nq], self.ones_f[64:65, 0:64], o_[64:65, 0:nq], True, True, [self.b_ones_f, bo_], [bpb_])
                self.tt("dve", y_[:, 0:nq], o_[0:64, 0:nq], pb_[0:64, 0:nq], ALU.mult, [bo_, bpb_], [by_])
                r0 = (h % 2) * 64
                self.dma("sp", self.ysT3[r0:r0 + 64, 6 + h // 2, q0:q0 + nq], y_[:, 0:nq], [by_], [self.dbuf["ysT"]])
        self.barrier()


K.phase_attn = phase_attn


def phase_hyfilt(self, l, Ls, zname, tname, Gname):
    A = self.A
    CS = min(512, Ls)
    with ExitStack() as es:
        w1 = self.sb(es, "hf_w1", [33, 64], F32); w2 = self.sb(es, "hf_w2", [64, 64], F32)
        w3 = self.sb(es, "hf_w3", [64, 512], F32); bw = Buf()
        cols = self.sb(es, "hf_cols", [64, 4], F32)
        dl = self.sb(es, "hf_dl", [128, 2], F32)
        self.dma("sp", w1[:], A["hyena_w1"][l, :, :], [], [bw])
        self.dma("sp", w2[:], A["hyena_w2"][l, :, :], [], [bw])
        self.dma("sp", w3[:], A["hyena_w3"][l, :, :], [], [bw])
        for i, nme in enumerate(("hyena_b1", "hyena_freq1", "hyena_b2", "hyena_freq2")):
            self.dma("sp", cols[:, i:i + 1], A[nme][l:l + 1, :].rearrange("o n -> n o"), [], [bw],
                     allow_slow_non_contiguous=True)
        self.dma("sp", dl[:], A["hy_delta"].rearrange("(c p) o -> p (c o)", p=128), [], [bw],
                 allow_slow_non_contiguous=True)
        self.ts("dve", dl[:], dl[:], -1.0, None, ALU.mult, None, [bw], [bw])
        z = [self.sb(es, "hf_z%d" % i, [33, CS], F32) for i in range(2)]; bz = [Buf(), Buf()]
        tb = [self.sb(es, "hf_t%d" % i, [128, CS], F32) for i in range(2)]; btb = [Buf(), Buf()]
        a1s = [self.sb(es, "hf_a1_%d" % i, [64, CS], F32) for i in range(2)]; ba1s = [Buf(), Buf()]
        a2s = [self.sb(es, "hf_a2_%d" % i, [64, CS], F32) for i in range(2)]; ba2s = [Buf(), Buf()]
        tmps = [self.sb(es, "hf_tmp%d" % i, [64, CS], F32) for i in range(2)]
        win = [self.sb(es, "hf_win%d" % i, [128, CS], F32) for i in range(2)]; bwin = [Buf(), Buf()]
        g = [self.sb(es, "hf_g%d" % i, [128, CS], BF16) for i in range(2)]; bg = [Buf(), Buf()]
        Gap = A[Gname]
        gi_ = [0]

        def chunk_gen(ci):
            n0 = ci * CS
            s = ci % 2
            a1, ba1, a2, ba2, tmp = a1s[s], ba1s[s], a2s[s], ba2s[s], tmps[s]
            self.dma("sp", z[s][:], A[zname][:, n0:n0 + CS], [], [bz[s]])
            self.dma("sp", tb[s][:], A[tname][0:1, n0:n0 + CS].partition_broadcast(128), [], [btb[s]])
            yield
            p1, bp1 = self.psf[0 + 3 * s]
            self.mm(p1[0:64, 0:CS], w1[:, :], z[s][:, :], True, True, [bw, bz[s]], [bp1])
            self.ts("dve", a1[:], p1[0:64, 0:CS], cols[:, 0:1], cols[:, 1:2], ALU.add, ALU.mult, [bp1, bw], [ba1])
            yield
            self.range_reduce(a1[:], tmp[:], [ba1], [ba1])
            yield
            self.act(a1[:], a1[:], AF.Sin, [ba1], [ba1])
            yield True
            p2, bp2 = self.psf[1 + 3 * s]
            self.mm(p2[0:64, 0:CS], w2[:, :], a1[:, :], True, True, [bw, ba1], [bp2])
            self.ts("dve", a2[:], p2[0:64, 0:CS], cols[:, 2:3], cols[:, 3:4], ALU.add, ALU.mult, [bp2, bw], [ba2])
            yield
            self.range_reduce(a2[:], tmp[:], [ba2, ba1], [ba2, ba1])
            yield
            self.act(a2[:], a2[:], AF.Sin, [ba2], [ba2])
            yield
            for cc in range(2):
                cb = (256 if n0 < Ls else 0) + cc * 128
                p3, bp3 = self.psf[2 + 3 * s] if cc == 0 else self.psb_f32[s]
                self.mm(p3[:, 0:CS], w3[:, cb:cb + 128], a2[:, :], True, True, [bw, ba2], [bp3])
                w_, bw_ = win[gi_[0] % 2], bwin[gi_[0] % 2]
                g_, bg_ = g[gi_[0] % 2], bg[gi_[0] % 2]
                gi_[0] += 1
                self.act(w_[:], tb[s][:], AF.Exp, [btb[s], bw], [bw_], scale=dl[:, cc:cc + 1])
                self.tt("dve", g_[:], p3[:, 0:CS], w_[:], ALU.mult, [bp3, bw_], [bg_])
                self.dma("sp", Gap[cc * 128:(cc + 1) * 128, n0:n0 + CS], g_[:], [bg_], [self.dbuf[Gname]])
                yield

        run_pipeline((chunk_gen(ci) for ci in range(2 * Ls // CS)), depth=2)
        self.barrier()


K.phase_hyfilt = phase_hyfilt


def phase_hyena(self, l, ctx_out):
    A = self.A
    seqs = [(LAT0, LC, L, "hyG")]
    if ctx_out:
        seqs = [(CTX0, 0, LC, "hyGc")] + seqs
    self.phase_hyfilt(l, L, "hy_z", "hy_t", "hyG")
    if ctx_out:
        self.phase_hyfilt(l, LC, "hy_zc", "hy_tc", "hyGc")
    with ExitStack() as es:
        w, bw = self.load_w_in(es, l, "hy_w", OFF_HYENA, 768)
        taps = self.sb(es, "hy_taps", [128, 6, 3], F32); btaps = Buf()
        for c6 in range(6):
            self.dma("sp", taps[:, c6, :], A["hyena_conv"][l, :, c6 * 128:(c6 + 1) * 128].rearrange("j p -> p j"),
                     [], [btaps], allow_slow_non_contiguous=True)
        skip = self.sb(es, "hy_skip", [128, 2], F32)
        self.dma("sp", skip[:], A["hyena_skip"][l:l + 1, :].rearrange("o (c p) -> p (o c)", p=128), [], [btaps],
                 allow_slow_non_contiguous=True)
        hch = [self.sb(es, "hy_h%d" % i, [128, 8, 512], BF16) for i in range(2)]; bhch = [Buf(), Buf()]
        praw = self.sb(es, "hy_praw", [128, L + 2], F32); bpraw = Buf()
        tmp = self.sb(es, "hy_tmp", [128, L], F32); btmp = Buf()
        u = self.sb(es, "hy_u", [128, L], F32); bu = Buf()
        x0c = self.sb(es, "hy_x0", [128, L], F32); bx0 = Buf()
        ub = self.sb(es, "hy_ub", [128, L], BF16); bub = Buf()
        utok = self.sb(es, "hy_utok", [128, 128, L // 128], BF16); butok = Buf()
        ut = self.sb(es, "hy_ut", [128, 512], BF16); but = Buf()
        S = [self.sb(es, "hy_S%d" % i, [128, 63 * 128], BF16) for i in range(2)]; bS = [Buf(), Buf()]
        hi = 0
        si = 0
        for (col0, tok0, n, Gname) in seqs:
            nb = n // 128
            W = (2 * nb - 1) * 128
            Gt = self.dram[Gname]
            ytok = praw[:, 0:n].rearrange("p (a c) -> p a c", c=128)
            for cc in range(2):
                for part, cb in (("x1", 256), ("v", 512), ("x0", 0)):
                    c6 = cb // 128 + cc
                    self.memset("pool", praw[:, 0:1], 0.0, [bpraw])
                    self.memset("pool", praw[:, n + 1:n + 2], 0.0, [bpraw])
                    for t0 in range(0, n, 512):
                        nn = min(512, n - t0)
                        h_, bh_ = hch[hi % 2], bhch[hi % 2]
                        hi += 1
                        self.dma("sp", h_[:, :, 0:nn], self.hT3[:, :, col0 + t0:col0 + t0 + nn], [self.dbuf["hT"]], [bh_])
                        ps, bp = self.psf[2 + hi % 2]
                        for k in range(8):
                            self.mm(ps[:, 0:nn], w[:, k, cb + cc * 128:cb + (cc + 1) * 128], h_[:, k, 0:nn],
                                    k == 0, k == 7, [bw, bh_], [bp])
                        self.copy("act", praw[:, 1 + t0:1 + t0 + nn], ps[:, 0:nn], [bp], [bpraw])
                    dst, bdst = {"x1": (u, bu), "v": (tmp, btmp), "x0": (x0c, bx0)}[part]
                    self.ts("dve", dst[:, 0:n], praw[:, 0:n], taps[:, c6, 0:1], None, ALU.mult, None, [bpraw, btaps], [bdst])
                    self.stt(dst[:, 0:n], praw[:, 1:n + 1], taps[:, c6, 1:2], dst[:, 0:n], ALU.mult, ALU.add,
                             [bpraw, btaps, bdst], [bdst])
                    self.stt(dst[:, 0:n], praw[:, 2:n + 2], taps[:, c6, 2:3], dst[:, 0:n], ALU.mult, ALU.add,
                             [bpraw, btaps, bdst], [bdst])
                    if part == "v":
                        self.tt("dve", u[:, 0:n], u[:, 0:n], tmp[:, 0:n], ALU.mult, [bu, btmp], [bu])
                        self.copy("act", ub[:, 0:n], u[:, 0:n], [bu], [bub])
                for b0 in range(0, nb, 4):
                    nbb = min(4, nb - b0)
                    pt, bpt = self.psb[(b0 // 4) % 2]
                    for bb in range(nbb):
                        b_ = b0 + bb
                        self.tr(pt[:, bb * 128:(bb + 1) * 128], ub[:, b_ * 128:(b_ + 1) * 128], self.ident_b[:],
                                [bub, self.b_ident_b], [bpt], inc=(bb == nbb - 1))
                    self.copy("dve", ut[:, 0:nbb * 128], pt[:, 0:nbb * 128], [bpt], [but])
                    pf, bpf = self.psf[4 + (b0 // 4) % 2]
                    self.mm(pf[:, 0:nbb * 128], self.flip_b[:], ut[:, 0:nbb * 128], True, True, [self.b_flip_b, but], [bpf])
                    self.copy("act", utok[:, :, b0:b0 + nbb].rearrange("p c b -> p b c"),
                              pf[:, 0:nbb * 128].rearrange("p (b c) -> p b c", c=128), [bpf], [butok])
                for c0 in range(0, 128, 16):
                    py, bpy = self.psf[(c0 // 16) % 2]
                    for cl in range(16):
                        c = c0 + cl
                        cg = cc * 128 + c
                        S_, bS_ = S[si % 2], bS[si % 2]
                        si += 1
                        src = bass.AP(Gt, cg * 2 * n + 1, [[1, 128], [1, W]])
                        self.dma("sp", S_[:, 0:W], src, [self.dbuf[Gname]], [bS_])
                        ds = [0] + [d for d in range(-(nb - 1), nb) if d != 0]
                        for ii, d in enumerate(ds):
                            a0 = max(0, d)
                            a1_ = min(nb - 1, nb - 1 + d)
                            off = (nb - 1 + d) * 128
                            self.mm(py[:, cl * nb + a0:cl * nb + a1_ + 1], S_[:, off:off + 128],
                                    utok[:, c, a0 - d:a1_ - d + 1], ii == 0, ii == len(ds) - 1, [bS_, butok], [bpy])
                    eng = "dve" if (c0 // 16) % 2 == 0 else "act"
                    self.copy(eng, ytok[:, :, c0:c0 + 16], py[:, 0:16 * nb].rearrange("p (c a) -> p a c", a=nb),
                              [bpy], [bpraw])
                for a0 in range(0, nb, 4):
                    na = min(4, nb - a0)
                    pf, bpf = self.psf[2 + (a0 // 4) % 2]
                    for aa in range(na):
                        self.tr(pf[:, aa * 128:(aa + 1) * 128], ytok[:, a0 + aa, :], self.ident_f[:],
                                [bpraw, self.b_ident_f], [bpf], inc=(aa == na - 1))
                    self.copy("act", tmp[:, a0 * 128:(a0 + na) * 128], pf[:, 0:na * 128], [bpf], [btmp])
                self.stt(tmp[:, 0:n], u[:, 0:n], skip[:, cc:cc + 1], tmp[:, 0:n], ALU.mult, ALU.add, [bu, btaps, btmp], [btmp])
                self.tt("dve", ub[:, 0:n], tmp[:, 0:n], x0c[:, 0:n], ALU.mult, [btmp, bx0], [bub])
                self.dma("sp", self.ysT3[:, 2 + cc, tok0:tok0 + n], ub[:, 0:n], [bub], [self.dbuf["ysT"]])
        self.barrier()


K.phase_hyena = phase_hyena


NCH = NTOK // CH
C0 = math.exp(-0.5)


def phase_rwkv_prep(self, l):
    A = self.A
    with ExitStack() as es:
        w, bw = self.load_w_in(es, l, "rk_w", 0, 1024)
        mu = self.sb(es, "rk_mu", [128, 8], F32); bmu = Buf()
        om = self.sb(es, "rk_om", [128, 8], F32)
        hm = self.sb(es, "rk_hm", [128, 8], F32)
        self.dma("sp", mu[:], A["rwkv_mu"][l:l + 1, :].rearrange("o (j p) -> p (o j)", p=128), [], [bmu],
                 allow_slow_non_contiguous=True)
        self.ts("dve", om[:], mu[:], -1.0, 1.0, ALU.mult, ALU.add, [bmu], [bmu])
        self.ts("dve", hm[:], mu[:], 0.5, None, ALU.mult, None, [bmu], [bmu])
        colv = self.sb(es, "rk_colv", [128, 16], F32); bcv = Buf()
        for i, nme in enumerate(("rwkv_k_k", "rwkv_k_a", "rwkv_r_k")):
            self.dma("sp", colv[:, 2 * i:2 * i + 2], A[nme][l:l + 1, :].rearrange("o (c p) -> p (o c)", p=128), [], [bcv],
                     allow_slow_non_contiguous=True)
        for d in range(2):
            self.dma("sp", colv[:, 8 + 2 * d:10 + 2 * d], A["rwkv_w0"][l, d:d + 1, :].rearrange("o (c p) -> p (o c)", p=128),
                     [], [bcv], allow_slow_non_contiguous=True)
            self.dma("sp", colv[:, 12 + 2 * d:14 + 2 * d], A["rwkv_a0"][l, d:d + 1, :].rearrange("o (c p) -> p (o c)", p=128),
                     [], [bcv], allow_slow_non_contiguous=True)
        self.ts("dve", colv[:, 6:8], colv[:, 2:4], -1.0, 1.0, ALU.mult, ALU.add, [bcv], [bcv])
        wup = self.sb(es, "rk_wup", [64, 2, 256], F32)
        aup = self.sb(es, "rk_aup", [128, 2, 256], F32)
        gup = self.sb(es, "rk_gup", [128, 256], F32); bwl = Buf()
        for d in range(2):
            self.dma("sp", wup[:, d, :], A["rwkv_w_up"][l, d, :, :], [], [bwl])
            self.dma("sp", aup[64:128, d, :], A["rwkv_a_up"][l, d, :, :], [], [bwl])
        self.dma("sp", gup[:], A["rwkv_g_up"][l, :, :], [], [bwl])
        ones512 = self.sb(es, "rk_ones", [128, CH], F32); bones = Buf()
        self.memset("pool", ones512[:], 1.0, [bones])
        hch = [self.sb(es, "rk_h%d" % i, [128, 8, 514], BF16) for i in range(2)]; bhch = [Buf(), Buf()]
        pj = [self.sb(es, "rk_p%d" % j, [128, 512], F32) for j in range(8)]; bpj = [Buf() for _ in range(8)]
        NWK = 22
        wk = [self.sb(es, "rk_wk%d" % i, [128, 512], F32) for i in range(NWK)]
        bwk = [Buf() for _ in range(NWK)]
        tmo = [self.sb(es, "rk_tmo%d" % i, [128, 4, 128], F32) for i in range(3)]; btmo = [Buf(), Buf(), Buf()]
        wk1 = {i_: self.sb(es, "rk_wkb%d" % i_, [128, 512], F32) for i_ in range(7, NWK)}
        bwk1 = {i_: Buf() for i_ in range(7, NWK)}
        gcs2 = [self.sb(es, "rk_gcs%d" % i, [128, 8], F32) for i in range(2)]; bgcs2 = [Buf(), Buf()]

        def run_rr(gens):
            gens = list(gens)
            while gens:
                for g_ in list(gens):
                    try:
                        next(g_)
                    except StopIteration:
                        gens.remove(g_)
        fmA = A["rk_fm"].rearrange("(d k c) n -> d k c n", d=2, k=4)
        tmA = A["rk_tm"].rearrange("(d n) (k c) -> d n k c", d=2, k=2)
        gcA = A["rk_gc"].rearrange("(d c) n -> d c n", d=2)
        hi = 0
        ti_ = [0]
        psi = [0]

        def nps():
            psi[0] += 1
            return self.psf[psi[0] % 6]

        def to_tm(src, bsrc, nn, dstfn):
            nt = nn // 128
            pf, bpf = nps()
            for a in range(nt):
                self.tr(pf[:, a * 128:(a + 1) * 128], src[:, a * 128:(a + 1) * 128], self.ident_f[:],
                        [bsrc, self.b_ident_f], [bpf], inc=(a == nt - 1))
            t_, bt_ = tmo[ti_[0] % 3], btmo[ti_[0] % 3]
            ti_[0] += 1
            self.copy("act", t_[:, 0:nt, :], pf[:, 0:nt * 128].rearrange("p (a c) -> p a c", c=128), [bpf], [bt_])
            return t_, bt_, nt

        for (col0, tok0, n) in [(CTX0, 0, LC), (LAT0, LC, L)]:
            for t0 in range(0, n, 512):
                nn = min(512, n - t0)
                tk = tok0 + t0
                nsub = nn // CH
                h_, bh_ = hch[hi % 2], bhch[hi % 2]
                hi += 1
                c0 = col0 + t0
                self.dma("sp", h_[:, :, 0:nn + 2], self.hT3[:, :, c0 - 1:c0 + nn + 1], [self.dbuf["hT"]], [bh_])
                for j in range(8):
                    pa, bpa = nps()
                    for k in range(8):
                        self.mm(pa[:, 0:nn], w[:, k, j * 128:(j + 1) * 128], h_[:, k, 1:nn + 1], k == 0, k == 7, [bw, bh_], [bpa])
                    pb, bpb = nps()
                    for k in range(8):
                        self.mm(pb[:, 0:nn], w[:, k, j * 128:(j + 1) * 128], h_[:, k, 0:nn], k == 0, False, [bw, bh_], [bpb], inc=False)
                    for k in range(8):
                        self.mm(pb[:, 0:nn], w[:, k, j * 128:(j + 1) * 128], h_[:, k, 2:nn + 2], False, k == 7, [bw, bh_], [bpb])
                    self.act(pj[j][:, 0:nn], pa[:, 0:nn], AF.Identity, [bpa, bmu], [bpj[j]], scale=om[:, j:j + 1])
                    self.stt(pj[j][:, 0:nn], pb[:, 0:nn], hm[:, j:j + 1], pj[j][:, 0:nn], ALU.mult, ALU.add,
                             [bpb, bmu, bpj[j]], [bpj[j]])
                tw, btw = wk[0], bwk[0]
                sg, bsg = wk[1], bwk[1]
                self.act(tw[0:64, 0:nn], pj[6][0:64, 0:nn], AF.Tanh, [bpj[6]], [btw])
                self.act(sg[:, 0:nn], pj[7][:, 0:nn], AF.Sigmoid, [bpj[7]], [bsg])
                for cc in range(2):
                    S = slice(0, nn)
                    r_, br_ = pj[cc], bpj[cc]
                    k_, bk_ = pj[2 + cc], bpj[2 + cc]
                    v_, bv_ = pj[4 + cc], bpj[4 + cc]
                    pg, bpg = nps()
                    self.mm(pg[:, S], gup[:, cc * 128:(cc + 1) * 128], sg[:, S], True, True, [bwl, bsg], [bpg])
                    gT, bgT = wk[2], bwk[2]
                    self.copy("act", gT[:, S], pg[:, S], [bpg], [bgT])
                    self.dma("sp", A["rk_g"][cc * 128:(cc + 1) * 128, tk:tk + nn], gT[:, S], [bgT], [self.dbuf["rk_g"]])
                    t_, bt_, nt = to_tm(v_, bv_, nn, None)
                    self.dma("sp", A["rk_v"][tk:tk + nn, cc * 128:(cc + 1) * 128].rearrange("(a p) c -> p a c", p=128),
                             t_[:, 0:nt, :], [bt_], [self.dbuf["rk_v"]])
                    kx, bkx = wk[3], bwk[3]
                    sq, bsq = wk[4], bwk[4]
                    kk, bkk = wk[5], bwk[5]
                    self.ts("dve", kx[:, S], k_[:, S], colv[:, 0 + cc:1 + cc], None, ALU.mult, None, [bk_, bcv], [bkx])
                    self.tt("pool", sq[:, S], kx[:, S], kx[:, S], ALU.mult, [bkx], [bsq])
                    pn, bpn = nps()
                    self.mm(pn[:, S], self.blk_f[:], sq[:, S], True, True, [self.b_blk_f, bsq], [bpn])
                    self.ts("dve", sq[:, S], pn[:, S], 1e-24, None, ALU.max, None, [bpn], [bsq])
                    self.act(sq[:, S], sq[:, S], AF.Sqrt, [bsq], [bsq])
                    self.op("dve", lambda e, sq=sq, S=S: e.reciprocal(out=sq[:, S], in_=sq[:, S]), [bsq], [bsq])
                    self.tt("dve", kk[:, S], kx[:, S], sq[:, S], ALU.mult, [bkx, bsq], [bkk])
                    ksum, bks = wk[6], bwk[6]
                    def dchain(d, cc=cc, S=S, nn=nn, nsub=nsub, tk=tk, r_=r_, br_=br_, k_=k_, bk_=bk_, kk=kk, bkk=bkk, ksum=ksum, bks=bks, tw=tw, btw=btw):
                        W = (lambda i_: (wk[i_], bwk[i_])) if d == 0 else (lambda i_: (wk1[i_], bwk1[i_]))
                        gcs, bgcs = gcs2[d], bgcs2[d]
                        pw, bpw = nps()
                        self.mm(pw[:, S], wup[:, d, cc * 128:(cc + 1) * 128], tw[0:64, S], True, True, [bwl, btw], [bpw])
                        sgm, bsgm = W(7)
                        self.act(sgm[:, S], pw[:, S], AF.Sigmoid, [bpw, bcv], [bsgm], bias=colv[:, 8 + 2 * d + cc:9 + 2 * d + cc])
                        yield
                        lw, blw = W(8)
                        self.ts("pool", lw[:, S], sgm[:, S], -C0, None, ALU.mult, None, [bsgm], [blw])
                        pa_, bpa_ = nps()
                        self.mm(pa_[:, S], aup[64:128, d, cc * 128:(cc + 1) * 128], pj[6][64:128, S], True, True, [bwl, bpj[6]], [bpa_])
                        a_, ba_ = W(9)
                        self.act(a_[:, S], pa_[:, S], AF.Sigmoid, [bpa_, bcv], [ba_], bias=colv[:, 12 + 2 * d + cc:13 + 2 * d + cc])
                        yield
                        kd, bkd = W(10)
                        self.ts("dve", kd[:, S], a_[:, S], colv[:, 2 + cc:3 + cc], colv[:, 6 + cc:7 + cc], ALU.mult, ALU.add,
                                [ba_, bcv], [bkd])
                        self.tt("dve", kd[:, S], kd[:, S], k_[:, S], ALU.mult, [bkd, bk_], [bkd])
                        b_, bb_ = W(11)
                        self.tt("pool", b_[:, S], kk[:, S], a_[:, S], ALU.mult, [bkk, ba_], [bb_])
                        if d == 0:
                            self.copy("pool", ksum[:, S], kd[:, S], [bkd], [bks])
                        else:
                            self.tt("pool", ksum[:, S], ksum[:, S], kd[:, S], ALU.add, [bks, bkd], [bks])
                        yield
                        ci_, bci = W(12)
                        for sb_ in range(nsub):
                            sl = slice(sb_ * CH, (sb_ + 1) * CH)
                            self.op("dve", lambda e, ci_=ci_, lw=lw, sl=sl: e.tensor_tensor_scan(
                                out=ci_[:, sl], data0=ones512[:, 0:CH], data1=lw[:, sl], initial=0.0,
                                op0=ALU.mult, op1=ALU.add), [blw, bones], [bci])
                        yield
                        self.copy("dve", gcs[:, 0:nsub], ci_[:, CH - 1:nn:CH], [bci], [bgcs])
                        if d == 1:
                            for sb_ in range(nsub):
                                sl = slice(sb_ * CH, (sb_ + 1) * CH)
                                self.ts("dve", ci_[:, sl], ci_[:, sl], -1.0, gcs[:, sb_:sb_ + 1], ALU.mult, ALU.add, [bci, bgcs], [bci])
                            self.tt("dve", ci_[:, S], ci_[:, S], lw[:, S], ALU.add, [bci, blw], [bci])
                        yield
                        ce, bce = W(13)
                        self.tt("pool", ce[:, S], ci_[:, S], lw[:, S], ALU.subtract, [bci, blw], [bce])
                        yield
                        e1, be1 = W(14)
                        e2, be2 = W(15)
                        e3, be3 = W(16)
                        e4, be4 = W(17)
                        self.act(e1[:, S], ce[:, S], AF.Exp, [bce], [be1])
                        self.act(e2[:, S], ci_[:, S], AF.Exp, [bci], [be2])
                        self.act(e3[:, S], ci_[:, S], AF.Exp, [bci], [be3], scale=-1.0)
                        for sb_ in range(nsub):
                            sl = slice(sb_ * CH, (sb_ + 1) * CH)
                            self.act(e4[:, sl], ci_[:, sl], AF.Exp, [bci, bgcs], [be4], scale=-1.0, bias=gcs[:, sb_:sb_ + 1])
                        yield
                        gce, bgce = W(18)
                        self.act(gce[:, 0:nsub], gcs[:, 0:nsub], AF.Exp, [bgcs], [bgce])
                        self.dma("sp", gcA[d, cc * 128:(cc + 1) * 128, tk // CH:tk // CH + nsub], gce[:, 0:nsub], [bgce],
                                 [self.dbuf["rk_gc"]], allow_slow_non_contiguous=True)
                        yield
                        o_, bo_ = W(19)
                        for kind, (x_, bx_, e_, be_) in enumerate(((kk, bkk, e1, be1), (r_, br_, e2, be2),
                                                                    (b_, bb_, e3, be3), (kd, bkd, e3, be3))):
                            o_, bo_ = W(19 + kind % 2)
                            self.tt("dve" if kind % 2 == 0 else "pool", o_[:, S], x_[:, S], e_[:, S], ALU.mult, [bx_, be_], [bo_])
                            self.dma("sp", fmA[d, kind, cc * 128:(cc + 1) * 128, tk:tk + nn], o_[:, S], [bo_], [self.dbuf["rk_fm"]])
                        yield
                        for kind, (x_, bx_) in enumerate(((b_, bb_), (kd, bkd))):
                            o_, bo_ = W(21)
                            self.tt("dve", o_[:, S], x_[:, S], e4[:, S], ALU.mult, [bx_, be4], [bo_])
                            t_, bt_, nt = to_tm(o_, bo_, nn, None)
                            self.dma("sp", tmA[d, tk:tk + nn, kind, cc * 128:(cc + 1) * 128].rearrange("(a p) c -> p a c", p=128),
                                     t_[:, 0:nt, :], [bt_], [self.dbuf["rk_tm"]])

                    run_rr([dchain(0), dchain(1)])
                    self.stt(ksum[:, S], r_[:, S], colv[:, 4 + cc:5 + cc], ksum[:, S], ALU.mult, ALU.mult, [br_, bcv, bks], [bks])
                    pbn, bpbn = nps()
                    self.mm(pbn[:, S], self.blk_f[:], ksum[:, S], True, True, [self.b_blk_f, bks], [bpbn])
                    bo2, bbo2 = wk[2], bwk[2]
                    self.tt("dve", bo2[:, S], pbn[:, S], v_[:, S], ALU.mult, [bpbn, bv_], [bbo2])
                    self.dma("sp", A["rk_bonus"][cc * 128:(cc + 1) * 128, tk:tk + nn], bo2[:, S], [bbo2], [self.dbuf["rk_bonus"]])
        self.barrier()


K.phase_rwkv_prep = phase_rwkv_prep


def phase_rwkv_scan(self, l, ctx_out):
    A = self.A
    fmA = A["rk_fm"].rearrange("(d k c) n -> d k c n", d=2, k=4)
    tmA = A["rk_tm"].rearrange("(d n) (k c) -> d n k c", d=2, k=2)
    gcA = A["rk_gc"].rearrange("(d c) n -> d c n", d=2)
    yA = A["rk_y"].rearrange("(d n) c -> d n c", d=2)
    F32R = mybir.dt.float32r

    def R_(ap):
        return ap.bitcast(F32R)
    with ExitStack() as es:
        base = self.sb(es, "sc_mbase", [64, 4, 64], F32); bmk = Buf()
        for i, (pat, cm, cmp_) in enumerate((([[1, 64]], -1, ALU.is_gt), ([[1, 64]], -1, ALU.is_ge),
                                             ([[-1, 64]], 1, ALU.is_gt), ([[-1, 64]], 1, ALU.is_ge))):
            self.memset("pool", base[:, i, :], 1.0, [bmk])
            self.op("pool", lambda e, i=i, pat=pat, cm=cm, cmp_=cmp_: e.affine_select(
                out=base[:, i, :], in_=base[:, i, :], pattern=pat, compare_op=cmp_, fill=0.0, base=0,
                channel_multiplier=cm), [bmk], [bmk])
        amask = self.sb(es, "sc_amask", [64, 2, 128], F32)
        mmask = self.sb(es, "sc_mmask", [64, 2, 128], F32)
        ntmask = self.sb(es, "sc_ntmask", [64, 2, 64], F32)
        for d in range(2):
            st_, in_ = (0, 1) if d == 0 else (2, 3)
            self.ts("dve", amask[:, d, 0:64], base[:, st_, :], -1.0, None, ALU.mult, None, [bmk], [bmk])
            self.copy("dve", amask[:, d, 64:128], base[:, st_, :], [bmk], [bmk])
            self.copy("dve", mmask[:, d, 0:64], base[:, in_, :], [bmk], [bmk])
            self.copy("dve", mmask[:, d, 64:128], base[:, in_, :], [bmk], [bmk])
            self.ts("dve", ntmask[:, d, :], base[:, 2 if d == 0 else 0, :], -1.0, None, ALU.mult, None, [bmk], [bmk])
        idb = self.ident_f[0:64, 0:64].unsqueeze(1).to_broadcast([64, 4, 64])
        R = []
        for d in range(2):
            r = {}
            r["gcs"] = self.sb(es, "sc_gcs%d" % d, [64, 4, NCH], F32); r["bgcs"] = Buf()
            self.dma("sp", r["gcs"][:], gcA[d].rearrange("(h k) n -> k h n", k=64), [self.dbuf["rk_gc"]], [r["bgcs"]])
            for nm, shp in (("fm", [64, 4, 4, 64]), ("tm", [64, 3, 256]), ("fmr", [64, 4, 4, 64]), ("tmr", [64, 3, 256]), ("AMa", [64, 4, 128]), ("AMm", [64, 4, 128]),
                            ("NT", [64, 4, 64]), ("AkV", [64, 4, 64]), ("X", [64, 4, 64]), ("XT", [64, 4, 64]),
                            ("P0", [64, 4, 64]), ("P1", [64, 4, 64]), ("Q0", [64, 4, 64]), ("Q1", [64, 4, 64])):
                r[nm] = [self.sb(es, "sc_%s%d_%d" % (nm, d, s_), shp, F32) for s_ in range(2)]
                r["b" + nm] = [Buf(), Buf()]
            for nm in ("RHS", "Zs", "Ys", "ST2", "STr"):
                r[nm] = self.sb(es, "sc_%s%d" % (nm, d), [64, 4, 64], F32); r["b" + nm] = Buf()
            r["ST"] = [self.sb(es, "sc_ST%d_%d" % (d, s_), [64, 4, 64], F32) for s_ in range(2)]
            r["bST"] = [Buf(), Buf()]
            self.memset("pool", r["ST"][0][:], 0.0, [r["bST"][0]])
            self.copy("dve", R_(r["STr"][:]), r["ST"][0][:], [r["bST"][0]], [r["bSTr"]])
            r["B0"] = self.psf[2 * d]
            r["B1"] = self.psf[2 * d + 1]
            r["B3"] = self.psf[4 + d]
            r["B2"] = (self.psb[d][0][:].bitcast(F32), self.psb[d][1])
            r["order"] = ([0, 1, 2, 3] + list(range(4, NCH))) if d == 0 else ([3, 2, 1, 0] + list(range(NCH - 1, 3, -1)))
            R.append(r)

        def v4(ap):
            return ap.rearrange("p (h t) -> p h t", h=4)

        def pre(d, g, s_):
            r = R[d]
            fm, bfm = r["fm"][s_], r["bfm"][s_]
            tm, btm = r["tm"][s_], r["btm"][s_]
            tsl = slice(g * CH, (g + 1) * CH)
            for kind in range(4):
                self.dma("sp", fm[:, :, kind, :], fmA[d, kind].rearrange("(h k) n -> k h n", k=64)[:, :, tsl],
                         [self.dbuf["rk_fm"]], [bfm])
            self.dma("sp", tm[:, 0:2, :], tmA[d, tsl, :, :], [self.dbuf["rk_tm"]], [btm])
            self.dma("sp", tm[:, 2, :], A["rk_v"][tsl, :], [self.dbuf["rk_v"]], [btm])
            yield
            fmr, bfmr = r["fmr"][s_], r["bfmr"][s_]
            tmr, btmr = r["tmr"][s_], r["btmr"][s_]
            self.copy("pool", R_(fmr[:]), fm[:], [bfm], [bfmr])
            self.copy("act", R_(tmr[:]), tm[:], [btm], [btmr])
            yield
            fm, bfm, tm, btm = fmr, bfmr, tmr, btmr
            b0, bb0 = r["B0"]
            b1, bb1 = r["B1"]
            b2, bb2 = r["B2"]
            AMa, bAMa = r["AMa"][s_], r["bAMa"][s_]
            AMm, bAMm = r["AMm"][s_], r["bAMm"][s_]
            NT, bNT = r["NT"][s_], r["bNT"][s_]
            AkV, bAkV = r["AkV"][s_], r["bAkV"][s_]
            X, bX = r["X"][s_], r["bX"][s_]
            for h in range(4):
                self.mm(b0[0:64, h * 128:h * 128 + 64], R_(fm[:, h, 2, :]), R_(fm[:, h, 0, :]), True, True, [bfm], [bb0], inc=False)
                self.mm(b0[0:64, h * 128 + 64:h * 128 + 128], R_(fm[:, h, 3, :]), R_(fm[:, h, 0, :]), True, True, [bfm], [bb0], inc=(h == 3))
            for h in range(4):
                self.mm(b1[0:64, h * 64:(h + 1) * 64], R_(fm[:, h, 0, :]), R_(fm[:, h, 2, :]), True, True, [bfm], [bb1], inc=(h == 3))
            for h in range(4):
                self.mm(b2[0:64, h * 128:h * 128 + 64], R_(fm[:, h, 2, :]), R_(fm[:, h, 1, :]), True, True, [bfm], [bb2], inc=False)
                self.mm(b2[0:64, h * 128 + 64:h * 128 + 128], R_(fm[:, h, 3, :]), R_(fm[:, h, 1, :]), True, True, [bfm], [bb2], inc=(h == 3))
            yield
            self.tt("dve", R_(AMa[:]), v4(b0[0:64, :]), amask[:, d, :].unsqueeze(1).to_broadcast([64, 4, 128]), ALU.mult, [bb0, bmk], [bAMa])
            self.tt("dve", R_(NT[:]), v4(b1[0:64, 0:256]), ntmask[:, d, :].unsqueeze(1).to_broadcast([64, 4, 64]), ALU.mult, [bb1, bmk], [bNT])
            self.tt("dve", R_(AMm[:]), v4(b2[0:64, :]), mmask[:, d, :].unsqueeze(1).to_broadcast([64, 4, 128]), ALU.mult, [bb2, bmk], [bAMm])
            yield
            self.tt("dve", R_(X[:]), AMa[:, :, 0:64], idb, ALU.add, [bAMa, self.b_ident_f], [bX])
            Pl = [(AMa[:, :, 0:64], bAMa)]
            Ql = [(NT[:], bNT)]
            for j in range(1, 6):
                Pl.append((r["P%d" % (j % 2)][s_][:], r["bP%d" % (j % 2)][s_]))
                Ql.append((r["Q%d" % (j % 2)][s_][:], r["bQ%d" % (j % 2)][s_]))
            for stg in range(1, 7):
                if stg == 1:
                    for h in range(4):
                        self.mm(b1[0:64, 256 + h * 64:256 + (h + 1) * 64], R_(AMa[:, h, 64:128]), R_(tm[:, 2, h * 64:(h + 1) * 64]), True, True,
                                [bAMa, btm], [bb1], inc=(h == 3))
                if stg <= 5:
                    (Pp, bPp), (Qp, bQp) = Pl[stg - 1], Ql[stg - 1]
                    if stg < 5:
                        for h in range(4):
                            self.mm(b0[0:64, h * 64:(h + 1) * 64], R_(Qp[:, h, :]), R_(Pp[:, h, :]), True, True, [bPp, bQp], [bb0], inc=False)
                    for h in range(4):
                        self.mm(b0[0:64, 256 + h * 64:256 + (h + 1) * 64], R_(Pp[:, h, :]), R_(Qp[:, h, :]), True, True, [bPp, bQp], [bb0],
                                inc=(h == 3))
                if stg >= 2:
                    Qa, bQa = Ql[stg - 1]
                    for h in range(4):
                        self.mm(b1[0:64, h * 64:(h + 1) * 64], R_(Qa[:, h, :]), R_(X[:, h, :]), True, True, [bQa, bX], [bb1], inc=(h == 3))
                yield
                if stg == 1:
                    self.copy("dve", AkV[:], v4(b1[0:64, 256:512]), [bb1], [bAkV])
                if stg >= 2:
                    self.tt("dve", R_(X[:]), v4(b1[0:64, 0:256]), X[:], ALU.add, [bb1, bX], [bX])
                if stg <= 5:
                    if stg < 5:
                        Pn, bPn = Pl[stg]
                        self.copy("act", R_(Pn), v4(b0[0:64, 0:256]), [bb0], [bPn])
                    Qn, bQn = Ql[stg]
                    self.copy("act", R_(Qn), v4(b0[0:64, 256:512]), [bb0], [bQn])
                yield

        def seq(d, g, s_, i):
            r = R[d]
            fm, bfm = r["fmr"][s_], r["bfmr"][s_]
            tm, btm = r["tmr"][s_], r["btmr"][s_]
            b2, bb2 = r["B2"]
            b3, bb3 = r["B3"]
            STr, bSTr = r["STr"], r["bSTr"]
            STc, bSTc = r["ST"][i % 2], r["bST"][i % 2]
            STn, bSTn = r["ST"][(i + 1) % 2], r["bST"][(i + 1) % 2]
            X, bX = r["X"][s_], r["bX"][s_]
            AMm, bAMm = r["AMm"][s_], r["bAMm"][s_]
            AkV, bAkV = r["AkV"][s_], r["bAkV"][s_]
            RHS, bRHS = r["RHS"], r["bRHS"]
            Zs, bZs = r["Zs"], r["bZs"]
            Ys, bYs = r["Ys"], r["bYs"]
            ST2, bST2 = r["ST2"], r["bST2"]
            for h in range(4):
                self.mm(b3[0:64, h * 64:(h + 1) * 64], R_(fm[:, h, 0, :]), R_(STr[:, h, :]), True, True, [bfm, bSTr], [bb3], inc=(h == 3))
            self.tt("pool", ST2[:], STc[:], r["gcs"][:, :, g:g + 1].to_broadcast([64, 4, 64]), ALU.mult, [bSTc, r["bgcs"]], [bST2])
            yield
            self.stt(R_(RHS[:]), v4(b3[0:64, 0:256]), -1.0, AkV[:], ALU.mult, ALU.subtract, [bb3, bAkV], [bRHS])
            yield
            for h in range(4):
                self.mm(b3[0:64, 256 + h * 64:256 + (h + 1) * 64], R_(X[:, h, :]), R_(RHS[:, h, :]), True, True, [bX, bRHS], [bb3], inc=(h == 3))
            yield
            self.copy("act", R_(Zs[:]), v4(b3[0:64, 256:512]), [bb3], [bZs])
            yield
            emit = ctx_out or g >= 4
            for h in range(4):
                self.mm(b2[0:64, h * 64:(h + 1) * 64], R_(tm[:, 0, h * 64:(h + 1) * 64]), R_(Zs[:, h, :]), True, False, [btm, bZs], [bb2], inc=False)
                self.mm(b2[0:64, h * 64:(h + 1) * 64], R_(tm[:, 1, h * 64:(h + 1) * 64]), R_(tm[:, 2, h * 64:(h + 1) * 64]), False, True,
                        [btm], [bb2], inc=(h == 3 and not emit))
            if emit:
                for h in range(4):
                    o = b2[0:64, 256 + h * 64:256 + (h + 1) * 64]
                    self.mm(o, R_(fm[:, h, 1, :]), R_(STr[:, h, :]), True, False, [bfm, bSTr], [bb2], inc=False)
                    self.mm(o, R_(AMm[:, h, 0:64]), R_(Zs[:, h, :]), False, False, [bAMm, bZs], [bb2], inc=False)
                    self.mm(o, R_(AMm[:, h, 64:128]), R_(tm[:, 2, h * 64:(h + 1) * 64]), False, True, [bAMm, btm], [bb2], inc=(h == 3))
            yield
            self.tt("dve", STn[:], v4(b2[0:64, 0:256]), ST2[:], ALU.add, [bb2, bST2], [bSTn])
            self.copy("act", R_(STr[:]), STn[:], [bSTn], [bSTr])
            if emit:
                self.copy("dve", Ys[:], v4(b2[0:64, 256:512]), [bb2], [bYs])
                self.dma("sp", yA[d, g * CH:(g + 1) * CH, :], Ys[:].rearrange("p h t -> p (h t)"), [bYs], [self.dbuf["rk_y"]])
            yield

        def run_rr(gens):
            gens = list(gens)
            while gens:
                for g_ in list(gens):
                    try:
                        next(g_)
                    except StopIteration:
                        gens.remove(g_)

        run_rr([pre(d, R[d]["order"][0], 0) for d in range(2)])
        for i in range(NCH):
            gl = []
            for d in range(2):
                if i + 1 < NCH:
                    gl.append(pre(d, R[d]["order"][i + 1], (i + 1) % 2))
                gl.append(seq(d, R[d]["order"][i], i % 2, i))
            run_rr(gl)
        self.barrier()


K.phase_rwkv_scan = phase_rwkv_scan


def phase_rwkv_out(self, l, tiles):
    A = self.A
    yA = A["rk_y"].rearrange("(d n) c -> d n c", d=2)
    with ExitStack() as es:
        gb = self.sb(es, "ro_gb", [128, 2, 256], F32); bgb = Buf()
        self.dma("sp", gb[:, 0, :], A["rwkv_lnx_g"][l:l + 1, :].partition_broadcast(128), [], [bgb])
        self.dma("sp", gb[:, 1, :], A["rwkv_lnx_b"][l:l + 1, :].partition_broadcast(128), [], [bgb])
        NB = 2
        yf = [self.sb(es, "ro_yf%d" % i, [128, 256], F32) for i in range(NB)]
        yb = [self.sb(es, "ro_yb%d" % i, [128, 256], F32) for i in range(NB)]
        bo = [self.sb(es, "ro_bo%d" % i, [128, 2, 128], F32) for i in range(NB)]
        gt = [self.sb(es, "ro_gt%d" % i, [128, 2, 128], F32) for i in range(NB)]
        bin_ = [Buf() for _ in range(NB)]
        st = self.sb(es, "ro_st", [128, 4, 6], F32); mv = self.sb(es, "ro_mv", [128, 4, 2], F32)
        rs = self.sb(es, "ro_rs", [128, 4], F32); bs = Buf()
        yn = self.sb(es, "ro_yn", [128, 256], F32); byn = Buf()
        res = [self.sb(es, "ro_res%d" % i, [128, 2, 128], BF16) for i in range(NB)]; bres = [Buf() for _ in range(NB)]
        tmp = self.sb(es, "ro_tmp", [128, 2, 128], F32); btmp = Buf()
        def tile_gen(n, ti):
            s = n % NB
            tk = ti * 128
            self.dma("sp", yf[s][:], yA[0, tk:tk + 128, :], [self.dbuf["rk_y"]], [bin_[s]])
            self.dma("sp", yb[s][:], yA[1, tk:tk + 128, :], [self.dbuf["rk_y"]], [bin_[s]])
            self.dma("sp", bo[s][:], A["rk_bonus"].rearrange("(c p) n -> p c n", p=128)[:, :, tk:tk + 128], [self.dbuf["rk_bonus"]], [bin_[s]])
            self.dma("sp", gt[s][:], A["rk_g"].rearrange("(c p) n -> p c n", p=128)[:, :, tk:tk + 128], [self.dbuf["rk_g"]], [bin_[s]])
            yield
            self.tt("dve", yf[s][:], yf[s][:], yb[s][:], ALU.add, [bin_[s]], [bin_[s]])
            for h in range(4):
                self.op("dve", lambda e, h=h, s=s: e.bn_stats(out=st[:, h, :], in_=yf[s][:, h * 64:(h + 1) * 64]), [bin_[s]], [bs])
                self.op("dve", lambda e, h=h: e.bn_aggr(out=mv[:, h, :], in_=st[:, h, :]), [bs], [bs])
            yield True
            self.act(rs[:], mv[:, :, 1], AF.Sqrt, [bs], [bs], bias=64e-5, scale=1.0)
            self.op("dve", lambda e: e.reciprocal(out=rs[:], in_=rs[:]), [bs], [bs])
            for h in range(4):
                self.ts("dve", yn[:, h * 64:(h + 1) * 64], yf[s][:, h * 64:(h + 1) * 64], mv[:, h, 0:1], rs[:, h:h + 1],
                        ALU.subtract, ALU.mult, [bin_[s], bs], [byn])
            self.tt("pool", yn[:], yn[:], gb[:, 0, :], ALU.mult, [byn, bgb], [byn])
            self.tt("pool", yn[:], yn[:], gb[:, 1, :], ALU.add, [byn, bgb], [byn])
            yield
            pf, bpf = self.psf[n % 2]
            for cc in range(2):
                self.tr(pf[:, cc * 128:(cc + 1) * 128], yn[:, cc * 128:(cc + 1) * 128], self.ident_f[:], [byn, self.b_ident_f], [bpf],
                        inc=(cc == 1))
            yield
            self.tt("dve", tmp[:], pf[:, 0:256].rearrange("p (c t) -> p c t", c=2), bo[s][:], ALU.add, [bpf, bin_[s]], [btmp])
            self.tt("dve", res[s][:], tmp[:], gt[s][:], ALU.mult, [btmp, bin_[s]], [bres[s]])
            self.dma("sp", self.ysT3[:, 0:2, tk:tk + 128], res[s][:], [bres[s]], [self.dbuf["ysT"]])
            yield

        run_pipeline((tile_gen(n, ti) for n, ti in enumerate(tiles)), depth=2)
        self.barrier()


K.phase_rwkv_out = phase_rwkv_out


def phase_rwkv(self, l, ctx_out):
    self.phase_rwkv_prep(l)
    self.phase_rwkv_scan(l, ctx_out)
    self.phase_rwkv_out(l, list(range(NTILE)) if ctx_out else list(range(2, NTILE)))


def phase_rwkv_out_merge(self, l, ctx_out, tiles_merge):
    with ExitStack() as es:
        pre = self.merge_load(es, l)
        self.phase_rwkv_out(l, list(range(NTILE)) if ctx_out else list(range(2, NTILE)))
        self.phase_merge(l, tiles_merge, pre=pre)


K.phase_rwkv_out_merge = phase_rwkv_out_merge


K.phase_rwkv = phase_rwkv


def build_program(dbg=False):
    nc = bass.Bass("TRN2", target_bir_lowering=False)
    k = K(nc, dbg=dbg)
    k.setup()
    alltiles = list(range(NTILE))
    lat = list(range(2, NTILE))
    for l in range(DEPTH):
        ctx_out = l < DEPTH - 1
        k.phase_mod(l)
        k.phase_h(l, alltiles)
        k.phase_sconv(l, ctx_out)
        k.phase_attn(l, ctx_out)
        k.phase_hyena(l, ctx_out)
        k.phase_rwkv_prep(l)
        k.phase_rwkv_scan(l, ctx_out)
        tl = alltiles if ctx_out else lat
        k.phase_rwkv_out_merge(l, ctx_out, tl)
        k.phase_moe(l, tl, not ctx_out)
    k.P.finish([k.dbuf["out"]])
    return nc, k


_CACHE = {}


def kernel(**inputs):
    n = 8
    if "nc" not in _CACHE:
        _CACHE["nc"] = build_program()[0]
    nc = _CACHE["nc"]
    maps = make_in_maps(inputs, list(range(n)))
    res = run_bass_kernel_spmd(nc, maps, core_ids=list(range(n)))
    out = np.stack([np.asarray(res.results[i]["out"], dtype=np.float32) for i in range(n)], 0)
    return out
```

```python
from contextlib import ExitStack
import math
import numpy as np
import concourse.bass as bass
import concourse.mybir as mybir
from concourse.bass_utils import run_bass_kernel_spmd

F32 = mybir.dt.float32
BF16 = mybir.dt.bfloat16
I32 = mybir.dt.int32
AF = mybir.ActivationFunctionType
ALU = mybir.AluOpType
AX = mybir.AxisListType


class Buf:
    __slots__ = ("name", "w", "r", "excl")

    def __init__(self, name="", excl=False):
        self.name = name
        self.w = {}
        self.r = {}
        self.excl = excl


class Prog:
    CE = ["pe", "dve", "act", "pool", "sp"]
    SAME = {"pe": False, "dve": True, "act": True, "pool": True, "sp": False}
    NDQ = 6

    def __init__(self, nc):
        self.nc = nc
        self.ops = {e: [] for e in self.CE}
        self.cnt = {}
        self.sem = {}
        self.seen = {e: {} for e in self.CE}
        for e in self.CE:
            self.sem[e] = nc.alloc_semaphore("sem_" + e)
            self.cnt[e] = 0
        self.dq = {}
        self.dq_next = {}
        for q in ("sp", "pool", "act"):
            names = []
            for i in range(self.NDQ):
                n = "dq_%s_%d" % (q, i)
                self.sem[n] = nc.alloc_semaphore("sem_" + n)
                self.cnt[n] = 0
                names.append(n)
            self.dq[q] = names
            self.dq_next[q] = 0
        self.nins = 0

    def _mult(self, e):
        return 16 if e.startswith("dq_") else 1

    def _waits(self, eng, reads, writes, extra=()):
        need = {}

        def add(e2, c):
            if c > need.get(e2, 0):
                need[e2] = c
        for b in reads:
            for e2, c in b.w.items():
                add(e2, c)
            if b.excl:
                for e2, c in b.r.items():
                    if e2 != eng:
                        add(e2, c)
        for b in writes:
            for e2, c in b.w.items():
                add(e2, c)
            for e2, c in b.r.items():
                add(e2, c)
        for e2, c in extra:
            add(e2, c)
        out = []
        seen = self.seen[eng]
        for e2, c in need.items():
            if e2 == eng and not self.SAME[eng]:
                continue
            if c > seen.get(e2, 0):
                seen[e2] = c
                out.append((self.sem[e2], c * self._mult(e2)))
        return out

    def op(self, eng, fn, reads=(), writes=(), inc=True):
        waits = self._waits(eng, reads, writes)
        if inc:
            self.cnt[eng] += 1
            my = self.cnt[eng]
        else:
            my = self.cnt[eng] + 1
        self.ops[eng].append((waits, fn, self.sem[eng] if inc else None, 1))
        for b in reads:
            b.r[eng] = max(b.r.get(eng, 0), my)
        for b in writes:
            b.w[eng] = my
            b.r = {}
        self.nins += 1

    def dma(self, q, fn, reads=(), writes=()):
        names = self.dq[q]
        n = names[self.dq_next[q] % len(names)]
        self.dq_next[q] += 1
        extra = [(n, self.cnt[n])] if self.cnt[n] > 0 else []
        waits = self._waits(q, reads, writes, extra)
        self.cnt[n] += 1
        my = self.cnt[n]
        self.ops[q].append((waits, fn, self.sem[n], 16))
        for b in reads:
            b.r[n] = max(b.r.get(n, 0), my)
        for b in writes:
            b.w[n] = my
            b.r = {}
        self.nins += 1

    def finish(self, final_bufs):
        waits = self._waits("sp", final_bufs, ())
        self.ops["sp"].append((waits, None, None, 0))
        nc = self.nc
        ops = self.ops
        with nc.Block() as block:
            def emit(engobj, lst):
                for waits, fn, sem, k in lst:
                    for s, v in waits:
                        engobj.wait_ge(s, v)
                    if fn is not None:
                        ins = fn(engobj)
                        if sem is not None:
                            ins.then_inc(sem, k)

            @block.tensor
            def _(e):
                emit(e, ops["pe"])

            @block.vector
            def _(e):
                emit(e, ops["dve"])

            @block.scalar
            def _(e):
                emit(e, ops["act"])

            @block.gpsimd
            def _(e):
                emit(e, ops["pool"])

            @block.sync
            def _(e):
                emit(e, ops["sp"])


D = 1024
L = 4096
LC = 256
NTOK = L + LC
NTILE = NTOK // 128
NTP = NTOK + 4
CTX0 = 1
LAT0 = 259
DEPTH = 2
IN_COLS = 7168
OFF_HYENA = 1024
OFF_SCONV = 1792
OFF_ATTN = 2560
OFF_GATE = 3072
NE = 16
DE = 512
ALPHA = (2 * DEPTH) ** 0.25
LN_EPS = 1e-6
PI = math.pi
TWO_PI = 2.0 * math.pi
MAGIC = 12582912.0
CH = 64
SBUF_LIMIT = 208 * 1024


def tok_col(t):
    return CTX0 + t if t < LC else LAT0 + (t - LC)


class K:
    def __init__(self, nc, dbg=False):
        self.nc = nc
        self.P = Prog(nc)
        self.dbg = dbg
        self.dram = {}
        self.dbuf = {}

    def din(self, name, shape, dt=F32):
        t = self.nc.dram_tensor(name, list(shape), dt, kind="ExternalInput")
        self.dram[name] = t
        self.dbuf[name] = Buf(name)
        return t.ap()

    def dscr(self, name, shape, dt=F32, out=False):
        kind = "ExternalOutput" if (out or self.dbg) else "Internal"
        t = self.nc.dram_tensor(name, list(shape), dt, kind=kind)
        self.dram[name] = t
        self.dbuf[name] = Buf(name)
        return t.ap()

    def sb(self, es, name, shape, dt=F32):
        self._uid = getattr(self, "_uid", 0) + 1
        t = es.enter_context(self.nc.sbuf_tensor("%s_u%d" % (name, self._uid), list(shape), dt))
        nb = 1
        for d_ in shape[1:]:
            nb *= d_
        nb *= 2 if dt == BF16 else 4
        nb = (nb + 31) // 32 * 32
        self.sb_used = getattr(self, "sb_used", 17 * 1024) + nb
        self.sb_peak = max(getattr(self, "sb_peak", 0), self.sb_used)

        def _free(nb=nb):
            self.sb_used -= nb
        es.callback(_free)
        if self.sb_used > SBUF_LIMIT:
            raise RuntimeError("SBUF budget exceeded at %s: %d" % (name, self.sb_used))
        return t

    def op(self, eng, fn, r=(), w=(), inc=True):
        self.P.op(eng, fn, r, w, inc)

    def dma(self, q, out, in_, r=(), w=(), **kw):
        self.P.dma(q, lambda e: e.dma_start(out=out, in_=in_, **kw), r, w)

    def copy(self, eng, out, in_, r, w):
        if eng == "act":
            self.op("act", lambda e: e.activation(out=out, in_=in_, func=AF.Copy), r, w)
        else:
            self.op(eng, lambda e: e.tensor_copy(out=out, in_=in_), r, w)

    def act(self, out, in_, func, r, w, bias=0.0, scale=1.0):
        self.op("act", lambda e: e.activation(out=out, in_=in_, func=func, bias=bias, scale=scale), r, w)

    def ts(self, eng, out, in0, s1, s2, op0, op1, r, w):
        if op1 is None:
            self.op(eng, lambda e: e.tensor_scalar(out=out, in0=in0, scalar1=s1, scalar2=None, op0=op0), r, w)
        else:
            self.op(eng, lambda e: e.tensor_scalar(out=out, in0=in0, scalar1=s1, scalar2=s2, op0=op0, op1=op1), r, w)

    def tt(self, eng, out, in0, in1, op, r, w):
        self.op(eng, lambda e: e.tensor_tensor(out=out, in0=in0, in1=in1, op=op), r, w)

    def stt(self, out, in0, scalar, in1, op0, op1, r, w):
        self.op("dve", lambda e: e.scalar_tensor_tensor(out=out, in0=in0, scalar=scalar, in1=in1, op0=op0, op1=op1), r, w)

    def mm(self, out, lhsT, rhs, start, stop, r, w, inc=None):
        if inc is None:
            inc = stop
        self.op("pe", lambda e: e.matmul(out, lhsT=lhsT, rhs=rhs, start=start, stop=stop), r, w, inc)

    def tr(self, out, in_, ident, r, w, inc=True):
        self.op("pe", lambda e: e.transpose(out=out, in_=in_, identity=ident), r, w, inc)

    def memset(self, eng, ap, val, w):
        self.op(eng, lambda e: e.memset(ap, val), (), w)

    def barrier(self):
        P = self.P
        allc = [(e, c) for e, c in P.cnt.items() if c > 0]
        for eng in P.CE:
            waits = []
            for e2, c in allc:
                if e2 == eng and not P.SAME[eng]:
                    continue
                if c > P.seen[eng].get(e2, 0):
                    P.seen[eng][e2] = c
                    waits.append((P.sem[e2], c * P._mult(e2)))
            if waits:
                P.ops[eng].append((waits, None, None, 0))

    def range_reduce(self, x, tmp, r, w):
        bufs = list(set(list(r) + list(w)))
        self.ts("dve", tmp, x, 1.0 / TWO_PI, MAGIC, ALU.mult, ALU.add, bufs, bufs)
        self.ts("dve", tmp, tmp, -MAGIC, None, ALU.add, None, bufs, bufs)
        self.stt(x, tmp, -TWO_PI, x, ALU.mult, ALU.add, bufs, bufs)
        self.ts("dve", tmp, x, PI, -TWO_PI, ALU.is_gt, ALU.mult, bufs, bufs)
        self.tt("dve", x, x, tmp, ALU.add, bufs, bufs)
        self.ts("dve", tmp, x, -PI, TWO_PI, ALU.is_lt, ALU.mult, bufs, bufs)
        self.tt("dve", x, x, tmp, ALU.add, bufs, bufs)
        self.ts("dve", x, x, -PI, PI, ALU.max, ALU.min, bufs, bufs)

    def setup(self):
        nc = self.nc
        A = {}
        self.A = A
        A["x"] = self.din("x", [L, D])
        A["c"] = self.din("c", [1, D])
        A["ctx"] = self.din("ctx", [LC, D])
        A["c_ctx"] = self.din("c_ctx", [1, D])
        A["ada_w"] = self.din("ada_w", [DEPTH, D, 6 * D])
        A["ada_b"] = self.din("ada_b", [DEPTH, 6 * D])
        A["w_in"] = self.din("w_in", [DEPTH, D, IN_COLS])
        A["rwkv_mu"] = self.din("rwkv_mu", [DEPTH, 1024])
        A["rwkv_w0"] = self.din("rwkv_w0", [DEPTH, 2, 256])
        A["rwkv_w_up"] = self.din("rwkv_w_up", [DEPTH, 2, 64, 256])
        A["rwkv_a0"] = self.din("rwkv_a0", [DEPTH, 2, 256])
        A["rwkv_a_up"] = self.din("rwkv_a_up", [DEPTH, 2, 64, 256])
        A["rwkv_g_up"] = self.din("rwkv_g_up", [DEPTH, 128, 256])
        for n in ("rwkv_k_k", "rwkv_k_a", "rwkv_r_k", "rwkv_lnx_g", "rwkv_lnx_b", "hyena_skip"):
            A[n] = self.din(n, [DEPTH, 256])
        A["hyena_conv"] = self.din("hyena_conv", [DEPTH, 3, 768])
        A["hyena_w1"] = self.din("hyena_w1", [DEPTH, 33, 64])
        A["hyena_b1"] = self.din("hyena_b1", [DEPTH, 64])
        A["hyena_freq1"] = self.din("hyena_freq1", [DEPTH, 64])
        A["hyena_w2"] = self.din("hyena_w2", [DEPTH, 64, 64])
        A["hyena_b2"] = self.din("hyena_b2", [DEPTH, 64])
        A["hyena_freq2"] = self.din("hyena_freq2", [DEPTH, 64])
        A["hyena_w3"] = self.din("hyena_w3", [DEPTH, 64, 512])
        A["sconv_w"] = self.din("sconv_w", [DEPTH, 3, 256])
        A["attn_q_norm"] = self.din("attn_q_norm", [DEPTH, 64])
        A["attn_k_norm"] = self.din("attn_k_norm", [DEPTH, 64])
        A["w_branch"] = self.din("w_branch", [DEPTH, 4, 256, D])
        A["w_out"] = self.din("w_out", [DEPTH, D, D])
        for n in ("ln1_g", "ln1_b", "ln2_g", "ln2_b"):
            A[n] = self.din(n, [DEPTH, D])
        A["router_w"] = self.din("router_w", [D, NE])
        A["router_bias"] = self.din("router_bias", [1, NE])
        A["exp_w1"] = self.din("exp_w1", [DEPTH, NE, D, DE])
        A["exp_w3"] = self.din("exp_w3", [DEPTH, NE, D, DE])
        A["exp_w2"] = self.din("exp_w2", [DEPTH, NE, DE, D])
        A["rope_cs"] = self.din("rope_cs", [L, 64])
        A["hy_z"] = self.din("hy_z", [33, 2 * L])
        A["hy_zc"] = self.din("hy_zc", [33, 2 * LC])
        A["hy_t"] = self.din("hy_t", [1, 2 * L])
        A["hy_tc"] = self.din("hy_tc", [1, 2 * LC])
        A["hy_delta"] = self.din("hy_delta", [256, 1])
        A["out"] = self.dscr("out", [L, D], F32, out=True)
        A["xres"] = self.dscr("xres", [NTOK, D], F32)
        A["hT"] = self.dscr("hT", [128, 8 * NTP], BF16)
        A["hfT"] = self.dscr("hfT", [128, 8 * NTOK], BF16)
        A["ysT"] = self.dscr("ysT", [128, 8 * NTOK], BF16)
        A["rk_fm"] = self.dscr("rk_fm", [2 * 4 * 256, NTOK], F32)
        A["rk_tm"] = self.dscr("rk_tm", [2 * NTOK, 2 * 256], F32)
        A["rk_v"] = self.dscr("rk_v", [NTOK, 256], F32)
        A["rk_gc"] = self.dscr("rk_gc", [2 * 256, NTOK // 64], F32)
        A["rk_g"] = self.dscr("rk_g", [256, NTOK], F32)
        A["rk_bonus"] = self.dscr("rk_bonus", [256, NTOK], F32)
        A["rk_y"] = self.dscr("rk_y", [2 * NTOK, 256], F32)
        A["ewb1"] = self.dscr("ewb1", [NE * D, DE], BF16)
        A["ewb3"] = self.dscr("ewb3", [NE * D, DE], BF16)
        A["ewb2"] = self.dscr("ewb2", [NE * DE, D], BF16)
        A["hyG"] = self.dscr("hyG", [256, 2 * L], BF16)
        A["hyGc"] = self.dscr("hyGc", [256, 2 * LC], BF16)
        self.hT3 = A["hT"].rearrange("p (k n) -> p k n", k=8)
        self.hfT3 = A["hfT"].rearrange("p (k n) -> p k n", k=8)
        self.ysT3 = A["ysT"].rearrange("p (k n) -> p k n", k=8)

        self.ges = ExitStack()
        es = self.ges
        self.ident_b = self.sb(es, "ident_b", [128, 128], BF16); self.b_ident_b = Buf()
        self.ident_f = self.sb(es, "ident_f", [128, 128], F32); self.b_ident_f = Buf()
        self.flip_b = self.sb(es, "flip_b", [128, 128], BF16); self.b_flip_b = Buf()
        self.ones_f = self.sb(es, "ones_f", [128, 128], F32); self.b_ones_f = Buf()
        self.blk_f = self.sb(es, "blk_f", [128, 128], F32); self.b_blk_f = Buf()
        self.sel2 = self.sb(es, "sel2", [2, 2, 128], F32); self.b_sel2 = Buf()
        self.modT = self.sb(es, "modT", [128, 48, 2], F32); self.b_modT = Buf()
        self.gbc = self.sb(es, "gbc", [128, 4, D], F32); self.b_gbc = Buf()
        self.lnbc = self.sb(es, "lnbc", [128, 4, D], F32); self.b_lnbc = Buf()
        self.gates = self.sb(es, "gates", [128, NTILE, NE], F32); self.b_gates = Buf()
        self.rw32 = self.sb(es, "rw32", [128, 8, NE], F32); self.b_rw32 = Buf()
        self.rbias = self.sb(es, "rbias", [128, NE], F32); self.b_rbias = Buf()
        self.psf = []
        for i in range(6):
            t = nc.alloc_psum_tensor("psf%d" % i, [128, 512], F32)
            self.psf.append((t, Buf("psf%d" % i, excl=True)))
        self.psb = []
        for i in range(2):
            t = nc.alloc_psum_tensor("psb%d" % i, [128, 1024], BF16)
            self.psb.append((t, Buf("psb%d" % i, excl=True)))

        self.psb_f32 = [(t_[:].bitcast(F32), b_) for (t_, b_) in self.psb]
        G = "pool"
        tmpf = self.sb(es, "tmp_idf", [128, 128], F32); b_tmp = Buf()
        self.memset(G, self.ident_f[:], 0.0, [self.b_ident_f])
        self.op(G, lambda e: e.affine_select(out=self.ident_f[:], in_=self.ident_f[:], pattern=[[-1, 128]],
                                              compare_op=ALU.not_equal, fill=1.0, base=0, channel_multiplier=1),
                [self.b_ident_f], [self.b_ident_f])
        self.copy("dve", self.ident_b[:], self.ident_f[:], [self.b_ident_f], [self.b_ident_b])
        self.memset(G, tmpf[:], 0.0, [b_tmp])
        self.op(G, lambda e: e.affine_select(out=tmpf[:], in_=tmpf[:], pattern=[[1, 128]],
                                              compare_op=ALU.not_equal, fill=1.0, base=-127, channel_multiplier=1),
                [b_tmp], [b_tmp])
        self.copy("dve", self.flip_b[:], tmpf[:], [b_tmp], [self.b_flip_b])
        self.memset(G, self.ones_f[:], 1.0, [self.b_ones_f])
        self.memset(G, self.blk_f[:], 0.0, [self.b_blk_f])
        self.memset(G, self.blk_f[0:64, 0:64], 1.0, [self.b_blk_f])
        self.memset(G, self.blk_f[64:128, 64:128], 1.0, [self.b_blk_f])
        self.memset(G, self.sel2[:], 0.0, [self.b_sel2])
        self.op(G, lambda e: e.affine_select(out=self.sel2[:], in_=self.sel2[:], pattern=[[-1, 2], [0, 128]],
                                              compare_op=ALU.not_equal, fill=1.0, base=0, channel_multiplier=1),
                [self.b_sel2], [self.b_sel2])
        self.dma("sp", self.rw32[:], A["router_w"].rearrange("(k p) e -> p k e", p=128), [], [self.b_rw32])
        self.dma("sp", self.rbias[:], A["router_bias"][0:1, :].partition_broadcast(128), [], [self.b_rbias])
        bx = self.dbuf["xres"]
        self.dma("sp", A["xres"][0:LC, :], A["ctx"][:, :], [], [bx])
        self.dma("sp", A["xres"][LC:NTOK, :], A["x"][:, :], [], [bx])
        zt = self.sb(es, "zpad", [128, 8, 2], BF16); bz = Buf()
        self.memset(G, zt[:], 0.0, [bz])
        bh = self.dbuf["hT"]
        self.dma("sp", self.hT3[:, :, 0:1], zt[:, :, 0:1], [bz], [bh], allow_slow_non_contiguous=True)
        self.dma("sp", self.hT3[:, :, 257:259], zt[:, :, 0:2], [bz], [bh], allow_slow_non_contiguous=True)
        self.dma("sp", self.hT3[:, :, NTP - 1:NTP], zt[:, :, 0:1], [bz], [bh], allow_slow_non_contiguous=True)

    def phase_mod(self, l):
        A = self.A
        with ExitStack() as es:
            self.modrow = self.sb(es, "modrow", [2, 6 * D], F32); self.b_modrow = Buf()
            cT = self.sb(es, "cT", [128, 8, 2], F32); b_cT = Buf()
            sT = self.sb(es, "sT", [128, 8, 2], F32); b_sT = Buf()
            brow = self.sb(es, "brow", [1, 6 * D], F32); b_brow = Buf()
            wch = [self.sb(es, "adaw%d" % i, [128, 8, 512], F32) for i in range(2)]
            b_wch = [Buf(), Buf()]
            self.dma("sp", cT[:, :, 0], A["c"].rearrange("o (k p) -> p (o k)", p=128), [], [b_cT],
                     allow_slow_non_contiguous=True)
            self.dma("sp", cT[:, :, 1], A["c_ctx"].rearrange("o (k p) -> p (o k)", p=128), [], [b_cT],
                     allow_slow_non_contiguous=True)
            self.dma("sp", brow[:], A["ada_b"][l:l + 1, :], [], [b_brow])
            self.act(sT[:], cT[:], AF.Silu, [b_cT], [b_sT])
            for j in range(12):
                w, bw = wch[j % 2], b_wch[j % 2]
                self.dma("sp", w[:], A["ada_w"][l, :, j * 512:(j + 1) * 512].rearrange("(k p) n -> p k n", p=128),
                         [], [bw])
                ps, bp = self.psf[j % 2]
                for k in range(8):
                    self.mm(ps[0:2, :], sT[:, k, :], w[:, k, :], k == 0, False, [b_sT, bw], [bp], inc=False)
                self.mm(ps[0:2, :], self.ones_f[0:1, 0:2], brow[0:1, j * 512:(j + 1) * 512], False, True,
                        [self.b_ones_f, b_brow], [bp])
                self.copy("act", self.modrow[0:2, j * 512:(j + 1) * 512], ps[0:2, :], [bp], [self.b_modrow])
            for j in range(48):
                ps, bp = self.psf[2 + j % 2]
                self.tr(ps[:, 0:2], self.modrow[0:2, j * 128:(j + 1) * 128], self.ident_f[0:2, 0:2],
                        [self.b_modrow, self.b_ident_f], [bp])
                self.copy("dve", self.modT[:, j, :], ps[:, 0:2], [bp], [self.b_modT])
            for base in (8, 32):
                self.ts("dve", self.modT[:, base:base + 8, :], self.modT[:, base:base + 8, :], 1.0, None,
                        ALU.add, None, [self.b_modT], [self.b_modT])
            i = 0
            for gi, col0 in ((0, 2 * D), (2, 5 * D)):
                for r_ in range(2):
                    for hh in range(2):
                        ps, bp = self.psf[4 + i % 2]
                        i += 1
                        self.mm(ps[:, :], self.sel2[0:2, r_, :], self.modrow[0:2, col0 + hh * 512:col0 + (hh + 1) * 512],
                                True, True, [self.b_sel2, self.b_modrow], [bp])
                        self.copy("act", self.gbc[:, gi + r_, hh * 512:(hh + 1) * 512], ps[:, :], [bp], [self.b_gbc])
            for i_, n in enumerate(("ln1_g", "ln1_b", "ln2_g", "ln2_b")):
                self.dma("sp", self.lnbc[:, i_, :], A[n][l:l + 1, :].partition_broadcast(128), [], [self.b_lnbc])
            self.barrier()

    def ln_stats(self, xt, bx, st, mv, rs, bs):
        for i in range(2):
            self.op("dve", lambda e, i=i: e.bn_stats(out=st[:, i, :], in_=xt[:, i * 512:(i + 1) * 512]), [bx], [bs])
        self.op("dve", lambda e: e.bn_aggr(out=mv[:], in_=st[:].rearrange("p a b -> p (a b)")), [bs], [bs])
        self.act(rs[:], mv[:, 1:2], AF.Sqrt, [bs], [bs], bias=LN_EPS, scale=1.0)
        self.op("dve", lambda e: e.reciprocal(out=rs[:], in_=rs[:]), [bs], [bs])

    def phase_h(self, l, tiles):
        A = self.A
        bxres = self.dbuf["xres"]
        bhT = self.dbuf["hT"]
        with ExitStack() as es:
            NB = 3
            xt = [self.sb(es, "h_x%d" % i, [128, D], F32) for i in range(NB)]
            bxt = [Buf() for _ in range(NB)]
            xn = [self.sb(es, "h_xn%d" % i, [128, D], BF16) for i in range(NB)]
            bxn = [Buf() for _ in range(NB)]
            st = [self.sb(es, "h_st%d" % i, [128, 2, 6], F32) for i in range(NB)]
            mv = [self.sb(es, "h_mv%d" % i, [128, 2], F32) for i in range(NB)]
            rs = [self.sb(es, "h_rs%d" % i, [128, 1], F32) for i in range(NB)]
            bs = [Buf() for _ in range(NB)]
            ho = [self.sb(es, "h_o%d" % i, [128, 8, 128], BF16) for i in range(NB)]
            bho = [Buf() for _ in range(NB)]
            def tile_gen(n, ti):
                s = n % NB
                r_ = 1 if ti < 2 else 0
                col = tok_col(ti * 128)
                self.dma("sp", xt[s][:], A["xres"][ti * 128:(ti + 1) * 128, :], [bxres], [bxt[s]])
                yield
                self.ln_stats(xt[s], bxt[s], st[s], mv[s], rs[s], bs[s])
                yield
                self.ts("dve", xn[s][:], xt[s][:], mv[s][:, 0:1], rs[s][:, 0:1], ALU.subtract, ALU.mult,
                        [bxt[s], bs[s]], [bxn[s]])
                yield True
                pt, bpt = self.psb[n % 2]
                for k in range(8):
                    self.tr(pt[:, k * 128:(k + 1) * 128], xn[s][:, k * 128:(k + 1) * 128], self.ident_b[:],
                            [bxn[s], self.b_ident_b], [bpt], inc=(k == 7))
                yield
                for k in range(8):
                    self.act(ho[s][:, k, :], pt[:, k * 128:(k + 1) * 128], AF.Identity, [bpt, self.b_modT], [bho[s]],
                             bias=self.modT[:, k, r_:r_ + 1], scale=self.modT[:, 8 + k, r_:r_ + 1])
                yield
                self.dma("sp", self.hT3[:, :, col:col + 128], ho[s][:], [bho[s]], [bhT])
                yield

            run_pipeline((tile_gen(n, ti) for n, ti in enumerate(tiles)), depth=3)
            self.barrier()


def const_tables():
    f32 = np.float32
    t = np.arange(L)
    rows = (t // 64).astype(f32)
    cols = (t % 64).astype(f32)
    inv = (np.float32(10000.0) ** (-np.arange(0, 32, 2, dtype=f32) / np.float32(32))).astype(f32)
    ang = np.concatenate([rows[:, None] * inv, cols[:, None] * inv], -1).astype(f32)
    rope_cs = np.concatenate([np.cos(ang), np.sin(ang)], -1).astype(f32)

    def ztab(Ls):
        tt = np.linspace(0.0, 1.0, Ls, dtype=f32)
        wpos = (f32(2.0 * math.pi) * np.arange(Ls, dtype=f32) / f32(Ls)).astype(f32)
        f = np.linspace(1e-4, 15, 16, dtype=f32)[None, :]
        z = np.concatenate([tt[:, None], np.cos(f * wpos[:, None]), -np.sin(f * wpos[:, None])], -1).astype(f32)
        pos = np.abs(np.arange(2 * Ls) - Ls)
        pos = np.minimum(pos, Ls - 1)
        return np.ascontiguousarray(z[pos].T), np.ascontiguousarray(tt[pos][None, :])
    hy_z, hy_t = ztab(L)
    hy_zc, hy_tc = ztab(LC)
    max_decay = math.log(1e-2) / 0.3
    min_decay = math.log(1e-2) / 1.5
    delta = np.abs(np.linspace(min_decay, max_decay, 256, dtype=f32)).astype(f32)[:, None]
    return dict(rope_cs=rope_cs, hy_z=hy_z, hy_t=hy_t, hy_zc=hy_zc, hy_tc=hy_tc, hy_delta=delta)


def make_in_maps(inputs, cores):
    f32 = np.float32
    shared = {}
    for k, v in inputs.items():
        if k in ("x", "c", "ctx"):
            continue
        v = np.ascontiguousarray(np.asarray(v, dtype=f32))
        if k in ("c_ctx", "router_bias"):
            v = v.reshape(1, -1)
        shared[k] = v
    shared.update(const_tables())
    maps = []
    for b in cores:
        m = dict(shared)
        m["x"] = np.ascontiguousarray(np.asarray(inputs["x"][b], dtype=f32))
        m["c"] = np.ascontiguousarray(np.asarray(inputs["c"][b], dtype=f32)).reshape(1, -1)
        m["ctx"] = np.ascontiguousarray(np.asarray(inputs["ctx"][b], dtype=f32))
        maps.append(m)
    return maps


def run_pipeline(gens, depth=2):
    it = iter(gens)
    active = []
    ready = True
    done = False
    while True:
        if ready and not done and len(active) < depth:
            try:
                active.append(next(it))
                ready = False
            except StopIteration:
                done = True
        if not active:
            if done:
                break
            ready = True
            continue
        for g_ in list(active):
            try:
                v = next(g_)
                if v is True and g_ is active[-1]:
                    ready = True
            except StopIteration:
                if g_ is active[-1]:
                    ready = True
                active.remove(g_)


def _seqs(ctx_out):
    s = [(LAT0, LC, L)]
    if ctx_out:
        s = [(CTX0, 0, LC)] + s
    return s


def load_hT(self, es):
    hT = self.sb(es, "hT_sb", [128, 8, NTP], BF16)
    b = Buf("hT_sb")
    for k in range(8):
        self.dma("sp", hT[:, k, :], self.hT3[:, k, :], [self.dbuf["hT"]], [b])
    return hT, b


def load_w_in(self, es, l, name, col0, ncols):
    w = self.sb(es, name, [128, 8, ncols], BF16)
    b = Buf(name)
    src = self.A["w_in"][l, :, col0:col0 + ncols].rearrange("(k p) n -> p k n", p=128)
    step = 1024
    for k in range(8):
        for c0 in range(0, ncols, step):
            c1 = min(ncols, c0 + step)
            self.dma("pool", w[:, k, c0:c1], src[:, k, c0:c1], [], [b])
    return w, b


K.load_hT = load_hT
K.load_w_in = load_w_in


def phase_sconv(self, l, ctx_out):
    A = self.A
    with ExitStack() as es:
        hT, bhT = self.load_hT(es)
        w, bw = self.load_w_in(es, l, "sc_w", OFF_SCONV, 768)
        taps = self.sb(es, "sc_taps", [128, 2, 3], F32); btaps = Buf()
        for cc in range(2):
            self.dma("sp", taps[:, cc, :], A["sconv_w"][l, :, cc * 128:(cc + 1) * 128].rearrange("j p -> p j"),
                     [], [btaps], allow_slow_non_contiguous=True)
        m = self.sb(es, "sc_m", [128, L + 2], F32); bm = Buf()
        bg = self.sb(es, "sc_bg", [128, L], F32); bbg = Buf()
        o = self.sb(es, "sc_o", [128, L], F32); bo = Buf()
        res = self.sb(es, "sc_res", [128, L], BF16); bres = Buf()
        cg = [self.sb(es, "sc_cg%d" % i, [128, 512], F32) for i in range(2)]
        bcg = [Buf(), Buf()]
        it = 0
        for cc in range(2):
            for (col0, tok0, n) in _seqs(ctx_out):
                self.memset("pool", m[:, 0:1], 0.0, [bm])
                self.memset("pool", m[:, n + 1:n + 2], 0.0, [bm])
                for t0 in range(0, n, 512):
                    nn = min(512, n - t0)
                    pss = []
                    for pi, cb in enumerate((256, 512, 0)):
                        ps, bp = self.psf[(it * 3 + pi) % 6]
                        for k in range(8):
                            self.mm(ps[:, 0:nn], w[:, k, cb + cc * 128:cb + (cc + 1) * 128],
                                    hT[:, k, col0 + t0:col0 + t0 + nn], k == 0, k == 7, [bw, bhT], [bp])
                        pss.append((ps, bp))
                    c_, bc_ = cg[it % 2], bcg[it % 2]
                    self.copy("act", c_[:, 0:nn], pss[0][0][:, 0:nn], [pss[0][1]], [bc_])
                    self.tt("dve", m[:, 1 + t0:1 + t0 + nn], pss[1][0][:, 0:nn], c_[:, 0:nn], ALU.mult,
                            [pss[1][1], bc_], [bm])
                    self.copy("act", bg[:, t0:t0 + nn], pss[2][0][:, 0:nn], [pss[2][1]], [bbg])
                    it += 1
                self.ts("dve", o[:, 0:n], m[:, 0:n], taps[:, cc, 0:1], None, ALU.mult, None, [bm, btaps], [bo])
                self.stt(o[:, 0:n], m[:, 1:n + 1], taps[:, cc, 1:2], o[:, 0:n], ALU.mult, ALU.add, [bm, btaps, bo], [bo])
                self.stt(o[:, 0:n], m[:, 2:n + 2], taps[:, cc, 2:3], o[:, 0:n], ALU.mult, ALU.add, [bm, btaps, bo], [bo])
                self.tt("dve", res[:, 0:n], o[:, 0:n], bg[:, 0:n], ALU.mult, [bo, bbg], [bres])
                self.dma("sp", self.ysT3[:, 4 + cc, tok0:tok0 + n], res[:, 0:n], [bres], [self.dbuf["ysT"]])
        self.barrier()


K.phase_sconv = phase_sconv


def merge_load(self, es, l):
    A = self.A
    wg, bwg = self.load_w_in(es, l, "mg_wg", OFF_GATE, 4096)
    wb = self.sb(es, "mg_wb", [128, 8, D], BF16); bwb = Buf()
    wo = self.sb(es, "mg_wo", [128, 8, D], BF16); bwo = Buf()
    for n4 in range(4):
        for j in range(2):
            self.dma("pool", wb[:, 2 * n4 + j, :], A["w_branch"][l, n4, j * 128:(j + 1) * 128, :], [], [bwb])
    for k in range(8):
        self.dma("pool", wo[:, k, :], A["w_out"][l, k * 128:(k + 1) * 128, :], [], [bwo])
    return wg, bwg, wb, bwb, wo, bwo


K.merge_load = merge_load


def phase_merge(self, l, tiles, pre=None):
    A = self.A
    with ExitStack() as es:
        if pre is None:
            pre = self.merge_load(es, l)
        wg, bwg, wb, bwb, wo, bwo = pre
        NB = 2
        ys = [self.sb(es, "mg_ys%d" % i, [128, 8, 128], BF16) for i in range(NB)]; bys = [Buf() for _ in range(NB)]
        ht = [self.sb(es, "mg_ht%d" % i, [128, 8, 128], BF16) for i in range(NB)]; bht = [Buf() for _ in range(NB)]
        xt = [self.sb(es, "mg_x%d" % i, [128, D], F32) for i in range(NB)]; bxt = [Buf() for _ in range(NB)]
        gs = [self.sb(es, "mg_gs%d" % i, [128, D], F32) for i in range(2)]; bgs = [Buf(), Buf()]
        mg = self.sb(es, "mg_m", [128, D], F32); bmg = Buf()
        mgb = self.sb(es, "mg_mb", [128, D], BF16); bmgb = Buf()
        mT = self.sb(es, "mg_mT", [128, 8, 128], BF16); bmT = Buf()
        t1 = self.sb(es, "mg_t1", [128, D], F32); bt1 = Buf()
        x1 = self.sb(es, "mg_x1", [128, D], F32); bx1 = Buf()
        xn = self.sb(es, "mg_xn", [128, D], F32); bxn = Buf()
        hf32 = self.sb(es, "mg_hf32", [128, 8, 128], F32); bhf32 = Buf()
        hfb = self.sb(es, "mg_hfb", [128, 8, 128], BF16); bhfb = Buf()
        st = self.sb(es, "mg_st", [128, 2, 6], F32); mv = self.sb(es, "mg_mv", [128, 2], F32)
        rs = self.sb(es, "mg_rs", [128, 1], F32); bs = Buf()
        rt = self.sb(es, "mg_rt", [128, 8, NE], F32); brt = Buf()
        bxres = self.dbuf["xres"]
        def tile_gen(n, ti):
            s = n % NB
            r_ = 1 if ti < 2 else 0
            col = tok_col(ti * 128)
            tk = ti * 128
            self.dma("sp", ys[s][:], self.ysT3[:, :, tk:tk + 128], [self.dbuf["ysT"]], [bys[s]])
            self.dma("sp", ht[s][:], self.hT3[:, :, col:col + 128], [self.dbuf["hT"]], [bht[s]])
            self.dma("sp", xt[s][:], A["xres"][tk:tk + 128, :], [bxres], [bxt[s]])
            yield
            for n4 in range(4):
                g_, bg_ = gs[n4 % 2], bgs[n4 % 2]
                for hh in range(2):
                    pg, bpg = self.psf[hh]
                    for k in range(8):
                        self.mm(pg[:, :], ht[s][:, k, :], wg[:, k, n4 * 1024 + hh * 512:n4 * 1024 + (hh + 1) * 512],
                                k == 0, k == 7, [bht[s], bwg], [bpg])
                    self.act(g_[:, hh * 512:(hh + 1) * 512], pg[:, :], AF.Sigmoid, [bpg], [bg_])
                for hh in range(2):
                    pz, bpz = self.psf[2 + hh]
                    for j in range(2):
                        self.mm(pz[:, :], ys[s][:, 2 * n4 + j, :], wb[:, 2 * n4 + j, hh * 512:(hh + 1) * 512],
                                j == 0, j == 1, [bys[s], bwb], [bpz])
                    sl = slice(hh * 512, (hh + 1) * 512)
                    if n4 == 0:
                        self.tt("dve", mg[:, sl], pz[:, :], g_[:, sl], ALU.mult, [bpz, bg_], [bmg])
                    else:
                        self.tt("dve", g_[:, sl], pz[:, :], g_[:, sl], ALU.mult, [bpz, bg_], [bg_])
                        if n4 < 3:
                            self.tt("pool", mg[:, sl], mg[:, sl], g_[:, sl], ALU.add, [bmg, bg_], [bmg])
                        else:
                            self.tt("pool", mgb[:, sl], mg[:, sl], g_[:, sl], ALU.add, [bmg, bg_], [bmgb])
                yield
            pt, bpt = self.psb[0]
            for k in range(8):
                self.tr(pt[:, k * 128:(k + 1) * 128], mgb[:, k * 128:(k + 1) * 128], self.ident_b[:],
                        [bmgb, self.b_ident_b], [bpt], inc=(k == 7))
            yield True
            self.copy("dve", mT[:].rearrange("p a b -> p (a b)"), pt[:, :], [bpt], [bmT])
            yield
            for hh in range(2):
                py, bpy = self.psf[4 + hh]
                sl = slice(hh * 512, (hh + 1) * 512)
                for k in range(8):
                    self.mm(py[:, :], mT[:, k, :], wo[:, k, sl], k == 0, k == 7, [bmT, bwo], [bpy])
                self.tt("dve", t1[:, sl], py[:, :], self.gbc[:, 0 + r_, sl], ALU.mult, [bpy, self.b_gbc], [bt1])
            yield
            self.stt(t1[:], xt[s][:], ALPHA, t1[:], ALU.mult, ALU.add, [bxt[s], bt1], [bt1])
            self.ln_stats(t1, bt1, st, mv, rs, bs)
            yield
            self.ts("dve", xn[:], t1[:], mv[:, 0:1], rs[:, 0:1], ALU.subtract, ALU.mult, [bt1, bs], [bxn])
            self.tt("pool", xn[:], xn[:], self.lnbc[:, 0, :], ALU.mult, [bxn, self.b_lnbc], [bxn])
            self.tt("pool", x1[:], xn[:], self.lnbc[:, 1, :], ALU.add, [bxn, self.b_lnbc], [bx1])
            self.dma("sp", A["xres"][tk:tk + 128, :], x1[:], [bx1], [bxres])
            yield
            self.ln_stats(x1, bx1, st, mv, rs, bs)
            self.ts("dve", xn[:], x1[:], mv[:, 0:1], rs[:, 0:1], ALU.subtract, ALU.mult, [bx1, bs], [bxn])
            yield
            for half in range(2):
                pf, bpf = self.psf[4 + half]
                for kk in range(4):
                    k = half * 4 + kk
                    self.tr(pf[:, kk * 128:(kk + 1) * 128], xn[:, k * 128:(k + 1) * 128], self.ident_f[:],
                            [bxn, self.b_ident_f], [bpf], inc=(kk == 3))
                for kk in range(4):
                    k = half * 4 + kk
                    self.act(hf32[:, k, :], pf[:, kk * 128:(kk + 1) * 128], AF.Identity, [bpf, self.b_modT], [bhf32],
                             bias=self.modT[:, 24 + k, r_:r_ + 1], scale=self.modT[:, 32 + k, r_:r_ + 1])
            yield
            self.copy("dve", hfb[:], hf32[:], [bhf32], [bhfb])
            self.dma("sp", self.hfT3[:, :, tk:tk + 128], hfb[:], [bhfb], [self.dbuf["hfT"]])
            pr, bpr = self.psb[1][0][:].bitcast(F32), self.psb[1][1]
            for k in range(8):
                self.mm(pr[:, 0:NE], hf32[:, k, :], self.rw32[:, k, :], k == 0, k == 7, [bhf32, self.b_rw32], [bpr])
            s_ = rt[:, 0, :]; sel = rt[:, 1, :]; tmp = rt[:, 2, :]; sel2_ = rt[:, 3, :]; msk = rt[:, 4, :]
            m1 = rt[:, 5, 0:4]; m2 = rt[:, 5, 4:8]; grp = rt[:, 5, 8:12]; gmx = rt[:, 5, 12:13]; oh = rt[:, 6, 0:4]
            den = rt[:, 6, 4:5]
            B = [brt]
            v3 = lambda a: a.rearrange("p (g e) -> p g e", g=4)
            b4 = lambda a: a.unsqueeze(2).to_broadcast([128, 4, 4])
            yield
            self.act(s_, pr[:, 0:NE], AF.Sigmoid, [bpr], B)
            self.tt("dve", sel, s_, self.rbias[:], ALU.add, B + [self.b_rbias], B)
            self.op("dve", lambda e: e.tensor_reduce(out=m1, in_=v3(sel), axis=AX.X, op=ALU.max), B, B)
            self.tt("dve", v3(tmp), v3(sel), b4(m1), ALU.is_equal, B, B)
            self.stt(sel2_, tmp, -1e30, sel, ALU.mult, ALU.add, B, B)
            self.op("dve", lambda e: e.tensor_reduce(out=m2, in_=v3(sel2_), axis=AX.X, op=ALU.max), B, B)
            self.tt("dve", grp, m1, m2, ALU.add, B, B)
            self.op("dve", lambda e: e.tensor_reduce(out=gmx, in_=grp, axis=AX.X, op=ALU.max), B, B)
            self.ts("dve", oh, grp, gmx, None, ALU.is_equal, None, B, B)
            self.tt("dve", v3(msk), v3(sel), b4(m2), ALU.is_ge, B, B)
            self.tt("dve", v3(msk), v3(msk), b4(oh), ALU.mult, B, B)
            self.tt("dve", tmp, msk, s_, ALU.mult, B, B)
            self.op("dve", lambda e: e.tensor_reduce(out=den, in_=tmp, axis=AX.X, op=ALU.add), B, B)
            self.op("dve", lambda e: e.reciprocal(out=den, in_=den), B, B)
            self.ts("dve", self.gates[:, ti, :], tmp, den, None, ALU.mult, None, B, [self.b_gates])
            yield

        run_pipeline((tile_gen(n, ti) for n, ti in enumerate(tiles)), depth=2)
        self.barrier()


K.phase_merge = phase_merge


def phase_moe(self, l, tiles, last):
    A = self.A
    nparts = 3
    per = (len(tiles) + nparts - 1) // nparts
    parts = [tiles[i:i + per] for i in range(0, len(tiles), per)]
    bxres = self.dbuf["xres"]
    with ExitStack() as es:
        hf = self.sb(es, "moe_hf", [128, 8, per * 128], BF16); bhf = Buf()
        acc = self.sb(es, "moe_acc", [128, per, D], F32); bacc = Buf()
        w1s = [self.sb(es, "moe_w1_%d" % i, [128, 8, DE], BF16) for i in range(2)]
        w3s = [self.sb(es, "moe_w3_%d" % i, [128, 8, DE], BF16) for i in range(2)]
        w2s = [self.sb(es, "moe_w2_%d" % i, [128, 4, D], BF16) for i in range(2)]
        bw = [Buf(), Buf()]
        sa = [self.sb(es, "moe_sa%d" % i, [128, 512], F32) for i in range(2)]; bsa = [Buf(), Buf()]
        aT = [self.sb(es, "moe_aT%d" % i, [128, 4, 512], BF16) for i in range(2)]; baT = [Buf(), Buf()]
        xt = [self.sb(es, "moe_x%d" % i, [128, D], F32) for i in range(2)]; bxt = [Buf(), Buf()]
        t1 = self.sb(es, "moe_t1", [128, D], F32); bt1 = Buf()
        xo = [self.sb(es, "moe_xo%d" % i, [128, D], F32) for i in range(2)]; bxo = [Buf(), Buf()]
        st = self.sb(es, "moe_st", [128, 2, 6], F32); mv = self.sb(es, "moe_mv", [128, 2], F32)
        rs = self.sb(es, "moe_rs", [128, 1], F32); bs = Buf()
        wi = 0
        ci = 0
        oi = 0
        pobanks = [self.psf[4], self.psf[5], (self.psb[0][0][:].bitcast(F32), self.psb[0][1]),
                   (self.psb[1][0][:].bitcast(F32), self.psb[1][1])]
        for part in parts:
            nt = len(part)
            tok0 = part[0] * 128
            ntok = nt * 128
            for k in range(8):
                self.dma("sp", hf[:, k, 0:ntok], self.hfT3[:, k, tok0:tok0 + ntok], [self.dbuf["hfT"]], [bhf])
            for e_ in range(NE):
                sl_ = wi % 2
                wi += 1
                self.dma("sp", w1s[sl_][:], A["ewb1"][e_ * D:(e_ + 1) * D, :].rearrange("(k p) n -> p k n", p=128),
                         [self.dbuf["ewb1"]], [bw[sl_]])
                self.dma("sp", w3s[sl_][:], A["ewb3"][e_ * D:(e_ + 1) * D, :].rearrange("(k p) n -> p k n", p=128),
                         [self.dbuf["ewb3"]], [bw[sl_]])
                self.dma("sp", w2s[sl_][:], A["ewb2"][e_ * DE:(e_ + 1) * DE, :].rearrange("(k p) n -> p k n", p=128),
                         [self.dbuf["ewb2"]], [bw[sl_]])
                for t0 in range(0, ntok, 512):
                    nn = min(512, ntok - t0)
                    a_, ba_ = aT[ci % 2], baT[ci % 2]
                    ci += 1
                    for f in range(4):
                        pa, bpa = self.psf[(2 * f) % 4]
                        pb, bpb = self.psf[(2 * f + 1) % 4]
                        for k in range(8):
                            self.mm(pa[:, 0:nn], w1s[sl_][:, k, f * 128:(f + 1) * 128], hf[:, k, t0:t0 + nn],
                                    k == 0, k == 7, [bw[sl_], bhf], [bpa])
                        for k in range(8):
                            self.mm(pb[:, 0:nn], w3s[sl_][:, k, f * 128:(f + 1) * 128], hf[:, k, t0:t0 + nn],
                                    k == 0, k == 7, [bw[sl_], bhf], [bpb])
                        s_, bs_ = sa[f % 2], bsa[f % 2]
                        self.act(s_[:, 0:nn], pa[:, 0:nn], AF.Silu, [bpa], [bs_])
                        self.tt("dve", a_[:, f, 0:nn], pb[:, 0:nn], s_[:, 0:nn], ALU.mult, [bpb, bs_], [ba_])
                    for tt_ in range(nn // 128):
                        j = (t0 + tt_ * 128) // 128
                        ti = part[j]
                        for hh in range(2):
                            oi += 1
                            po, bpo = pobanks[oi % 4]
                            sl = slice(hh * 512, (hh + 1) * 512)
                            for f in range(4):
                                self.mm(po[:, :], a_[:, f, tt_ * 128:(tt_ + 1) * 128], w2s[sl_][:, f, sl],
                                        f == 0, f == 3, [ba_, bw[sl_]], [bpo])
                            gcol = self.gates[:, ti, e_:e_ + 1]
                            if e_ == 0:
                                self.ts("dve", acc[:, j, sl], po[:, :], gcol, None, ALU.mult, None,
                                        [bpo, self.b_gates], [bacc])
                            else:
                                self.stt(acc[:, j, sl], po[:, :], gcol, acc[:, j, sl], ALU.mult, ALU.add,
                                         [bpo, self.b_gates, bacc], [bacc])
            for j, ti in enumerate(part):
                s = j % 2
                r_ = 1 if ti < 2 else 0
                tk = ti * 128
                self.dma("sp", xt[s][:], A["xres"][tk:tk + 128, :], [bxres], [bxt[s]])
                self.tt("pool", t1[:], acc[:, j, :], self.gbc[:, 2 + r_, :], ALU.mult, [bacc, self.b_gbc], [bt1])
                self.stt(t1[:], xt[s][:], ALPHA, t1[:], ALU.mult, ALU.add, [bxt[s], bt1], [bt1])
                self.ln_stats(t1, bt1, st, mv, rs, bs)
                self.ts("dve", t1[:], t1[:], mv[:, 0:1], rs[:, 0:1], ALU.subtract, ALU.mult, [bt1, bs], [bt1])
                self.tt("pool", t1[:], t1[:], self.lnbc[:, 2, :], ALU.mult, [bt1, self.b_lnbc], [bt1])
                self.tt("pool", xo[s][:], t1[:], self.lnbc[:, 3, :], ALU.add, [bt1, self.b_lnbc], [bxo[s]])
                if last:
                    self.dma("sp", A["out"][tk - LC:tk - LC + 128, :], xo[s][:], [bxo[s]], [self.dbuf["out"]])
                else:
                    self.dma("sp", A["xres"][tk:tk + 128, :], xo[s][:], [bxo[s]], [bxres])
        self.barrier()


K.phase_moe = phase_moe


def precast_experts(self, l):
    A = self.A
    for e_ in range(NE):
        self.dma("pool", A["ewb1"][e_ * D:(e_ + 1) * D, :], A["exp_w1"][l, e_, :, :], [], [self.dbuf["ewb1"]])
        self.dma("pool", A["ewb3"][e_ * D:(e_ + 1) * D, :], A["exp_w3"][l, e_, :, :], [], [self.dbuf["ewb3"]])
        self.dma("pool", A["ewb2"][e_ * DE:(e_ + 1) * DE, :], A["exp_w2"][l, e_, :, :], [], [self.dbuf["ewb2"]])


K.precast_experts = precast_experts


def phase_attn(self, l, ctx_out, embed_hyfilt=True):
    A = self.A
    with ExitStack() as es:
        w, bw = self.load_w_in(es, l, "at_w", OFF_ATTN, 512)
        hts = [self.sb(es, "at_ht%d" % i, [128, 8, 128], BF16) for i in range(3)]
        bhts = [Buf() for _ in range(3)]
        qT = self.sb(es, "at_qT", [64, 4, NTOK], BF16); bqT = Buf()
        kT = self.sb(es, "at_kT", [64, 2, NTOK], BF16); bkT = Buf()
        va = self.sb(es, "at_va", [128, NTILE, 2, 66], BF16); bva = Buf()
        gqk = self.sb(es, "at_g", [128, 6, 64], F32); bg = Buf()
        cs = self.sb(es, "at_cs", [128, 32, 64], F32); bcs = Buf()
        for h in range(6):
            src = A["attn_q_norm"] if h < 4 else A["attn_k_norm"]
            self.dma("sp", gqk[:, h, :], src[l:l + 1, :].partition_broadcast(128), [], [bg])
        self.dma("sp", cs[:], A["rope_cs"].rearrange("(j p) c -> p j c", p=128), [], [bcs])
        self.memset("pool", va[:, :, :, 64:66], 1.0, [bva])
        NB = 2
        sq = [self.sb(es, "at_sq%d" % i, [128, 6, 64], F32) for i in range(NB)]
        qn = [self.sb(es, "at_qn%d" % i, [128, 6, 64], F32) for i in range(NB)]
        t_a = [self.sb(es, "at_ta%d" % i, [128, 6, 32], F32) for i in range(NB)]
        t_b = [self.sb(es, "at_tb%d" % i, [128, 6, 32], F32) for i in range(NB)]
        qr = [self.sb(es, "at_qr%d" % i, [128, 6, 64], BF16) for i in range(NB)]
        ss = [self.sb(es, "at_ss%d" % i, [128, 8], F32) for i in range(NB)]
        bt = [Buf() for _ in range(NB)]
        bqr = [Buf() for _ in range(NB)]
        tiles = list(range(NTILE))

        def tile_gen(n, ti):
            s = n % NB
            col = tok_col(ti * 128)
            tk = ti * 128
            ps, bp = self.psf[n % 2]
            ht_, bht_ = hts[n % 3], bhts[n % 3]
            self.dma("sp", ht_[:], self.hT3[:, :, col:col + 128], [self.dbuf["hT"]], [bht_])
            for k in range(8):
                self.mm(ps[:, :], ht_[:, k, :], w[:, k, :], k == 0, k == 7, [bht_, bw], [bp])
            yield
            B = [bt[s]]
            p3 = ps[:, 0:384].rearrange("p (h d) -> p h d", h=6)
            self.copy("dve", va[:, ti, :, 0:64], ps[:, 384:512].rearrange("p (g d) -> p g d", g=2), [bp], [bva])
            self.copy("act", qn[s][:].rearrange("p h d -> p (h d)"), ps[:, 0:384], [bp], B)
            self.tt("dve", sq[s][:], qn[s][:], qn[s][:], ALU.mult, B, B)
            yield
            self.op("dve", lambda e, s=s: e.tensor_reduce(out=ss[s][:, 0:6], in_=sq[s][:], axis=AX.X, op=ALU.add), B, B)
            self.act(ss[s][:, 0:6], ss[s][:, 0:6], AF.Sqrt, B, B, bias=1e-6, scale=1.0 / 64.0)
            self.op("dve", lambda e, s=s: e.reciprocal(out=ss[s][:, 0:6], in_=ss[s][:, 0:6]), B, B)
            self.tt("dve", qn[s][:], qn[s][:], ss[s][:, 0:6].unsqueeze(2).to_broadcast([128, 6, 64]), ALU.mult, B, B)
            self.tt("pool", qn[s][:], qn[s][:], gqk[:], ALU.mult, B + [bg], B)
            yield True
            if ti >= 2:
                j = ti - 2
                cosb = cs[:, j, 0:32].unsqueeze(1).to_broadcast([128, 6, 32])
                sinb = cs[:, j, 32:64].unsqueeze(1).to_broadcast([128, 6, 32])
                x1 = qn[s][:, :, 0:64:2]
                x2 = qn[s][:, :, 1:64:2]
                self.tt("dve", t_a[s][:], x1, cosb, ALU.mult, B + [bcs], B)
                self.tt("dve", t_b[s][:], x2, sinb, ALU.mult, B + [bcs], B)
                self.tt("dve", qr[s][:, :, 0:64:2], t_a[s][:], t_b[s][:], ALU.subtract, B + [bqr[s]], [bqr[s]])
                self.tt("dve", t_a[s][:], x1, sinb, ALU.mult, B + [bcs, bqr[s]], B)
                self.tt("dve", t_b[s][:], x2, cosb, ALU.mult, B + [bcs, bqr[s]], B)
                self.tt("dve", qr[s][:, :, 1:64:2], t_a[s][:], t_b[s][:], ALU.add, B + [bqr[s]], [bqr[s]])
            else:
                self.copy("dve", qr[s][:], qn[s][:], B, [bqr[s]])
            yield
            pt, bpt = self.psb[n % 2]
            for h in range(6):
                self.tr(pt[0:64, h * 128:(h + 1) * 128], qr[s][:, h, :], self.ident_b[:], [bqr[s], self.b_ident_b],
                        [bpt], inc=(h == 5))
            yield
            self.copy("act", qT[:, :, tk:tk + 128], pt[0:64, 0:512].rearrange("p (h t) -> p h t", h=4), [bpt], [bqT])
            self.copy("dve", kT[:, :, tk:tk + 128], pt[0:64, 512:768].rearrange("p (h t) -> p h t", h=2), [bpt], [bkT])
            yield

        run_pipeline((tile_gen(n, ti) for n, ti in enumerate(tiles)), depth=2)
        self.precast_experts(l)
        self._hy_gens = []
        if embed_hyfilt:
            self._hy_gens.append(self.phase_hyfilt(l, L, "hy_z", "hy_t", "hyG", es=es))
            if ctx_out:
                self._hy_gens.append(self.phase_hyfilt(l, LC, "hy_zc", "hy_tc", "hyGc", es=es))
        pT = [self.sb(es, "at_pT%d" % i, [128, 512], BF16) for i in range(3)]; bpT = [Buf() for _ in range(3)]
        osb = [self.sb(es, "at_o%d" % i, [65, 512], F32) for i in range(2)]; bosb = [Buf(), Buf()]
        yo = [self.sb(es, "at_y%d" % i, [64, 512], BF16) for i in range(2)]; byo = [Buf(), Buf()]
        qsets = [(LC + c * 512, 512, list(range(NTILE))) for c in range(8)]
        if ctx_out:
            qsets = [(0, LC, [0, 1])] + qsets
        st_ = {'it': 0, 'ei': 0}

        def scores_gen():
            for (q0, nq, kts) in qsets:
                for h in range(4):
                    g = h // 2
                    it = st_['it']
                    po, bpo = self.psf[4 + it % 2]
                    o_, bo_ = osb[it % 2], bosb[it % 2]
                    y_, by_ = yo[it % 2], byo[it % 2]
                    st_['it'] += 1

                    def S(i):
                        kt = kts[i]
                        ps_, bp_ = self.psf[i % 3]
                        self.mm(ps_[:, 0:nq], kT[0:64, g, kt * 128:(kt + 1) * 128], qT[0:64, h, q0:q0 + nq], True, True,
                                [bkT, bqT], [bp_])
                    S(0)
                    for i, kt in enumerate(kts):
                        if i + 1 < len(kts):
                            S(i + 1)
                        ps_, bp_ = self.psf[i % 3]
                        ei = st_['ei']
                        p_, bp2 = pT[ei % 3], bpT[ei % 3]
                        st_['ei'] += 1
                        self.act(p_[:, 0:nq], ps_[:, 0:nq], AF.Exp, [bp_], [bp2], scale=0.125)
                        self.mm(po[0:65, 0:nq], va[:, kt, g, 0:65], p_[:, 0:nq], i == 0, i == len(kts) - 1, [bva, bp2], [bpo])
                        yield
                    self.copy("dve", o_[0:65, 0:nq], po[0:65, 0:nq], [bpo], [bo_])
                    self.op("dve", lambda e, o_=o_, nq=nq: e.reciprocal(out=o_[64:65, 0:nq], in_=o_[64:65, 0:nq]), [bo_], [bo_])
                    pb_, bpb_ = self.psf[3]
                    self.mm(pb_[0:64, 0:nq], self.ones_f[64:65, 0:64], o_[64:65, 0:nq], True, True, [self.b_ones_f, bo_], [bpb_])
                    self.tt("dve", y_[:, 0:nq], o_[0:64, 0:nq], pb_[0:64, 0:nq], ALU.mult, [bo_, bpb_], [by_])
                    r0 = (h % 2) * 64
                    self.dma("sp", self.ysT3[r0:r0 + 64, 6 + h // 2, q0:q0 + nq], y_[:, 0:nq], [by_], [self.dbuf["ysT"]])

        def filt_gen():
            for g_ in self._hy_gens:
                for _ in g_:
                    yield

        gens = [scores_gen()]
        if self._hy_gens:
            gens.append(filt_gen())
        while gens:
            for g_ in list(gens):
                try:
                    next(g_)
                except StopIteration:
                    gens.remove(g_)
        if self._hy_gens:
            self.hyfilt_done = True
        self.barrier()


K.phase_attn = phase_attn


def phase_hyfilt(self, l, Ls, zname, tname, Gname, es=None):
    A = self.A
    CS = min(512, Ls)
    embedded = es is not None
    own = ExitStack()
    if es is None:
        es = own
    if True:
        w1 = self.sb(es, "hf_w1", [33, 64], F32); w2 = self.sb(es, "hf_w2", [64, 64], F32)
        w3 = self.sb(es, "hf_w3", [64, 512], F32); bw = Buf()
        cols = self.sb(es, "hf_cols", [64, 4], F32)
        dl = self.sb(es, "hf_dl", [128, 2], F32)
        self.dma("sp", w1[:], A["hyena_w1"][l, :, :], [], [bw])
        self.dma("sp", w2[:], A["hyena_w2"][l, :, :], [], [bw])
        self.dma("sp", w3[:], A["hyena_w3"][l, :, :], [], [bw])
        for i, nme in enumerate(("hyena_b1", "hyena_freq1", "hyena_b2", "hyena_freq2")):
            self.dma("sp", cols[:, i:i + 1], A[nme][l:l + 1, :].rearrange("o n -> n o"), [], [bw],
                     allow_slow_non_contiguous=True)
        self.dma("sp", dl[:], A["hy_delta"].rearrange("(c p) o -> p (c o)", p=128), [], [bw],
                 allow_slow_non_contiguous=True)
        self.ts("dve", dl[:], dl[:], -1.0, None, ALU.mult, None, [bw], [bw])
        z = [self.sb(es, "hf_z%d" % i, [33, CS], F32) for i in range(2)]; bz = [Buf(), Buf()]
        tb = [self.sb(es, "hf_t%d" % i, [128, CS], F32) for i in range(2)]; btb = [Buf(), Buf()]
        a1s = [self.sb(es, "hf_a1_%d" % i, [64, CS], F32) for i in range(2)]; ba1s = [Buf(), Buf()]
        a2s = [self.sb(es, "hf_a2_%d" % i, [64, CS], F32) for i in range(2)]; ba2s = [Buf(), Buf()]
        tmps = [self.sb(es, "hf_tmp%d" % i, [64, CS], F32) for i in range(2)]
        win = [self.sb(es, "hf_win%d" % i, [128, CS], F32) for i in range(2)]; bwin = [Buf(), Buf()]
        g = [self.sb(es, "hf_g%d" % i, [128, CS], BF16) for i in range(2)]; bg = [Buf(), Buf()]
        Gap = A[Gname]
        gi_ = [0]

        def chunk_gen(ci):
            n0 = ci * CS
            s = ci % 2
            a1, ba1, a2, ba2, tmp = a1s[s], ba1s[s], a2s[s], ba2s[s], tmps[s]
            self.dma("sp", z[s][:], A[zname][:, n0:n0 + CS], [], [bz[s]])
            self.dma("sp", tb[s][:], A[tname][0:1, n0:n0 + CS].partition_broadcast(128), [], [btb[s]])
            yield
            p1, bp1 = self.psb_f32[0] if embedded else self.psf[0 + 3 * s]
            self.mm(p1[0:64, 0:CS], w1[:, :], z[s][:, :], True, True, [bw, bz[s]], [bp1])
            self.ts("dve", a1[:], p1[0:64, 0:CS], cols[:, 0:1], cols[:, 1:2], ALU.add, ALU.mult, [bp1, bw], [ba1])
            yield
            self.range_reduce(a1[:], tmp[:], [ba1], [ba1])
            yield
            self.act(a1[:], a1[:], AF.Sin, [ba1], [ba1])
            yield True
            p2, bp2 = self.psb_f32[1] if embedded else self.psf[1 + 3 * s]
            self.mm(p2[0:64, 0:CS], w2[:, :], a1[:, :], True, True, [bw, ba1], [bp2])
            self.ts("dve", a2[:], p2[0:64, 0:CS], cols[:, 2:3], cols[:, 3:4], ALU.add, ALU.mult, [bp2, bw], [ba2])
            yield
            self.range_reduce(a2[:], tmp[:], [ba2, ba1], [ba2, ba1])
            yield
            self.act(a2[:], a2[:], AF.Sin, [ba2], [ba2])
            yield
            for cc in range(2):
                cb = (256 if n0 < Ls else 0) + cc * 128
                p3, bp3 = self.psb_f32[cc] if embedded else (self.psf[2 + 3 * s] if cc == 0 else self.psb_f32[s])
                self.mm(p3[:, 0:CS], w3[:, cb:cb + 128], a2[:, :], True, True, [bw, ba2], [bp3])
                w_, bw_ = win[gi_[0] % 2], bwin[gi_[0] % 2]
                g_, bg_ = g[gi_[0] % 2], bg[gi_[0] % 2]
                gi_[0] += 1
                self.act(w_[:], tb[s][:], AF.Exp, [btb[s], bw], [bw_], scale=dl[:, cc:cc + 1])
                self.tt("dve", g_[:], p3[:, 0:CS], w_[:], ALU.mult, [bp3, bw_], [bg_])
                self.dma("sp", Gap[cc * 128:(cc + 1) * 128, n0:n0 + CS], g_[:], [bg_], [self.dbuf[Gname]])
                yield

        if embedded:
            def allchunks():
                for ci in range(2 * Ls // CS):
                    for _ in chunk_gen(ci):
                        yield
            return allchunks()
        run_pipeline((chunk_gen(ci) for ci in range(2 * Ls // CS)), depth=2)
        self.barrier()
        own.close()


K.phase_hyfilt = phase_hyfilt


def phase_hyena(self, l, ctx_out):
    A = self.A
    seqs = [(LAT0, LC, L, "hyG")]
    if ctx_out:
        seqs = [(CTX0, 0, LC, "hyGc")] + seqs
    if not getattr(self, "hyfilt_done", False):
        self.phase_hyfilt(l, L, "hy_z", "hy_t", "hyG")
        if ctx_out:
            self.phase_hyfilt(l, LC, "hy_zc", "hy_tc", "hyGc")
    self.hyfilt_done = False
    with ExitStack() as es:
        w, bw = self.load_w_in(es, l, "hy_w", OFF_HYENA, 768)
        taps = self.sb(es, "hy_taps", [128, 6, 3], F32); btaps = Buf()
        for c6 in range(6):
            self.dma("sp", taps[:, c6, :], A["hyena_conv"][l, :, c6 * 128:(c6 + 1) * 128].rearrange("j p -> p j"),
                     [], [btaps], allow_slow_non_contiguous=True)
        skip = self.sb(es, "hy_skip", [128, 2], F32)
        self.dma("sp", skip[:], A["hyena_skip"][l:l + 1, :].rearrange("o (c p) -> p (o c)", p=128), [], [btaps],
                 allow_slow_non_contiguous=True)
        hch = [self.sb(es, "hy_h%d" % i, [128, 8, 512], BF16) for i in range(2)]; bhch = [Buf(), Buf()]
        praw = self.sb(es, "hy_praw", [128, L + 2], F32); bpraw = Buf()
        tmp = self.sb(es, "hy_tmp", [128, L], F32); btmp = Buf()
        u = self.sb(es, "hy_u", [128, L], F32); bu = Buf()
        x0c = self.sb(es, "hy_x0", [128, L], F32); bx0 = Buf()
        ub = self.sb(es, "hy_ub", [128, L], BF16); bub = Buf()
        utok = self.sb(es, "hy_utok", [128, 128, L // 128], BF16); butok = Buf()
        ut = self.sb(es, "hy_ut", [128, 512], BF16); but = Buf()
        S = [self.sb(es, "hy_S%d" % i, [128, 63 * 128], BF16) for i in range(2)]; bS = [Buf(), Buf()]
        hi = 0
        si = 0
        for (col0, tok0, n, Gname) in seqs:
            nb = n // 128
            W = (2 * nb - 1) * 128
            Gt = self.dram[Gname]
            ytok = praw[:, 0:n].rearrange("p (a c) -> p a c", c=128)
            for cc in range(2):
                for part, cb in (("x1", 256), ("v", 512), ("x0", 0)):
                    c6 = cb // 128 + cc
                    self.memset("pool", praw[:, 0:1], 0.0, [bpraw])
                    self.memset("pool", praw[:, n + 1:n + 2], 0.0, [bpraw])
                    for t0 in range(0, n, 512):
                        nn = min(512, n - t0)
                        h_, bh_ = hch[hi % 2], bhch[hi % 2]
                        hi += 1
                        self.dma("sp", h_[:, :, 0:nn], self.hT3[:, :, col0 + t0:col0 + t0 + nn], [self.dbuf["hT"]], [bh_])
                        ps, bp = self.psf[2 + hi % 2]
                        for k in range(8):
                            self.mm(ps[:, 0:nn], w[:, k, cb + cc * 128:cb + (cc + 1) * 128], h_[:, k, 0:nn],
                                    k == 0, k == 7, [bw, bh_], [bp])
                        self.copy("act", praw[:, 1 + t0:1 + t0 + nn], ps[:, 0:nn], [bp], [bpraw])
                    dst, bdst = {"x1": (u, bu), "v": (tmp, btmp), "x0": (x0c, bx0)}[part]
                    self.ts("dve", dst[:, 0:n], praw[:, 0:n], taps[:, c6, 0:1], None, ALU.mult, None, [bpraw, btaps], [bdst])
                    self.stt(dst[:, 0:n], praw[:, 1:n + 1], taps[:, c6, 1:2], dst[:, 0:n], ALU.mult, ALU.add,
                             [bpraw, btaps, bdst], [bdst])
                    self.stt(dst[:, 0:n], praw[:, 2:n + 2], taps[:, c6, 2:3], dst[:, 0:n], ALU.mult, ALU.add,
                             [bpraw, btaps, bdst], [bdst])
                    if part == "v":
                        self.tt("dve", u[:, 0:n], u[:, 0:n], tmp[:, 0:n], ALU.mult, [bu, btmp], [bu])
                        self.copy("act", ub[:, 0:n], u[:, 0:n], [bu], [bub])
                for b0 in range(0, nb, 4):
                    nbb = min(4, nb - b0)
                    pt, bpt = self.psb[(b0 // 4) % 2]
                    for bb in range(nbb):
                        b_ = b0 + bb
                        self.tr(pt[:, bb * 128:(bb + 1) * 128], ub[:, b_ * 128:(b_ + 1) * 128], self.ident_b[:],
                                [bub, self.b_ident_b], [bpt], inc=(bb == nbb - 1))
                    self.copy("dve", ut[:, 0:nbb * 128], pt[:, 0:nbb * 128], [bpt], [but])
                    pf, bpf = self.psf[4 + (b0 // 4) % 2]
                    self.mm(pf[:, 0:nbb * 128], self.flip_b[:], ut[:, 0:nbb * 128], True, True, [self.b_flip_b, but], [bpf])
                    self.copy("act", utok[:, :, b0:b0 + nbb].rearrange("p c b -> p b c"),
                              pf[:, 0:nbb * 128].rearrange("p (b c) -> p b c", c=128), [bpf], [butok])
                for c0 in range(0, 128, 16):
                    py, bpy = self.psf[(c0 // 16) % 2]
                    for cl in range(16):
                        c = c0 + cl
                        cg = cc * 128 + c
                        S_, bS_ = S[si % 2], bS[si % 2]
                        si += 1
                        src = bass.AP(Gt, cg * 2 * n + 1, [[1, 128], [1, W]])
                        self.dma("sp", S_[:, 0:W], src, [self.dbuf[Gname]], [bS_])
                        ds = [0] + [d for d in range(-(nb - 1), nb) if d != 0]
                        for ii, d in enumerate(ds):
                            a0 = max(0, d)
                            a1_ = min(nb - 1, nb - 1 + d)
                            off = (nb - 1 + d) * 128
                            self.mm(py[:, cl * nb + a0:cl * nb + a1_ + 1], S_[:, off:off + 128],
                                    utok[:, c, a0 - d:a1_ - d + 1], ii == 0, ii == len(ds) - 1, [bS_, butok], [bpy])
                    eng = "dve" if (c0 // 16) % 2 == 0 else "act"
                    self.copy(eng, ytok[:, :, c0:c0 + 16], py[:, 0:16 * nb].rearrange("p (c a) -> p a c", a=nb),
                              [bpy], [bpraw])
                for a0 in range(0, nb, 4):
                    na = min(4, nb - a0)
                    pf, bpf = self.psf[2 + (a0 // 4) % 2]
                    for aa in range(na):
                        self.tr(pf[:, aa * 128:(aa + 1) * 128], ytok[:, a0 + aa, :], self.ident_f[:],
                                [bpraw, self.b_ident_f], [bpf], inc=(aa == na - 1))
                    self.copy("act", tmp[:, a0 * 128:(a0 + na) * 128], pf[:, 0:na * 128], [bpf], [btmp])
                self.stt(tmp[:, 0:n], u[:, 0:n], skip[:, cc:cc + 1], tmp[:, 0:n], ALU.mult, ALU.add, [bu, btaps, btmp], [btmp])
                self.tt("dve", ub[:, 0:n], tmp[:, 0:n], x0c[:, 0:n], ALU.mult, [btmp, bx0], [bub])
                self.dma("sp", self.ysT3[:, 2 + cc, tok0:tok0 + n], ub[:, 0:n], [bub], [self.dbuf["ysT"]])
        self.barrier()


K.phase_hyena = phase_hyena


NCH = NTOK // CH
C0 = math.exp(-0.5)


def phase_rwkv_prep(self, l):
    A = self.A
    with ExitStack() as es:
        w, bw = self.load_w_in(es, l, "rk_w", 0, 1024)
        mu = self.sb(es, "rk_mu", [128, 8], F32); bmu = Buf()
        om = self.sb(es, "rk_om", [128, 8], F32)
        hm = self.sb(es, "rk_hm", [128, 8], F32)
        self.dma("sp", mu[:], A["rwkv_mu"][l:l + 1, :].rearrange("o (j p) -> p (o j)", p=128), [], [bmu],
                 allow_slow_non_contiguous=True)
        self.ts("dve", om[:], mu[:], -1.0, 1.0, ALU.mult, ALU.add, [bmu], [bmu])
        self.ts("dve", hm[:], mu[:], 0.5, None, ALU.mult, None, [bmu], [bmu])
        colv = self.sb(es, "rk_colv", [128, 16], F32); bcv = Buf()
        for i, nme in enumerate(("rwkv_k_k", "rwkv_k_a", "rwkv_r_k")):
            self.dma("sp", colv[:, 2 * i:2 * i + 2], A[nme][l:l + 1, :].rearrange("o (c p) -> p (o c)", p=128), [], [bcv],
                     allow_slow_non_contiguous=True)
        for d in range(2):
            self.dma("sp", colv[:, 8 + 2 * d:10 + 2 * d], A["rwkv_w0"][l, d:d + 1, :].rearrange("o (c p) -> p (o c)", p=128),
                     [], [bcv], allow_slow_non_contiguous=True)
            self.dma("sp", colv[:, 12 + 2 * d:14 + 2 * d], A["rwkv_a0"][l, d:d + 1, :].rearrange("o (c p) -> p (o c)", p=128),
                     [], [bcv], allow_slow_non_contiguous=True)
        self.ts("dve", colv[:, 6:8], colv[:, 2:4], -1.0, 1.0, ALU.mult, ALU.add, [bcv], [bcv])
        wup = self.sb(es, "rk_wup", [64, 2, 256], F32)
        aup = self.sb(es, "rk_aup", [128, 2, 256], F32)
        gup = self.sb(es, "rk_gup", [128, 256], F32); bwl = Buf()
        for d in range(2):
            self.dma("sp", wup[:, d, :], A["rwkv_w_up"][l, d, :, :], [], [bwl])
            self.dma("sp", aup[64:128, d, :], A["rwkv_a_up"][l, d, :, :], [], [bwl])
        self.dma("sp", gup[:], A["rwkv_g_up"][l, :, :], [], [bwl])
        ones512 = self.sb(es, "rk_ones", [128, CH], F32); bones = Buf()
        self.memset("pool", ones512[:], 1.0, [bones])
        hch = [self.sb(es, "rk_h%d" % i, [128, 8, 514], BF16) for i in range(2)]; bhch = [Buf(), Buf()]
        pj = [self.sb(es, "rk_p%d" % j, [128, 512], F32) for j in range(8)]; bpj = [Buf() for _ in range(8)]
        NWK = 22
        wk = [self.sb(es, "rk_wk%d" % i, [128, 512], F32) for i in range(NWK)]
        bwk = [Buf() for _ in range(NWK)]
        tmo = [self.sb(es, "rk_tmo%d" % i, [128, 4, 128], F32) for i in range(3)]; btmo = [Buf(), Buf(), Buf()]
        wk1 = {i_: self.sb(es, "rk_wkb%d" % i_, [128, 512], F32) for i_ in range(7, NWK)}
        bwk1 = {i_: Buf() for i_ in range(7, NWK)}
        gcs2 = [self.sb(es, "rk_gcs%d" % i, [128, 8], F32) for i in range(2)]; bgcs2 = [Buf(), Buf()]

        def run_rr(gens):
            gens = list(gens)
            while gens:
                for g_ in list(gens):
                    try:
                        next(g_)
                    except StopIteration:
                        gens.remove(g_)
        fmA = A["rk_fm"].rearrange("(d k c) n -> d k c n", d=2, k=4)
        tmA = A["rk_tm"].rearrange("(d n) (k c) -> d n k c", d=2, k=2)
        gcA = A["rk_gc"].rearrange("(d c) n -> d c n", d=2)
        hi = 0
        ti_ = [0]
        psi = [0]

        def nps():
            psi[0] += 1
            return self.psf[psi[0] % 6]

        def to_tm(src, bsrc, nn, dstfn):
            nt = nn // 128
            pf, bpf = nps()
            for a in range(nt):
                self.tr(pf[:, a * 128:(a + 1) * 128], src[:, a * 128:(a + 1) * 128], self.ident_f[:],
                        [bsrc, self.b_ident_f], [bpf], inc=(a == nt - 1))
            t_, bt_ = tmo[ti_[0] % 3], btmo[ti_[0] % 3]
            ti_[0] += 1
            self.copy("act", t_[:, 0:nt, :], pf[:, 0:nt * 128].rearrange("p (a c) -> p a c", c=128), [bpf], [bt_])
            return t_, bt_, nt

        for (col0, tok0, n) in [(CTX0, 0, LC), (LAT0, LC, L)]:
            for t0 in range(0, n, 512):
                nn = min(512, n - t0)
                tk = tok0 + t0
                nsub = nn // CH
                h_, bh_ = hch[hi % 2], bhch[hi % 2]
                hi += 1
                c0 = col0 + t0
                self.dma("sp", h_[:, :, 0:nn + 2], self.hT3[:, :, c0 - 1:c0 + nn + 1], [self.dbuf["hT"]], [bh_])
                for j in range(8):
                    pa, bpa = nps()
                    for k in range(8):
                        self.mm(pa[:, 0:nn], w[:, k, j * 128:(j + 1) * 128], h_[:, k, 1:nn + 1], k == 0, k == 7, [bw, bh_], [bpa])
                    pb, bpb = nps()
                    for k in range(8):
                        self.mm(pb[:, 0:nn], w[:, k, j * 128:(j + 1) * 128], h_[:, k, 0:nn], k == 0, False, [bw, bh_], [bpb], inc=False)
                    for k in range(8):
                        self.mm(pb[:, 0:nn], w[:, k, j * 128:(j + 1) * 128], h_[:, k, 2:nn + 2], False, k == 7, [bw, bh_], [bpb])
                    self.act(pj[j][:, 0:nn], pa[:, 0:nn], AF.Identity, [bpa, bmu], [bpj[j]], scale=om[:, j:j + 1])
                    self.stt(pj[j][:, 0:nn], pb[:, 0:nn], hm[:, j:j + 1], pj[j][:, 0:nn], ALU.mult, ALU.add,
                             [bpb, bmu, bpj[j]], [bpj[j]])
                tw, btw = wk[0], bwk[0]
                sg, bsg = wk[1], bwk[1]
                self.act(tw[0:64, 0:nn], pj[6][0:64, 0:nn], AF.Tanh, [bpj[6]], [btw])
                self.act(sg[:, 0:nn], pj[7][:, 0:nn], AF.Sigmoid, [bpj[7]], [bsg])
                for cc in range(2):
                    S = slice(0, nn)
                    r_, br_ = pj[cc], bpj[cc]
                    k_, bk_ = pj[2 + cc], bpj[2 + cc]
                    v_, bv_ = pj[4 + cc], bpj[4 + cc]
                    pg, bpg = nps()
                    self.mm(pg[:, S], gup[:, cc * 128:(cc + 1) * 128], sg[:, S], True, True, [bwl, bsg], [bpg])
                    gT, bgT = wk[2], bwk[2]
                    self.copy("act", gT[:, S], pg[:, S], [bpg], [bgT])
                    self.dma("sp", A["rk_g"][cc * 128:(cc + 1) * 128, tk:tk + nn], gT[:, S], [bgT], [self.dbuf["rk_g"]])
                    t_, bt_, nt = to_tm(v_, bv_, nn, None)
                    self.dma("sp", A["rk_v"][tk:tk + nn, cc * 128:(cc + 1) * 128].rearrange("(a p) c -> p a c", p=128),
                             t_[:, 0:nt, :], [bt_], [self.dbuf["rk_v"]])
                    kx, bkx = wk[3], bwk[3]
                    sq, bsq = wk[4], bwk[4]
                    kk, bkk = wk[5], bwk[5]
                    self.ts("dve", kx[:, S], k_[:, S], colv[:, 0 + cc:1 + cc], None, ALU.mult, None, [bk_, bcv], [bkx])
                    self.tt("pool", sq[:, S], kx[:, S], kx[:, S], ALU.mult, [bkx], [bsq])
                    pn, bpn = nps()
                    self.mm(pn[:, S], self.blk_f[:], sq[:, S], True, True, [self.b_blk_f, bsq], [bpn])
                    self.ts("dve", sq[:, S], pn[:, S], 1e-24, None, ALU.max, None, [bpn], [bsq])
                    self.act(sq[:, S], sq[:, S], AF.Sqrt, [bsq], [bsq])
                    self.op("dve", lambda e, sq=sq, S=S: e.reciprocal(out=sq[:, S], in_=sq[:, S]), [bsq], [bsq])
                    self.tt("dve", kk[:, S], kx[:, S], sq[:, S], ALU.mult, [bkx, bsq], [bkk])
                    ksum, bks = wk[6], bwk[6]
                    def dchain(d, cc=cc, S=S, nn=nn, nsub=nsub, tk=tk, r_=r_, br_=br_, k_=k_, bk_=bk_, kk=kk, bkk=bkk, ksum=ksum, bks=bks, tw=tw, btw=btw):
                        W = (lambda i_: (wk[i_], bwk[i_])) if d == 0 else (lambda i_: (wk1[i_], bwk1[i_]))
                        gcs, bgcs = gcs2[d], bgcs2[d]
                        pw, bpw = nps()
                        self.mm(pw[:, S], wup[:, d, cc * 128:(cc + 1) * 128], tw[0:64, S], True, True, [bwl, btw], [bpw])
                        sgm, bsgm = W(7)
                        self.act(sgm[:, S], pw[:, S], AF.Sigmoid, [bpw, bcv], [bsgm], bias=colv[:, 8 + 2 * d + cc:9 + 2 * d + cc])
                        yield
                        lw, blw = W(8)
                        self.ts("pool", lw[:, S], sgm[:, S], -C0, None, ALU.mult, None, [bsgm], [blw])
                        pa_, bpa_ = nps()
                        self.mm(pa_[:, S], aup[64:128, d, cc * 128:(cc + 1) * 128], pj[6][64:128, S], True, True, [bwl, bpj[6]], [bpa_])
                        a_, ba_ = W(9)
                        self.act(a_[:, S], pa_[:, S], AF.Sigmoid, [bpa_, bcv], [ba_], bias=colv[:, 12 + 2 * d + cc:13 + 2 * d + cc])
                        yield
                        kd, bkd = W(10)
                        self.ts("dve", kd[:, S], a_[:, S], colv[:, 2 + cc:3 + cc], colv[:, 6 + cc:7 + cc], ALU.mult, ALU.add,
                                [ba_, bcv], [bkd])
                        self.tt("dve", kd[:, S], kd[:, S], k_[:, S], ALU.mult, [bkd, bk_], [bkd])
                        b_, bb_ = W(11)
                        self.tt("pool", b_[:, S], kk[:, S], a_[:, S], ALU.mult, [bkk, ba_], [bb_])
                        if d == 0:
                            self.copy("pool", ksum[:, S], kd[:, S], [bkd], [bks])
                        else:
                            self.tt("pool", ksum[:, S], ksum[:, S], kd[:, S], ALU.add, [bks, bkd], [bks])
                        yield
                        ci_, bci = W(12)
                        for sb_ in range(nsub):
                            sl = slice(sb_ * CH, (sb_ + 1) * CH)
                            self.op("dve", lambda e, ci_=ci_, lw=lw, sl=sl: e.tensor_tensor_scan(
                                out=ci_[:, sl], data0=ones512[:, 0:CH], data1=lw[:, sl], initial=0.0,
                                op0=ALU.mult, op1=ALU.add), [blw, bones], [bci])
                        yield
                        self.copy("dve", gcs[:, 0:nsub], ci_[:, CH - 1:nn:CH], [bci], [bgcs])
                        if d == 1:
                            for sb_ in range(nsub):
                                sl = slice(sb_ * CH, (sb_ + 1) * CH)
                                self.ts("dve", ci_[:, sl], ci_[:, sl], -1.0, gcs[:, sb_:sb_ + 1], ALU.mult, ALU.add, [bci, bgcs], [bci])
                            self.tt("dve", ci_[:, S], ci_[:, S], lw[:, S], ALU.add, [bci, blw], [bci])
                        yield
                        ce, bce = W(13)
                        self.tt("pool", ce[:, S], ci_[:, S], lw[:, S], ALU.subtract, [bci, blw], [bce])
                        yield
                        e1, be1 = W(14)
                        e2, be2 = W(15)
                        e3, be3 = W(16)
                        e4, be4 = W(17)
                        self.act(e1[:, S], ce[:, S], AF.Exp, [bce], [be1])
                        self.act(e2[:, S], ci_[:, S], AF.Exp, [bci], [be2])
                        self.act(e3[:, S], ci_[:, S], AF.Exp, [bci], [be3], scale=-1.0)
                        for sb_ in range(nsub):
                            sl = slice(sb_ * CH, (sb_ + 1) * CH)
                            self.act(e4[:, sl], ci_[:, sl], AF.Exp, [bci, bgcs], [be4], scale=-1.0, bias=gcs[:, sb_:sb_ + 1])
                        yield
                        gce, bgce = W(18)
                        self.act(gce[:, 0:nsub], gcs[:, 0:nsub], AF.Exp, [bgcs], [bgce])
                        self.dma("sp", gcA[d, cc * 128:(cc + 1) * 128, tk // CH:tk // CH + nsub], gce[:, 0:nsub], [bgce],
                                 [self.dbuf["rk_gc"]], allow_slow_non_contiguous=True)
                        yield
                        o_, bo_ = W(19)
                        for kind, (x_, bx_, e_, be_) in enumerate(((kk, bkk, e1, be1), (r_, br_, e2, be2),
                                                                    (b_, bb_, e3, be3), (kd, bkd, e3, be3))):
                            o_, bo_ = W(19 + kind % 2)
                            self.tt("dve" if kind % 2 == 0 else "pool", o_[:, S], x_[:, S], e_[:, S], ALU.mult, [bx_, be_], [bo_])
                            self.dma("sp", fmA[d, kind, cc * 128:(cc + 1) * 128, tk:tk + nn], o_[:, S], [bo_], [self.dbuf["rk_fm"]])
                        yield
                        for kind, (x_, bx_) in enumerate(((b_, bb_), (kd, bkd))):
                            o_, bo_ = W(21)
                            self.tt("dve", o_[:, S], x_[:, S], e4[:, S], ALU.mult, [bx_, be4], [bo_])
                            t_, bt_, nt = to_tm(o_, bo_, nn, None)
                            self.dma("sp", tmA[d, tk:tk + nn, kind, cc * 128:(cc + 1) * 128].rearrange("(a p) c -> p a c", p=128),
                                     t_[:, 0:nt, :], [bt_], [self.dbuf["rk_tm"]])

                    run_rr([dchain(0), dchain(1)])
                    self.stt(ksum[:, S], r_[:, S], colv[:, 4 + cc:5 + cc], ksum[:, S], ALU.mult, ALU.mult, [br_, bcv, bks], [bks])
                    pbn, bpbn = nps()
                    self.mm(pbn[:, S], self.blk_f[:], ksum[:, S], True, True, [self.b_blk_f, bks], [bpbn])
                    bo2, bbo2 = wk[2], bwk[2]
                    self.tt("dve", bo2[:, S], pbn[:, S], v_[:, S], ALU.mult, [bpbn, bv_], [bbo2])
                    self.dma("sp", A["rk_bonus"][cc * 128:(cc + 1) * 128, tk:tk + nn], bo2[:, S], [bbo2], [self.dbuf["rk_bonus"]])
        self.barrier()


K.phase_rwkv_prep = phase_rwkv_prep


def phase_rwkv_scan(self, l, ctx_out):
    A = self.A
    fmA = A["rk_fm"].rearrange("(d k c) n -> d k c n", d=2, k=4)
    tmA = A["rk_tm"].rearrange("(d n) (k c) -> d n k c", d=2, k=2)
    gcA = A["rk_gc"].rearrange("(d c) n -> d c n", d=2)
    yA = A["rk_y"].rearrange("(d n) c -> d n c", d=2)
    F32R = mybir.dt.float32r

    def R_(ap):
        return ap.bitcast(F32R)
    with ExitStack() as es:
        base = self.sb(es, "sc_mbase", [64, 4, 64], F32); bmk = Buf()
        for i, (pat, cm, cmp_) in enumerate((([[1, 64]], -1, ALU.is_gt), ([[1, 64]], -1, ALU.is_ge),
                                             ([[-1, 64]], 1, ALU.is_gt), ([[-1, 64]], 1, ALU.is_ge))):
            self.memset("pool", base[:, i, :], 1.0, [bmk])
            self.op("pool", lambda e, i=i, pat=pat, cm=cm, cmp_=cmp_: e.affine_select(
                out=base[:, i, :], in_=base[:, i, :], pattern=pat, compare_op=cmp_, fill=0.0, base=0,
                channel_multiplier=cm), [bmk], [bmk])
        amask = self.sb(es, "sc_amask", [64, 2, 128], F32)
        mmask = self.sb(es, "sc_mmask", [64, 2, 128], F32)
        ntmask = self.sb(es, "sc_ntmask", [64, 2, 64], F32)
        for d in range(2):
            st_, in_ = (0, 1) if d == 0 else (2, 3)
            self.ts("dve", amask[:, d, 0:64], base[:, st_, :], -1.0, None, ALU.mult, None, [bmk], [bmk])
            self.copy("dve", amask[:, d, 64:128], base[:, st_, :], [bmk], [bmk])
            self.copy("dve", mmask[:, d, 0:64], base[:, in_, :], [bmk], [bmk])
            self.copy("dve", mmask[:, d, 64:128], base[:, in_, :], [bmk], [bmk])
            self.ts("dve", ntmask[:, d, :], base[:, 2 if d == 0 else 0, :], -1.0, None, ALU.mult, None, [bmk], [bmk])
        idb = self.ident_f[0:64, 0:64].unsqueeze(1).to_broadcast([64, 4, 64])
        R = []
        for d in range(2):
            r = {}
            r["gcs"] = self.sb(es, "sc_gcs%d" % d, [64, 4, NCH], F32); r["bgcs"] = Buf()
            self.dma("sp", r["gcs"][:], gcA[d].rearrange("(h k) n -> k h n", k=64), [self.dbuf["rk_gc"]], [r["bgcs"]])
            for nm, shp in (("fm", [64, 4, 4, 64]), ("tm", [64, 3, 256]), ("fmr", [64, 4, 4, 64]), ("tmr", [64, 3, 256]), ("AMa", [64, 4, 128]), ("AMm", [64, 4, 128]),
                            ("NT", [64, 4, 64]), ("AkV", [64, 4, 64]), ("X", [64, 4, 64]), ("XT", [64, 4, 64]),
                            ("P0", [64, 4, 64]), ("P1", [64, 4, 64]), ("Q0", [64, 4, 64]), ("Q1", [64, 4, 64])):
                r[nm] = [self.sb(es, "sc_%s%d_%d" % (nm, d, s_), shp, F32) for s_ in range(2)]
                r["b" + nm] = [Buf(), Buf()]
            for nm in ("RHS", "Zs", "Ys", "ST2", "STr"):
                r[nm] = self.sb(es, "sc_%s%d" % (nm, d), [64, 4, 64], F32); r["b" + nm] = Buf()
            r["ST"] = [self.sb(es, "sc_ST%d_%d" % (d, s_), [64, 4, 64], F32) for s_ in range(2)]
            r["bST"] = [Buf(), Buf()]
            self.memset("pool", r["ST"][0][:], 0.0, [r["bST"][0]])
            self.copy("dve", R_(r["STr"][:]), r["ST"][0][:], [r["bST"][0]], [r["bSTr"]])
            r["B0"] = self.psf[2 * d]
            r["B1"] = self.psf[2 * d + 1]
            r["B3"] = self.psf[4 + d]
            r["B2"] = (self.psb[d][0][:].bitcast(F32), self.psb[d][1])
            r["order"] = ([0, 1, 2, 3] + list(range(4, NCH))) if d == 0 else ([3, 2, 1, 0] + list(range(NCH - 1, 3, -1)))
            R.append(r)

        def v4(ap):
            return ap.rearrange("p (h t) -> p h t", h=4)

        def pre(d, g, s_):
            r = R[d]
            fm, bfm = r["fm"][s_], r["bfm"][s_]
            tm, btm = r["tm"][s_], r["btm"][s_]
            tsl = slice(g * CH, (g + 1) * CH)
            for kind in range(4):
                self.dma("sp", fm[:, :, kind, :], fmA[d, kind].rearrange("(h k) n -> k h n", k=64)[:, :, tsl],
                         [self.dbuf["rk_fm"]], [bfm])
            self.dma("sp", tm[:, 0:2, :], tmA[d, tsl, :, :], [self.dbuf["rk_tm"]], [btm])
            self.dma("sp", tm[:, 2, :], A["rk_v"][tsl, :], [self.dbuf["rk_v"]], [btm])
            yield
            fmr, bfmr = r["fmr"][s_], r["bfmr"][s_]
            tmr, btmr = r["tmr"][s_], r["btmr"][s_]
            self.copy("pool", R_(fmr[:]), fm[:], [bfm], [bfmr])
            self.copy("act", R_(tmr[:]), tm[:], [btm], [btmr])
            yield
            fm, bfm, tm, btm = fmr, bfmr, tmr, btmr
            b0, bb0 = r["B0"]
            b1, bb1 = r["B1"]
            b2, bb2 = r["B2"]
            AMa, bAMa = r["AMa"][s_], r["bAMa"][s_]
            AMm, bAMm = r["AMm"][s_], r["bAMm"][s_]
            NT, bNT = r["NT"][s_], r["bNT"][s_]
            AkV, bAkV = r["AkV"][s_], r["bAkV"][s_]
            X, bX = r["X"][s_], r["bX"][s_]
            for h in range(4):
                self.mm(b0[0:64, h * 128:h * 128 + 64], R_(fm[:, h, 2, :]), R_(fm[:, h, 0, :]), True, True, [bfm], [bb0], inc=False)
                self.mm(b0[0:64, h * 128 + 64:h * 128 + 128], R_(fm[:, h, 3, :]), R_(fm[:, h, 0, :]), True, True, [bfm], [bb0], inc=(h == 3))
            for h in range(4):
                self.mm(b1[0:64, h * 64:(h + 1) * 64], R_(fm[:, h, 0, :]), R_(fm[:, h, 2, :]), True, True, [bfm], [bb1], inc=(h == 3))
            for h in range(4):
                self.mm(b2[0:64, h * 128:h * 128 + 64], R_(fm[:, h, 2, :]), R_(fm[:, h, 1, :]), True, True, [bfm], [bb2], inc=False)
                self.mm(b2[0:64, h * 128 + 64:h * 128 + 128], R_(fm[:, h, 3, :]), R_(fm[:, h, 1, :]), True, True, [bfm], [bb2], inc=(h == 3))
            yield
            self.tt("dve", R_(AMa[:]), v4(b0[0:64, :]), amask[:, d, :].unsqueeze(1).to_broadcast([64, 4, 128]), ALU.mult, [bb0, bmk], [bAMa])
            self.tt("dve", R_(NT[:]), v4(b1[0:64, 0:256]), ntmask[:, d, :].unsqueeze(1).to_broadcast([64, 4, 64]), ALU.mult, [bb1, bmk], [bNT])
            self.tt("dve", R_(AMm[:]), v4(b2[0:64, :]), mmask[:, d, :].unsqueeze(1).to_broadcast([64, 4, 128]), ALU.mult, [bb2, bmk], [bAMm])
            yield
            self.tt("dve", R_(X[:]), AMa[:, :, 0:64], idb, ALU.add, [bAMa, self.b_ident_f], [bX])
            Pl = [(AMa[:, :, 0:64], bAMa)]
            Ql = [(NT[:], bNT)]
            for j in range(1, 6):
                Pl.append((r["P%d" % (j % 2)][s_][:], r["bP%d" % (j % 2)][s_]))
                Ql.append((r["Q%d" % (j % 2)][s_][:], r["bQ%d" % (j % 2)][s_]))
            for stg in range(1, 7):
                if stg == 1:
                    for h in range(4):
                        self.mm(b1[0:64, 256 + h * 64:256 + (h + 1) * 64], R_(AMa[:, h, 64:128]), R_(tm[:, 2, h * 64:(h + 1) * 64]), True, True,
                                [bAMa, btm], [bb1], inc=(h == 3))
                if stg <= 5:
                    (Pp, bPp), (Qp, bQp) = Pl[stg - 1], Ql[stg - 1]
                    if stg < 5:
                        for h in range(4):
                            self.mm(b0[0:64, h * 64:(h + 1) * 64], R_(Qp[:, h, :]), R_(Pp[:, h, :]), True, True, [bPp, bQp], [bb0], inc=False)
                    for h in range(4):
                        self.mm(b0[0:64, 256 + h * 64:256 + (h + 1) * 64], R_(Pp[:, h, :]), R_(Qp[:, h, :]), True, True, [bPp, bQp], [bb0],
                                inc=(h == 3))
                if stg >= 2:
                    Qa, bQa = Ql[stg - 1]
                    for h in range(4):
                        self.mm(b1[0:64, h * 64:(h + 1) * 64], R_(Qa[:, h, :]), R_(X[:, h, :]), True, True, [bQa, bX], [bb1], inc=(h == 3))
                yield
                if stg == 1:
                    self.copy("dve", AkV[:], v4(b1[0:64, 256:512]), [bb1], [bAkV])
                if stg >= 2:
                    self.tt("dve", R_(X[:]), v4(b1[0:64, 0:256]), X[:], ALU.add, [bb1, bX], [bX])
                if stg <= 5:
                    if stg < 5:
                        Pn, bPn = Pl[stg]
                        self.copy("act", R_(Pn), v4(b0[0:64, 0:256]), [bb0], [bPn])
                    Qn, bQn = Ql[stg]
                    self.copy("act", R_(Qn), v4(b0[0:64, 256:512]), [bb0], [bQn])
                yield

        def seq(d, g, s_, i):
            r = R[d]
            fm, bfm = r["fmr"][s_], r["bfmr"][s_]
            tm, btm = r["tmr"][s_], r["btmr"][s_]
            b2, bb2 = r["B2"]
            b3, bb3 = r["B3"]
            STr, bSTr = r["STr"], r["bSTr"]
            STc, bSTc = r["ST"][i % 2], r["bST"][i % 2]
            STn, bSTn = r["ST"][(i + 1) % 2], r["bST"][(i + 1) % 2]
            X, bX = r["X"][s_], r["bX"][s_]
            AMm, bAMm = r["AMm"][s_], r["bAMm"][s_]
            AkV, bAkV = r["AkV"][s_], r["bAkV"][s_]
            RHS, bRHS = r["RHS"], r["bRHS"]
            Zs, bZs = r["Zs"], r["bZs"]
            Ys, bYs = r["Ys"], r["bYs"]
            ST2, bST2 = r["ST2"], r["bST2"]
            for h in range(4):
                self.mm(b3[0:64, h * 64:(h + 1) * 64], R_(fm[:, h, 0, :]), R_(STr[:, h, :]), True, True, [bfm, bSTr], [bb3], inc=(h == 3))
            self.tt("pool", ST2[:], STc[:], r["gcs"][:, :, g:g + 1].to_broadcast([64, 4, 64]), ALU.mult, [bSTc, r["bgcs"]], [bST2])
            yield
            self.stt(R_(RHS[:]), v4(b3[0:64, 0:256]), -1.0, AkV[:], ALU.mult, ALU.subtract, [bb3, bAkV], [bRHS])
            yield
            for h in range(4):
                self.mm(b3[0:64, 256 + h * 64:256 + (h + 1) * 64], R_(X[:, h, :]), R_(RHS[:, h, :]), True, True, [bX, bRHS], [bb3], inc=(h == 3))
            yield
            self.copy("act", R_(Zs[:]), v4(b3[0:64, 256:512]), [bb3], [bZs])
            yield
            emit = ctx_out or g >= 4
            for h in range(4):
                self.mm(b2[0:64, h * 64:(h + 1) * 64], R_(tm[:, 0, h * 64:(h + 1) * 64]), R_(Zs[:, h, :]), True, False, [btm, bZs], [bb2], inc=False)
                self.mm(b2[0:64, h * 64:(h + 1) * 64], R_(tm[:, 1, h * 64:(h + 1) * 64]), R_(tm[:, 2, h * 64:(h + 1) * 64]), False, True,
                        [btm], [bb2], inc=(h == 3 and not emit))
            if emit:
                for h in range(4):
                    o = b2[0:64, 256 + h * 64:256 + (h + 1) * 64]
                    self.mm(o, R_(fm[:, h, 1, :]), R_(STr[:, h, :]), True, False, [bfm, bSTr], [bb2], inc=False)
                    self.mm(o, R_(AMm[:, h, 0:64]), R_(Zs[:, h, :]), False, False, [bAMm, bZs], [bb2], inc=False)
                    self.mm(o, R_(AMm[:, h, 64:128]), R_(tm[:, 2, h * 64:(h + 1) * 64]), False, True, [bAMm, btm], [bb2], inc=(h == 3))
            yield
            self.tt("dve", STn[:], v4(b2[0:64, 0:256]), ST2[:], ALU.add, [bb2, bST2], [bSTn])
            self.copy("act", R_(STr[:]), STn[:], [bSTn], [bSTr])
            if emit:
                self.copy("dve", Ys[:], v4(b2[0:64, 256:512]), [bb2], [bYs])
                self.dma("sp", yA[d, g * CH:(g + 1) * CH, :], Ys[:].rearrange("p h t -> p (h t)"), [bYs], [self.dbuf["rk_y"]])
            yield

        def run_rr(gens):
            gens = list(gens)
            while gens:
                for g_ in list(gens):
                    try:
                        next(g_)
                    except StopIteration:
                        gens.remove(g_)

        run_rr([pre(d, R[d]["order"][0], 0) for d in range(2)])
        for i in range(NCH):
            gl = []
            for d in range(2):
                if i + 1 < NCH:
                    gl.append(pre(d, R[d]["order"][i + 1], (i + 1) % 2))
                gl.append(seq(d, R[d]["order"][i], i % 2, i))
            run_rr(gl)
        self.barrier()


K.phase_rwkv_scan = phase_rwkv_scan


def phase_rwkv_out(self, l, tiles):
    A = self.A
    yA = A["rk_y"].rearrange("(d n) c -> d n c", d=2)
    with ExitStack() as es:
        gb = self.sb(es, "ro_gb", [128, 2, 256], F32); bgb = Buf()
        self.dma("sp", gb[:, 0, :], A["rwkv_lnx_g"][l:l + 1, :].partition_broadcast(128), [], [bgb])
        self.dma("sp", gb[:, 1, :], A["rwkv_lnx_b"][l:l + 1, :].partition_broadcast(128), [], [bgb])
        NB = 2
        yf = [self.sb(es, "ro_yf%d" % i, [128, 256], F32) for i in range(NB)]
        yb = [self.sb(es, "ro_yb%d" % i, [128, 256], F32) for i in range(NB)]
        bo = [self.sb(es, "ro_bo%d" % i, [128, 2, 128], F32) for i in range(NB)]
        gt = [self.sb(es, "ro_gt%d" % i, [128, 2, 128], F32) for i in range(NB)]
        bin_ = [Buf() for _ in range(NB)]
        st = self.sb(es, "ro_st", [128, 4, 6], F32); mv = self.sb(es, "ro_mv", [128, 4, 2], F32)
        rs = self.sb(es, "ro_rs", [128, 4], F32); bs = Buf()
        yn = self.sb(es, "ro_yn", [128, 256], F32); byn = Buf()
        res = [self.sb(es, "ro_res%d" % i, [128, 2, 128], BF16) for i in range(NB)]; bres = [Buf() for _ in range(NB)]
        tmp = self.sb(es, "ro_tmp", [128, 2, 128], F32); btmp = Buf()
        def tile_gen(n, ti):
            s = n % NB
            tk = ti * 128
            self.dma("sp", yf[s][:], yA[0, tk:tk + 128, :], [self.dbuf["rk_y"]], [bin_[s]])
            self.dma("sp", yb[s][:], yA[1, tk:tk + 128, :], [self.dbuf["rk_y"]], [bin_[s]])
            self.dma("sp", bo[s][:], A["rk_bonus"].rearrange("(c p) n -> p c n", p=128)[:, :, tk:tk + 128], [self.dbuf["rk_bonus"]], [bin_[s]])
            self.dma("sp", gt[s][:], A["rk_g"].rearrange("(c p) n -> p c n", p=128)[:, :, tk:tk + 128], [self.dbuf["rk_g"]], [bin_[s]])
            yield
            self.tt("dve", yf[s][:], yf[s][:], yb[s][:], ALU.add, [bin_[s]], [bin_[s]])
            for h in range(4):
                self.op("dve", lambda e, h=h, s=s: e.bn_stats(out=st[:, h, :], in_=yf[s][:, h * 64:(h + 1) * 64]), [bin_[s]], [bs])
                self.op("dve", lambda e, h=h: e.bn_aggr(out=mv[:, h, :], in_=st[:, h, :]), [bs], [bs])
            yield True
            self.act(rs[:], mv[:, :, 1], AF.Sqrt, [bs], [bs], bias=64e-5, scale=1.0)
            self.op("dve", lambda e: e.reciprocal(out=rs[:], in_=rs[:]), [bs], [bs])
            for h in range(4):
                self.ts("dve", yn[:, h * 64:(h + 1) * 64], yf[s][:, h * 64:(h + 1) * 64], mv[:, h, 0:1], rs[:, h:h + 1],
                        ALU.subtract, ALU.mult, [bin_[s], bs], [byn])
            self.tt("pool", yn[:], yn[:], gb[:, 0, :], ALU.mult, [byn, bgb], [byn])
            self.tt("pool", yn[:], yn[:], gb[:, 1, :], ALU.add, [byn, bgb], [byn])
            yield
            pf, bpf = self.psf[n % 2]
            for cc in range(2):
                self.tr(pf[:, cc * 128:(cc + 1) * 128], yn[:, cc * 128:(cc + 1) * 128], self.ident_f[:], [byn, self.b_ident_f], [bpf],
                        inc=(cc == 1))
            yield
            self.tt("dve", tmp[:], pf[:, 0:256].rearrange("p (c t) -> p c t", c=2), bo[s][:], ALU.add, [bpf, bin_[s]], [btmp])
            self.tt("dve", res[s][:], tmp[:], gt[s][:], ALU.mult, [btmp, bin_[s]], [bres[s]])
            self.dma("sp", self.ysT3[:, 0:2, tk:tk + 128], res[s][:], [bres[s]], [self.dbuf["ysT"]])
            yield

        run_pipeline((tile_gen(n, ti) for n, ti in enumerate(tiles)), depth=2)
        self.barrier()


K.phase_rwkv_out = phase_rwkv_out


def phase_rwkv(self, l, ctx_out):
    self.phase_rwkv_prep(l)
    self.phase_rwkv_scan(l, ctx_out)
    self.phase_rwkv_out(l, list(range(NTILE)) if ctx_out else list(range(2, NTILE)))


def phase_rwkv_out_merge(self, l, ctx_out, tiles_merge):
    with ExitStack() as es:
        pre = self.merge_load(es, l)
        self.phase_rwkv_out(l, list(range(NTILE)) if ctx_out else list(range(2, NTILE)))
        self.phase_merge(l, tiles_merge, pre=pre)


K.phase_rwkv_out_merge = phase_rwkv_out_merge


K.phase_rwkv = phase_rwkv


def build_program(dbg=False):
    nc = bass.Bass("TRN2", target_bir_lowering=False)
    k = K(nc, dbg=dbg)
    k.setup()
    alltiles = list(range(NTILE))
    lat = list(range(2, NTILE))
    for l in range(DEPTH):
        ctx_out = l < DEPTH - 1
        k.phase_mod(l)
        k.phase_h(l, alltiles)
        k.phase_sconv(l, ctx_out)
        k.phase_attn(l, ctx_out)
        k.phase_hyena(l, ctx_out)
        k.phase_rwkv_prep(l)
        k.phase_rwkv_scan(l, ctx_out)
        tl = alltiles if ctx_out else lat
        k.phase_rwkv_out_merge(l, ctx_out, tl)
        k.phase_moe(l, tl, not ctx_out)
    k.P.finish([k.dbuf["out"]])
    return nc, k


_CACHE = {}


def kernel(**inputs):
    n = 8
    if "nc" not in _CACHE:
        _CACHE["nc"] = build_program()[0]
    nc = _CACHE["nc"]
    maps = make_in_maps(inputs, list(range(n)))
    res = run_bass_kernel_spmd(nc, maps, core_ids=list(range(n)))
    out = np.stack([np.asarray(res.results[i]["out"], dtype=np.float32) for i in range(n)], 0)
    return out
```

```python
from contextlib import ExitStack
import math
import numpy as np
import concourse.bass as bass
import concourse.mybir as mybir
from concourse.bass_utils import run_bass_kernel_spmd

F32 = mybir.dt.float32
BF16 = mybir.dt.bfloat16
I32 = mybir.dt.int32
AF = mybir.ActivationFunctionType
ALU = mybir.AluOpType
AX = mybir.AxisListType


class Buf:
    __slots__ = ("name", "w", "r", "excl")

    def __init__(self, name="", excl=False):
        self.name = name
        self.w = {}
        self.r = {}
        self.excl = excl


class Prog:
    CE = ["pe", "dve", "act", "pool", "sp"]
    SAME = {"pe": False, "dve": True, "act": True, "pool": True, "sp": False}
    NDQ = 6

    def __init__(self, nc):
        self.nc = nc
        self.ops = {e: [] for e in self.CE}
        self.cnt = {}
        self.sem = {}
        self.seen = {e: {} for e in self.CE}
        for e in self.CE:
            self.sem[e] = nc.alloc_semaphore("sem_" + e)
            self.cnt[e] = 0
        self.dq = {}
        self.dq_next = {}
        for q in ("sp", "pool", "act"):
            names = []
            for i in range(self.NDQ):
                n = "dq_%s_%d" % (q, i)
                self.sem[n] = nc.alloc_semaphore("sem_" + n)
                self.cnt[n] = 0
                names.append(n)
            self.dq[q] = names
            self.dq_next[q] = 0
        self.nins = 0

    def _mult(self, e):
        return 16 if e.startswith("dq_") else 1

    def _waits(self, eng, reads, writes, extra=()):
        need = {}

        def add(e2, c):
            if c > need.get(e2, 0):
                need[e2] = c
        for b in reads:
            for e2, c in b.w.items():
                add(e2, c)
            if b.excl:
                for e2, c in b.r.items():
                    if e2 != eng:
                        add(e2, c)
        for b in writes:
            for e2, c in b.w.items():
                add(e2, c)
            for e2, c in b.r.items():
                add(e2, c)
        for e2, c in extra:
            add(e2, c)
        out = []
        seen = self.seen[eng]
        for e2, c in need.items():
            if e2 == eng and not self.SAME[eng]:
                continue
            if c > seen.get(e2, 0):
                seen[e2] = c
                out.append((self.sem[e2], c * self._mult(e2)))
        return out

    def op(self, eng, fn, reads=(), writes=(), inc=True):
        waits = self._waits(eng, reads, writes)
        if inc:
            self.cnt[eng] += 1
            my = self.cnt[eng]
        else:
            my = self.cnt[eng] + 1
        self.ops[eng].append((waits, fn, self.sem[eng] if inc else None, 1))
        for b in reads:
            b.r[eng] = max(b.r.get(eng, 0), my)
        for b in writes:
            b.w[eng] = my
            b.r = {}
        self.nins += 1

    def dma(self, q, fn, reads=(), writes=()):
        names = self.dq[q]
        n = names[self.dq_next[q] % len(names)]
        self.dq_next[q] += 1
        extra = [(n, self.cnt[n])] if self.cnt[n] > 0 else []
        waits = self._waits(q, reads, writes, extra)
        self.cnt[n] += 1
        my = self.cnt[n]
        self.ops[q].append((waits, fn, self.sem[n], 16))
        for b in reads:
            b.r[n] = max(b.r.get(n, 0), my)
        for b in writes:
            b.w[n] = my
            b.r = {}
        self.nins += 1

    def finish(self, final_bufs):
        waits = self._waits("sp", final_bufs, ())
        self.ops["sp"].append((waits, None, None, 0))
        nc = self.nc
        ops = self.ops
        with nc.Block() as block:
            def emit(engobj, lst):
                for waits, fn, sem, k in lst:
                    for s, v in waits:
                        engobj.wait_ge(s, v)
                    if fn is not None:
                        ins = fn(engobj)
                        if sem is not None:
                            ins.then_inc(sem, k)

            @block.tensor
            def _(e):
                emit(e, ops["pe"])

            @block.vector
            def _(e):
                emit(e, ops["dve"])

            @block.scalar
            def _(e):
                emit(e, ops["act"])

            @block.gpsimd
            def _(e):
                emit(e, ops["pool"])

            @block.sync
            def _(e):
                emit(e, ops["sp"])


D = 1024
L = 4096
LC = 256
NTOK = L + LC
NTILE = NTOK // 128
NTP = NTOK + 4
CTX0 = 1
LAT0 = 259
DEPTH = 2
IN_COLS = 7168
OFF_HYENA = 1024
OFF_SCONV = 1792
OFF_ATTN = 2560
OFF_GATE = 3072
NE = 16
DE = 512
ALPHA = (2 * DEPTH) ** 0.25
LN_EPS = 1e-6
PI = math.pi
TWO_PI = 2.0 * math.pi
MAGIC = 12582912.0
CH = 64
SBUF_LIMIT = 208 * 1024


def tok_col(t):
    return CTX0 + t if t < LC else LAT0 + (t - LC)


class K:
    def __init__(self, nc, dbg=False):
        self.nc = nc
        self.P = Prog(nc)
        self.dbg = dbg
        self.dram = {}
        self.dbuf = {}

    def din(self, name, shape, dt=F32):
        t = self.nc.dram_tensor(name, list(shape), dt, kind="ExternalInput")
        self.dram[name] = t
        self.dbuf[name] = Buf(name)
        return t.ap()

    def dscr(self, name, shape, dt=F32, out=False):
        kind = "ExternalOutput" if (out or self.dbg) else "Internal"
        t = self.nc.dram_tensor(name, list(shape), dt, kind=kind)
        self.dram[name] = t
        self.dbuf[name] = Buf(name)
        return t.ap()

    def sb(self, es, name, shape, dt=F32):
        self._uid = getattr(self, "_uid", 0) + 1
        t = es.enter_context(self.nc.sbuf_tensor("%s_u%d" % (name, self._uid), list(shape), dt))
        nb = 1
        for d_ in shape[1:]:
            nb *= d_
        nb *= 2 if dt == BF16 else 4
        nb = (nb + 31) // 32 * 32
        self.sb_used = getattr(self, "sb_used", 17 * 1024) + nb
        self.sb_peak = max(getattr(self, "sb_peak", 0), self.sb_used)

        def _free(nb=nb):
            self.sb_used -= nb
        es.callback(_free)
        if self.sb_used > SBUF_LIMIT:
            raise RuntimeError("SBUF budget exceeded at %s: %d" % (name, self.sb_used))
        return t

    def op(self, eng, fn, r=(), w=(), inc=True):
        self.P.op(eng, fn, r, w, inc)

    def dma(self, q, out, in_, r=(), w=(), **kw):
        self.P.dma(q, lambda e: e.dma_start(out=out, in_=in_, **kw), r, w)

    def copy(self, eng, out, in_, r, w):
        if eng == "act":
            self.op("act", lambda e: e.activation(out=out, in_=in_, func=AF.Copy), r, w)
        else:
            self.op(eng, lambda e: e.tensor_copy(out=out, in_=in_), r, w)

    def act(self, out, in_, func, r, w, bias=0.0, scale=1.0):
        self.op("act", lambda e: e.activation(out=out, in_=in_, func=func, bias=bias, scale=scale), r, w)

    def ts(self, eng, out, in0, s1, s2, op0, op1, r, w):
        if op1 is None:
            self.op(eng, lambda e: e.tensor_scalar(out=out, in0=in0, scalar1=s1, scalar2=None, op0=op0), r, w)
        else:
            self.op(eng, lambda e: e.tensor_scalar(out=out, in0=in0, scalar1=s1, scalar2=s2, op0=op0, op1=op1), r, w)

    def tt(self, eng, out, in0, in1, op, r, w):
        self.op(eng, lambda e: e.tensor_tensor(out=out, in0=in0, in1=in1, op=op), r, w)

    def stt(self, out, in0, scalar, in1, op0, op1, r, w):
        self.op("dve", lambda e: e.scalar_tensor_tensor(out=out, in0=in0, scalar=scalar, in1=in1, op0=op0, op1=op1), r, w)

    def mm(self, out, lhsT, rhs, start, stop, r, w, inc=None):
        if inc is None:
            inc = stop
        self.op("pe", lambda e: e.matmul(out, lhsT=lhsT, rhs=rhs, start=start, stop=stop), r, w, inc)

    def tr(self, out, in_, ident, r, w, inc=True):
        self.op("pe", lambda e: e.transpose(out=out, in_=in_, identity=ident), r, w, inc)

    def memset(self, eng, ap, val, w):
        self.op(eng, lambda e: e.memset(ap, val), (), w)

    def barrier(self):
        P = self.P
        allc = [(e, c) for e, c in P.cnt.items() if c > 0]
        for eng in P.CE:
            waits = []
            for e2, c in allc:
                if e2 == eng and not P.SAME[eng]:
                    continue
                if c > P.seen[eng].get(e2, 0):
                    P.seen[eng][e2] = c
                    waits.append((P.sem[e2], c * P._mult(e2)))
            if waits:
                P.ops[eng].append((waits, None, None, 0))

    def range_reduce(self, x, tmp, r, w):
        bufs = list(set(list(r) + list(w)))
        self.ts("dve", tmp, x, 1.0 / TWO_PI, MAGIC, ALU.mult, ALU.add, bufs, bufs)
        self.ts("dve", tmp, tmp, -MAGIC, None, ALU.add, None, bufs, bufs)
        self.stt(x, tmp, -TWO_PI, x, ALU.mult, ALU.add, bufs, bufs)
        self.ts("dve", tmp, x, PI, -TWO_PI, ALU.is_gt, ALU.mult, bufs, bufs)
        self.tt("dve", x, x, tmp, ALU.add, bufs, bufs)
        self.ts("dve", tmp, x, -PI, TWO_PI, ALU.is_lt, ALU.mult, bufs, bufs)
        self.tt("dve", x, x, tmp, ALU.add, bufs, bufs)
        self.ts("dve", x, x, -PI, PI, ALU.max, ALU.min, bufs, bufs)

    def setup(self):
        nc = self.nc
        A = {}
        self.A = A
        A["x"] = self.din("x", [L, D])
        A["c"] = self.din("c", [1, D])
        A["ctx"] = self.din("ctx", [LC, D])
        A["c_ctx"] = self.din("c_ctx", [1, D])
        A["ada_w"] = self.din("ada_w", [DEPTH, D, 6 * D])
        A["ada_b"] = self.din("ada_b", [DEPTH, 6 * D])
        A["w_in"] = self.din("w_in", [DEPTH, D, IN_COLS])
        A["rwkv_mu"] = self.din("rwkv_mu", [DEPTH, 1024])
        A["rwkv_w0"] = self.din("rwkv_w0", [DEPTH, 2, 256])
        A["rwkv_w_up"] = self.din("rwkv_w_up", [DEPTH, 2, 64, 256])
        A["rwkv_a0"] = self.din("rwkv_a0", [DEPTH, 2, 256])
        A["rwkv_a_up"] = self.din("rwkv_a_up", [DEPTH, 2, 64, 256])
        A["rwkv_g_up"] = self.din("rwkv_g_up", [DEPTH, 128, 256])
        for n in ("rwkv_k_k", "rwkv_k_a", "rwkv_r_k", "rwkv_lnx_g", "rwkv_lnx_b", "hyena_skip"):
            A[n] = self.din(n, [DEPTH, 256])
        A["hyena_conv"] = self.din("hyena_conv", [DEPTH, 3, 768])
        A["hyena_w1"] = self.din("hyena_w1", [DEPTH, 33, 64])
        A["hyena_b1"] = self.din("hyena_b1", [DEPTH, 64])
        A["hyena_freq1"] = self.din("hyena_freq1", [DEPTH, 64])
        A["hyena_w2"] = self.din("hyena_w2", [DEPTH, 64, 64])
        A["hyena_b2"] = self.din("hyena_b2", [DEPTH, 64])
        A["hyena_freq2"] = self.din("hyena_freq2", [DEPTH, 64])
        A["hyena_w3"] = self.din("hyena_w3", [DEPTH, 64, 512])
        A["sconv_w"] = self.din("sconv_w", [DEPTH, 3, 256])
        A["attn_q_norm"] = self.din("attn_q_norm", [DEPTH, 64])
        A["attn_k_norm"] = self.din("attn_k_norm", [DEPTH, 64])
        A["w_branch"] = self.din("w_branch", [DEPTH, 4, 256, D])
        A["w_out"] = self.din("w_out", [DEPTH, D, D])
        for n in ("ln1_g", "ln1_b", "ln2_g", "ln2_b"):
            A[n] = self.din(n, [DEPTH, D])
        A["router_w"] = self.din("router_w", [D, NE])
        A["router_bias"] = self.din("router_bias", [1, NE])
        A["exp_w1"] = self.din("exp_w1", [DEPTH, NE, D, DE])
        A["exp_w3"] = self.din("exp_w3", [DEPTH, NE, D, DE])
        A["exp_w2"] = self.din("exp_w2", [DEPTH, NE, DE, D])
        A["rope_cs"] = self.din("rope_cs", [L, 64])
        A["hy_z"] = self.din("hy_z", [33, 2 * L])
        A["hy_zc"] = self.din("hy_zc", [33, 2 * LC])
        A["hy_t"] = self.din("hy_t", [1, 2 * L])
        A["hy_tc"] = self.din("hy_tc", [1, 2 * LC])
        A["hy_delta"] = self.din("hy_delta", [256, 1])
        A["out"] = self.dscr("out", [L, D], F32, out=True)
        A["xres"] = self.dscr("xres", [NTOK, D], F32)
        A["hT"] = self.dscr("hT", [128, 8 * NTP], BF16)
        A["hfT"] = self.dscr("hfT", [128, 8 * NTOK], BF16)
        A["ysT"] = self.dscr("ysT", [128, 8 * NTOK], BF16)
        A["rk_fm"] = self.dscr("rk_fm", [2 * 4 * 256, NTOK], F32)
        A["rk_tm"] = self.dscr("rk_tm", [2 * NTOK, 2 * 256], F32)
        A["rk_v"] = self.dscr("rk_v", [NTOK, 256], F32)
        A["rk_gc"] = self.dscr("rk_gc", [2 * 256, NTOK // 64], F32)
        A["rk_g"] = self.dscr("rk_g", [256, NTOK], F32)
        A["rk_bonus"] = self.dscr("rk_bonus", [256, NTOK], F32)
        A["rk_y"] = self.dscr("rk_y", [2 * NTOK, 256], F32)
        A["ewb1"] = self.dscr("ewb1", [NE * D, DE], BF16)
        A["ewb3"] = self.dscr("ewb3", [NE * D, DE], BF16)
        A["ewb2"] = self.dscr("ewb2", [NE * DE, D], BF16)
        A["hyG"] = self.dscr("hyG", [256, 2 * L], BF16)
        A["hyGc"] = self.dscr("hyGc", [256, 2 * LC], BF16)
        self.hT3 = A["hT"].rearrange("p (k n) -> p k n", k=8)
        self.hfT3 = A["hfT"].rearrange("p (k n) -> p k n", k=8)
        self.ysT3 = A["ysT"].rearrange("p (k n) -> p k n", k=8)

        self.ges = ExitStack()
        es = self.ges
        self.ident_b = self.sb(es, "ident_b", [128, 128], BF16); self.b_ident_b = Buf()
        self.ident_f = self.sb(es, "ident_f", [128, 128], F32); self.b_ident_f = Buf()
        self.flip_b = self.sb(es, "flip_b", [128, 128], BF16); self.b_flip_b = Buf()
        self.ones_f = self.sb(es, "ones_f", [128, 128], F32); self.b_ones_f = Buf()
        self.blk_f = self.sb(es, "blk_f", [128, 128], F32); self.b_blk_f = Buf()
        self.sel2 = self.sb(es, "sel2", [2, 2, 128], F32); self.b_sel2 = Buf()
        self.modT = self.sb(es, "modT", [128, 48, 2], F32); self.b_modT = Buf()
        self.gbc = self.sb(es, "gbc", [128, 4, D], F32); self.b_gbc = Buf()
        self.lnbc = self.sb(es, "lnbc", [128, 4, D], F32); self.b_lnbc = Buf()
        self.gates = self.sb(es, "gates", [128, NTILE, NE], F32); self.b_gates = Buf()
        self.rw32 = self.sb(es, "rw32", [128, 8, NE], F32); self.b_rw32 = Buf()
        self.rbias = self.sb(es, "rbias", [128, NE], F32); self.b_rbias = Buf()
        self.psf = []
        for i in range(6):
            t = nc.alloc_psum_tensor("psf%d" % i, [128, 512], F32)
            self.psf.append((t, Buf("psf%d" % i, excl=True)))
        self.psb = []
        for i in range(2):
            t = nc.alloc_psum_tensor("psb%d" % i, [128, 1024], BF16)
            self.psb.append((t, Buf("psb%d" % i, excl=True)))

        self.psb_f32 = [(t_[:].bitcast(F32), b_) for (t_, b_) in self.psb]
        G = "pool"
        tmpf = self.sb(es, "tmp_idf", [128, 128], F32); b_tmp = Buf()
        self.memset(G, self.ident_f[:], 0.0, [self.b_ident_f])
        self.op(G, lambda e: e.affine_select(out=self.ident_f[:], in_=self.ident_f[:], pattern=[[-1, 128]],
                                              compare_op=ALU.not_equal, fill=1.0, base=0, channel_multiplier=1),
                [self.b_ident_f], [self.b_ident_f])
        self.copy("dve", self.ident_b[:], self.ident_f[:], [self.b_ident_f], [self.b_ident_b])
        self.memset(G, tmpf[:], 0.0, [b_tmp])
        self.op(G, lambda e: e.affine_select(out=tmpf[:], in_=tmpf[:], pattern=[[1, 128]],
                                              compare_op=ALU.not_equal, fill=1.0, base=-127, channel_multiplier=1),
                [b_tmp], [b_tmp])
        self.copy("dve", self.flip_b[:], tmpf[:], [b_tmp], [self.b_flip_b])
        self.memset(G, self.ones_f[:], 1.0, [self.b_ones_f])
        self.memset(G, self.blk_f[:], 0.0, [self.b_blk_f])
        self.memset(G, self.blk_f[0:64, 0:64], 1.0, [self.b_blk_f])
        self.memset(G, self.blk_f[64:128, 64:128], 1.0, [self.b_blk_f])
        self.memset(G, self.sel2[:], 0.0, [self.b_sel2])
        self.op(G, lambda e: e.affine_select(out=self.sel2[:], in_=self.sel2[:], pattern=[[-1, 2], [0, 128]],
                                              compare_op=ALU.not_equal, fill=1.0, base=0, channel_multiplier=1),
                [self.b_sel2], [self.b_sel2])
        self.dma("sp", self.rw32[:], A["router_w"].rearrange("(k p) e -> p k e", p=128), [], [self.b_rw32])
        self.dma("sp", self.rbias[:], A["router_bias"][0:1, :].partition_broadcast(128), [], [self.b_rbias])
        bx = self.dbuf["xres"]
        self.dma("sp", A["xres"][0:LC, :], A["ctx"][:, :], [], [bx])
        self.dma("sp", A["xres"][LC:NTOK, :], A["x"][:, :], [], [bx])
        zt = self.sb(es, "zpad", [128, 8, 2], BF16); bz = Buf()
        self.memset(G, zt[:], 0.0, [bz])
        bh = self.dbuf["hT"]
        self.dma("sp", self.hT3[:, :, 0:1], zt[:, :, 0:1], [bz], [bh], allow_slow_non_contiguous=True)
        self.dma("sp", self.hT3[:, :, 257:259], zt[:, :, 0:2], [bz], [bh], allow_slow_non_contiguous=True)
        self.dma("sp", self.hT3[:, :, NTP - 1:NTP], zt[:, :, 0:1], [bz], [bh], allow_slow_non_contiguous=True)

    def phase_mod(self, l):
        A = self.A
        with ExitStack() as es:
            self.modrow = self.sb(es, "modrow", [2, 6 * D], F32); self.b_modrow = Buf()
            cT = self.sb(es, "cT", [128, 8, 2], F32); b_cT = Buf()
            sT = self.sb(es, "sT", [128, 8, 2], F32); b_sT = Buf()
            brow = self.sb(es, "brow", [1, 6 * D], F32); b_brow = Buf()
            wch = [self.sb(es, "adaw%d" % i, [128, 8, 512], F32) for i in range(2)]
            b_wch = [Buf(), Buf()]
            self.dma("sp", cT[:, :, 0], A["c"].rearrange("o (k p) -> p (o k)", p=128), [], [b_cT],
                     allow_slow_non_contiguous=True)
            self.dma("sp", cT[:, :, 1], A["c_ctx"].rearrange("o (k p) -> p (o k)", p=128), [], [b_cT],
                     allow_slow_non_contiguous=True)
            self.dma("sp", brow[:], A["ada_b"][l:l + 1, :], [], [b_brow])
            self.act(sT[:], cT[:], AF.Silu, [b_cT], [b_sT])
            for j in range(12):
                w, bw = wch[j % 2], b_wch[j % 2]
                self.dma("sp", w[:], A["ada_w"][l, :, j * 512:(j + 1) * 512].rearrange("(k p) n -> p k n", p=128),
                         [], [bw])
                ps, bp = self.psf[j % 2]
                for k in range(8):
                    self.mm(ps[0:2, :], sT[:, k, :], w[:, k, :], k == 0, False, [b_sT, bw], [bp], inc=False)
                self.mm(ps[0:2, :], self.ones_f[0:1, 0:2], brow[0:1, j * 512:(j + 1) * 512], False, True,
                        [self.b_ones_f, b_brow], [bp])
                self.copy("act", self.modrow[0:2, j * 512:(j + 1) * 512], ps[0:2, :], [bp], [self.b_modrow])
            for j in range(48):
                ps, bp = self.psf[2 + j % 2]
                self.tr(ps[:, 0:2], self.modrow[0:2, j * 128:(j + 1) * 128], self.ident_f[0:2, 0:2],
                        [self.b_modrow, self.b_ident_f], [bp])
                self.copy("dve", self.modT[:, j, :], ps[:, 0:2], [bp], [self.b_modT])
            for base in (8, 32):
                self.ts("dve", self.modT[:, base:base + 8, :], self.modT[:, base:base + 8, :], 1.0, None,
                        ALU.add, None, [self.b_modT], [self.b_modT])
            i = 0
            for gi, col0 in ((0, 2 * D), (2, 5 * D)):
                for r_ in range(2):
                    for hh in range(2):
                        ps, bp = self.psf[4 + i % 2]
                        i += 1
                        self.mm(ps[:, :], self.sel2[0:2, r_, :], self.modrow[0:2, col0 + hh * 512:col0 + (hh + 1) * 512],
                                True, True, [self.b_sel2, self.b_modrow], [bp])
                        self.copy("act", self.gbc[:, gi + r_, hh * 512:(hh + 1) * 512], ps[:, :], [bp], [self.b_gbc])
            for i_, n in enumerate(("ln1_g", "ln1_b", "ln2_g", "ln2_b")):
                self.dma("sp", self.lnbc[:, i_, :], A[n][l:l + 1, :].partition_broadcast(128), [], [self.b_lnbc])
            self.barrier()

    def ln_stats(self, xt, bx, st, mv, rs, bs):
        for i in range(2):
            self.op("dve", lambda e, i=i: e.bn_stats(out=st[:, i, :], in_=xt[:, i * 512:(i + 1) * 512]), [bx], [bs])
        self.op("dve", lambda e: e.bn_aggr(out=mv[:], in_=st[:].rearrange("p a b -> p (a b)")), [bs], [bs])
        self.act(rs[:], mv[:, 1:2], AF.Sqrt, [bs], [bs], bias=LN_EPS, scale=1.0)
        self.op("dve", lambda e: e.reciprocal(out=rs[:], in_=rs[:]), [bs], [bs])

    def phase_h(self, l, tiles):
        A = self.A
        bxres = self.dbuf["xres"]
        bhT = self.dbuf["hT"]
        with ExitStack() as es:
            NB = 3
            xt = [self.sb(es, "h_x%d" % i, [128, D], F32) for i in range(NB)]
            bxt = [Buf() for _ in range(NB)]
            xn = [self.sb(es, "h_xn%d" % i, [128, D], BF16) for i in range(NB)]
            bxn = [Buf() for _ in range(NB)]
            st = [self.sb(es, "h_st%d" % i, [128, 2, 6], F32) for i in range(NB)]
            mv = [self.sb(es, "h_mv%d" % i, [128, 2], F32) for i in range(NB)]
            rs = [self.sb(es, "h_rs%d" % i, [128, 1], F32) for i in range(NB)]
            bs = [Buf() for _ in range(NB)]
            ho = [self.sb(es, "h_o%d" % i, [128, 8, 128], BF16) for i in range(NB)]
            bho = [Buf() for _ in range(NB)]
            def tile_gen(n, ti):
                s = n % NB
                r_ = 1 if ti < 2 else 0
                col = tok_col(ti * 128)
                self.dma("sp", xt[s][:], A["xres"][ti * 128:(ti + 1) * 128, :], [bxres], [bxt[s]])
                yield
                self.ln_stats(xt[s], bxt[s], st[s], mv[s], rs[s], bs[s])
                yield
                self.ts("dve", xn[s][:], xt[s][:], mv[s][:, 0:1], rs[s][:, 0:1], ALU.subtract, ALU.mult,
                        [bxt[s], bs[s]], [bxn[s]])
                yield True
                pt, bpt = self.psb[n % 2]
                for k in range(8):
                    self.tr(pt[:, k * 128:(k + 1) * 128], xn[s][:, k * 128:(k + 1) * 128], self.ident_b[:],
                            [bxn[s], self.b_ident_b], [bpt], inc=(k == 7))
                yield
                for k in range(8):
                    self.act(ho[s][:, k, :], pt[:, k * 128:(k + 1) * 128], AF.Identity, [bpt, self.b_modT], [bho[s]],
                             bias=self.modT[:, k, r_:r_ + 1], scale=self.modT[:, 8 + k, r_:r_ + 1])
                yield
                self.dma("sp", self.hT3[:, :, col:col + 128], ho[s][:], [bho[s]], [bhT])
                yield

            run_pipeline((tile_gen(n, ti) for n, ti in enumerate(tiles)), depth=3)
            self.barrier()


def const_tables():
    f32 = np.float32
    t = np.arange(L)
    rows = (t // 64).astype(f32)
    cols = (t % 64).astype(f32)
    inv = (np.float32(10000.0) ** (-np.arange(0, 32, 2, dtype=f32) / np.float32(32))).astype(f32)
    ang = np.concatenate([rows[:, None] * inv, cols[:, None] * inv], -1).astype(f32)
    rope_cs = np.concatenate([np.cos(ang), np.sin(ang)], -1).astype(f32)

    def ztab(Ls):
        tt = np.linspace(0.0, 1.0, Ls, dtype=f32)
        wpos = (f32(2.0 * math.pi) * np.arange(Ls, dtype=f32) / f32(Ls)).astype(f32)
        f = np.linspace(1e-4, 15, 16, dtype=f32)[None, :]
        z = np.concatenate([tt[:, None], np.cos(f * wpos[:, None]), -np.sin(f * wpos[:, None])], -1).astype(f32)
        pos = np.abs(np.arange(2 * Ls) - Ls)
        pos = np.minimum(pos, Ls - 1)
        return np.ascontiguousarray(z[pos].T), np.ascontiguousarray(tt[pos][None, :])
    hy_z, hy_t = ztab(L)
    hy_zc, hy_tc = ztab(LC)
    max_decay = math.log(1e-2) / 0.3
    min_decay = math.log(1e-2) / 1.5
    delta = np.abs(np.linspace(min_decay, max_decay, 256, dtype=f32)).astype(f32)[:, None]
    return dict(rope_cs=rope_cs, hy_z=hy_z, hy_t=hy_t, hy_zc=hy_zc, hy_tc=hy_tc, hy_delta=delta)


def make_in_maps(inputs, cores):
    f32 = np.float32
    shared = {}
    for k, v in inputs.items():
        if k in ("x", "c", "ctx"):
            continue
        v = np.ascontiguousarray(np.asarray(v, dtype=f32))
        if k in ("c_ctx", "router_bias"):
            v = v.reshape(1, -1)
        shared[k] = v
    shared.update(const_tables())
    maps = []
    for b in cores:
        m = dict(shared)
        m["x"] = np.ascontiguousarray(np.asarray(inputs["x"][b], dtype=f32))
        m["c"] = np.ascontiguousarray(np.asarray(inputs["c"][b], dtype=f32)).reshape(1, -1)
        m["ctx"] = np.ascontiguousarray(np.asarray(inputs["ctx"][b], dtype=f32))
        maps.append(m)
    return maps


def run_pipeline(gens, depth=2):
    it = iter(gens)
    active = []
    ready = True
    done = False
    while True:
        if ready and not done and len(active) < depth:
            try:
                active.append(next(it))
                ready = False
            except StopIteration:
                done = True
        if not active:
            if done:
                break
            ready = True
            continue
        for g_ in list(active):
            try:
                v = next(g_)
                if v is True and g_ is active[-1]:
                    ready = True
            except StopIteration:
                if g_ is active[-1]:
                    ready = True
                active.remove(g_)


def _seqs(ctx_out):
    s = [(LAT0, LC, L)]
    if ctx_out:
        s = [(CTX0, 0, LC)] + s
    return s


def load_hT(self, es):
    hT = self.sb(es, "hT_sb", [128, 8, NTP], BF16)
    b = Buf("hT_sb")
    for k in range(8):
        self.dma("sp", hT[:, k, :], self.hT3[:, k, :], [self.dbuf["hT"]], [b])
    return hT, b


def load_w_in(self, es, l, name, col0, ncols):
    w = self.sb(es, name, [128, 8, ncols], BF16)
    b = Buf(name)
    src = self.A["w_in"][l, :, col0:col0 + ncols].rearrange("(k p) n -> p k n", p=128)
    step = 1024
    for k in range(8):
        for c0 in range(0, ncols, step):
            c1 = min(ncols, c0 + step)
            self.dma("pool", w[:, k, c0:c1], src[:, k, c0:c1], [], [b])
    return w, b


K.load_hT = load_hT
K.load_w_in = load_w_in


def phase_sconv(self, l, ctx_out):
    A = self.A
    with ExitStack() as es:
        hT, bhT = self.load_hT(es)
        w, bw = self.load_w_in(es, l, "sc_w", OFF_SCONV, 768)
        taps = self.sb(es, "sc_taps", [128, 2, 3], F32); btaps = Buf()
        for cc in range(2):
            self.dma("sp", taps[:, cc, :], A["sconv_w"][l, :, cc * 128:(cc + 1) * 128].rearrange("j p -> p j"),
                     [], [btaps], allow_slow_non_contiguous=True)
        m = self.sb(es, "sc_m", [128, L + 2], F32); bm = Buf()
        bg = self.sb(es, "sc_bg", [128, L], F32); bbg = Buf()
        o = self.sb(es, "sc_o", [128, L], F32); bo = Buf()
        res = self.sb(es, "sc_res", [128, L], BF16); bres = Buf()
        cg = [self.sb(es, "sc_cg%d" % i, [128, 512], F32) for i in range(2)]
        bcg = [Buf(), Buf()]
        it = 0
        for cc in range(2):
            for (col0, tok0, n) in _seqs(ctx_out):
                self.memset("pool", m[:, 0:1], 0.0, [bm])
                self.memset("pool", m[:, n + 1:n + 2], 0.0, [bm])
                for t0 in range(0, n, 512):
                    nn = min(512, n - t0)
                    pss = []
                    for pi, cb in enumerate((256, 512, 0)):
                        ps, bp = self.psf[(it * 3 + pi) % 6]
                        for k in range(8):
                            self.mm(ps[:, 0:nn], w[:, k, cb + cc * 128:cb + (cc + 1) * 128],
                                    hT[:, k, col0 + t0:col0 + t0 + nn], k == 0, k == 7, [bw, bhT], [bp])
                        pss.append((ps, bp))
                    c_, bc_ = cg[it % 2], bcg[it % 2]
                    self.copy("act", c_[:, 0:nn], pss[0][0][:, 0:nn], [pss[0][1]], [bc_])
                    self.tt("dve", m[:, 1 + t0:1 + t0 + nn], pss[1][0][:, 0:nn], c_[:, 0:nn], ALU.mult,
                            [pss[1][1], bc_], [bm])
                    self.copy("act", bg[:, t0:t0 + nn], pss[2][0][:, 0:nn], [pss[2][1]], [bbg])
                    it += 1
                self.ts("dve", o[:, 0:n], m[:, 0:n], taps[:, cc, 0:1], None, ALU.mult, None, [bm, btaps], [bo])
                self.stt(o[:, 0:n], m[:, 1:n + 1], taps[:, cc, 1:2], o[:, 0:n], ALU.mult, ALU.add, [bm, btaps, bo], [bo])
                self.stt(o[:, 0:n], m[:, 2:n + 2], taps[:, cc, 2:3], o[:, 0:n], ALU.mult, ALU.add, [bm, btaps, bo], [bo])
                self.tt("dve", res[:, 0:n], o[:, 0:n], bg[:, 0:n], ALU.mult, [bo, bbg], [bres])
                self.dma("sp", self.ysT3[:, 4 + cc, tok0:tok0 + n], res[:, 0:n], [bres], [self.dbuf["ysT"]])
        self.barrier()


K.phase_sconv = phase_sconv


def merge_load(self, es, l):
    A = self.A
    wg, bwg = self.load_w_in(es, l, "mg_wg", OFF_GATE, 4096)
    wb = self.sb(es, "mg_wb", [128, 8, D], BF16); bwb = Buf()
    wo = self.sb(es, "mg_wo", [128, 8, D], BF16); bwo = Buf()
    for n4 in range(4):
        for j in range(2):
            self.dma("pool", wb[:, 2 * n4 + j, :], A["w_branch"][l, n4, j * 128:(j + 1) * 128, :], [], [bwb])
    for k in range(8):
        self.dma("pool", wo[:, k, :], A["w_out"][l, k * 128:(k + 1) * 128, :], [], [bwo])
    return wg, bwg, wb, bwb, wo, bwo


K.merge_load = merge_load


def phase_merge(self, l, tiles, pre=None):
    A = self.A
    with ExitStack() as es:
        if pre is None:
            pre = self.merge_load(es, l)
        wg, bwg, wb, bwb, wo, bwo = pre
        NB = 2
        ys = [self.sb(es, "mg_ys%d" % i, [128, 8, 128], BF16) for i in range(NB)]; bys = [Buf() for _ in range(NB)]
        ht = [self.sb(es, "mg_ht%d" % i, [128, 8, 128], BF16) for i in range(NB)]; bht = [Buf() for _ in range(NB)]
        xt = [self.sb(es, "mg_x%d" % i, [128, D], F32) for i in range(NB)]; bxt = [Buf() for _ in range(NB)]
        gs = [self.sb(es, "mg_gs%d" % i, [128, D], F32) for i in range(2)]; bgs = [Buf(), Buf()]
        mg = self.sb(es, "mg_m", [128, D], F32); bmg = Buf()
        mgb = self.sb(es, "mg_mb", [128, D], BF16); bmgb = Buf()
        mT = self.sb(es, "mg_mT", [128, 8, 128], BF16); bmT = Buf()
        t1 = self.sb(es, "mg_t1", [128, D], F32); bt1 = Buf()
        x1 = self.sb(es, "mg_x1", [128, D], F32); bx1 = Buf()
        xn = self.sb(es, "mg_xn", [128, D], F32); bxn = Buf()
        hf32 = self.sb(es, "mg_hf32", [128, 8, 128], F32); bhf32 = Buf()
        hfb = self.sb(es, "mg_hfb", [128, 8, 128], BF16); bhfb = Buf()
        st = self.sb(es, "mg_st", [128, 2, 6], F32); mv = self.sb(es, "mg_mv", [128, 2], F32)
        rs = self.sb(es, "mg_rs", [128, 1], F32); bs = Buf()
        rt = self.sb(es, "mg_rt", [128, 8, NE], F32); brt = Buf()
        bxres = self.dbuf["xres"]
        def tile_gen(n, ti):
            s = n % NB
            r_ = 1 if ti < 2 else 0
            col = tok_col(ti * 128)
            tk = ti * 128
            self.dma("sp", ys[s][:], self.ysT3[:, :, tk:tk + 128], [self.dbuf["ysT"]], [bys[s]])
            self.dma("sp", ht[s][:], self.hT3[:, :, col:col + 128], [self.dbuf["hT"]], [bht[s]])
            self.dma("sp", xt[s][:], A["xres"][tk:tk + 128, :], [bxres], [bxt[s]])
            yield
            for n4 in range(4):
                g_, bg_ = gs[n4 % 2], bgs[n4 % 2]
                for hh in range(2):
                    pg, bpg = self.psf[hh]
                    for k in range(8):
                        self.mm(pg[:, :], ht[s][:, k, :], wg[:, k, n4 * 1024 + hh * 512:n4 * 1024 + (hh + 1) * 512],
                                k == 0, k == 7, [bht[s], bwg], [bpg])
                    self.act(g_[:, hh * 512:(hh + 1) * 512], pg[:, :], AF.Sigmoid, [bpg], [bg_])
                for hh in range(2):
                    pz, bpz = self.psf[2 + hh]
                    for j in range(2):
                        self.mm(pz[:, :], ys[s][:, 2 * n4 + j, :], wb[:, 2 * n4 + j, hh * 512:(hh + 1) * 512],
                                j == 0, j == 1, [bys[s], bwb], [bpz])
                    sl = slice(hh * 512, (hh + 1) * 512)
                    if n4 == 0:
                        self.tt("dve", mg[:, sl], pz[:, :], g_[:, sl], ALU.mult, [bpz, bg_], [bmg])
                    else:
                        self.tt("dve", g_[:, sl], pz[:, :], g_[:, sl], ALU.mult, [bpz, bg_], [bg_])
                        if n4 < 3:
                            self.tt("pool", mg[:, sl], mg[:, sl], g_[:, sl], ALU.add, [bmg, bg_], [bmg])
                        else:
                            self.tt("pool", mgb[:, sl], mg[:, sl], g_[:, sl], ALU.add, [bmg, bg_], [bmgb])
                yield
            pt, bpt = self.psb[0]
            for k in range(8):
                self.tr(pt[:, k * 128:(k + 1) * 128], mgb[:, k * 128:(k + 1) * 128], self.ident_b[:],
                        [bmgb, self.b_ident_b], [bpt], inc=(k == 7))
            yield True
            self.copy("dve", mT[:].rearrange("p a b -> p (a b)"), pt[:, :], [bpt], [bmT])
            yield
            for hh in range(2):
                py, bpy = self.psf[4 + hh]
                sl = slice(hh * 512, (hh + 1) * 512)
                for k in range(8):
                    self.mm(py[:, :], mT[:, k, :], wo[:, k, sl], k == 0, k == 7, [bmT, bwo], [bpy])
                self.tt("dve", t1[:, sl], py[:, :], self.gbc[:, 0 + r_, sl], ALU.mult, [bpy, self.b_gbc], [bt1])
            yield
            self.stt(t1[:], xt[s][:], ALPHA, t1[:], ALU.mult, ALU.add, [bxt[s], bt1], [bt1])
            self.ln_stats(t1, bt1, st, mv, rs, bs)
            yield
            self.ts("dve", xn[:], t1[:], mv[:, 0:1], rs[:, 0:1], ALU.subtract, ALU.mult, [bt1, bs], [bxn])
            self.tt("pool", xn[:], xn[:], self.lnbc[:, 0, :], ALU.mult, [bxn, self.b_lnbc], [bxn])
            self.tt("pool", x1[:], xn[:], self.lnbc[:, 1, :], ALU.add, [bxn, self.b_lnbc], [bx1])
            self.dma("sp", A["xres"][tk:tk + 128, :], x1[:], [bx1], [bxres])
            yield
            self.ln_stats(x1, bx1, st, mv, rs, bs)
            self.ts("dve", xn[:], x1[:], mv[:, 0:1], rs[:, 0:1], ALU.subtract, ALU.mult, [bx1, bs], [bxn])
            yield
            for half in range(2):
                pf, bpf = self.psf[4 + half]
                for kk in range(4):
                    k = half * 4 + kk
                    self.tr(pf[:, kk * 128:(kk + 1) * 128], xn[:, k * 128:(k + 1) * 128], self.ident_f[:],
                            [bxn, self.b_ident_f], [bpf], inc=(kk == 3))
                for kk in range(4):
                    k = half * 4 + kk
                    self.act(hf32[:, k, :], pf[:, kk * 128:(kk + 1) * 128], AF.Identity, [bpf, self.b_modT], [bhf32],
                             bias=self.modT[:, 24 + k, r_:r_ + 1], scale=self.modT[:, 32 + k, r_:r_ + 1])
            yield
            self.copy("dve", hfb[:], hf32[:], [bhf32], [bhfb])
            self.dma("sp", self.hfT3[:, :, tk:tk + 128], hfb[:], [bhfb], [self.dbuf["hfT"]])
            pr, bpr = self.psb[1][0][:].bitcast(F32), self.psb[1][1]
            for k in range(8):
                self.mm(pr[:, 0:NE], hf32[:, k, :], self.rw32[:, k, :], k == 0, k == 7, [bhf32, self.b_rw32], [bpr])
            s_ = rt[:, 0, :]; sel = rt[:, 1, :]; tmp = rt[:, 2, :]; sel2_ = rt[:, 3, :]; msk = rt[:, 4, :]
            m1 = rt[:, 5, 0:4]; m2 = rt[:, 5, 4:8]; grp = rt[:, 5, 8:12]; gmx = rt[:, 5, 12:13]; oh = rt[:, 6, 0:4]
            den = rt[:, 6, 4:5]
            B = [brt]
            v3 = lambda a: a.rearrange("p (g e) -> p g e", g=4)
            b4 = lambda a: a.unsqueeze(2).to_broadcast([128, 4, 4])
            yield
            self.act(s_, pr[:, 0:NE], AF.Sigmoid, [bpr], B)
            self.tt("dve", sel, s_, self.rbias[:], ALU.add, B + [self.b_rbias], B)
            self.op("dve", lambda e: e.tensor_reduce(out=m1, in_=v3(sel), axis=AX.X, op=ALU.max), B, B)
            self.tt("dve", v3(tmp), v3(sel), b4(m1), ALU.is_equal, B, B)
            self.stt(sel2_, tmp, -1e30, sel, ALU.mult, ALU.add, B, B)
            self.op("dve", lambda e: e.tensor_reduce(out=m2, in_=v3(sel2_), axis=AX.X, op=ALU.max), B, B)
            self.tt("dve", grp, m1, m2, ALU.add, B, B)
            self.op("dve", lambda e: e.tensor_reduce(out=gmx, in_=grp, axis=AX.X, op=ALU.max), B, B)
            self.ts("dve", oh, grp, gmx, None, ALU.is_equal, None, B, B)
            self.tt("dve", v3(msk), v3(sel), b4(m2), ALU.is_ge, B, B)
            self.tt("dve", v3(msk), v3(msk), b4(oh), ALU.mult, B, B)
            self.tt("dve", tmp, msk, s_, ALU.mult, B, B)
            self.op("dve", lambda e: e.tensor_reduce(out=den, in_=tmp, axis=AX.X, op=ALU.add), B, B)
            self.op("dve", lambda e: e.reciprocal(out=den, in_=den), B, B)
            self.ts("dve", self.gates[:, ti, :], tmp, den, None, ALU.mult, None, B, [self.b_gates])
            yield

        run_pipeline((tile_gen(n, ti) for n, ti in enumerate(tiles)), depth=2)
        self.barrier()


K.phase_merge = phase_merge


def phase_moe(self, l, tiles, last):
    A = self.A
    nparts = 3
    per = (len(tiles) + nparts - 1) // nparts
    parts = [tiles[i:i + per] for i in range(0, len(tiles), per)]
    bxres = self.dbuf["xres"]
    with ExitStack() as es:
        hf = self.sb(es, "moe_hf", [128, 8, per * 128], BF16); bhf = Buf()
        acc = self.sb(es, "moe_acc", [128, per, D], F32); bacc = Buf()
        w1s = [self.sb(es, "moe_w1_%d" % i, [128, 8, DE], BF16) for i in range(2)]
        w3s = [self.sb(es, "moe_w3_%d" % i, [128, 8, DE], BF16) for i in range(2)]
        w2s = [self.sb(es, "moe_w2_%d" % i, [128, 4, D], BF16) for i in range(2)]
        bw = [Buf(), Buf()]
        sa = [self.sb(es, "moe_sa%d" % i, [128, 512], F32) for i in range(2)]; bsa = [Buf(), Buf()]
        aT = [self.sb(es, "moe_aT%d" % i, [128, 4, 512], BF16) for i in range(2)]; baT = [Buf(), Buf()]
        xt = [self.sb(es, "moe_x%d" % i, [128, D], F32) for i in range(2)]; bxt = [Buf(), Buf()]
        t1 = self.sb(es, "moe_t1", [128, D], F32); bt1 = Buf()
        xo = [self.sb(es, "moe_xo%d" % i, [128, D], F32) for i in range(2)]; bxo = [Buf(), Buf()]
        st = self.sb(es, "moe_st", [128, 2, 6], F32); mv = self.sb(es, "moe_mv", [128, 2], F32)
        rs = self.sb(es, "moe_rs", [128, 1], F32); bs = Buf()
        wi = 0
        ci = 0
        oi = 0
        pobanks = [self.psf[4], self.psf[5], (self.psb[0][0][:].bitcast(F32), self.psb[0][1]),
                   (self.psb[1][0][:].bitcast(F32), self.psb[1][1])]
        for part in parts:
            nt = len(part)
            tok0 = part[0] * 128
            ntok = nt * 128
            for k in range(8):
                self.dma("sp", hf[:, k, 0:ntok], self.hfT3[:, k, tok0:tok0 + ntok], [self.dbuf["hfT"]], [bhf])
            for e_ in range(NE):
                sl_ = wi % 2
                wi += 1
                self.dma("sp", w1s[sl_][:], A["ewb1"][e_ * D:(e_ + 1) * D, :].rearrange("(k p) n -> p k n", p=128),
                         [self.dbuf["ewb1"]], [bw[sl_]])
                self.dma("sp", w3s[sl_][:], A["ewb3"][e_ * D:(e_ + 1) * D, :].rearrange("(k p) n -> p k n", p=128),
                         [self.dbuf["ewb3"]], [bw[sl_]])
                self.dma("sp", w2s[sl_][:], A["ewb2"][e_ * DE:(e_ + 1) * DE, :].rearrange("(k p) n -> p k n", p=128),
                         [self.dbuf["ewb2"]], [bw[sl_]])
                for t0 in range(0, ntok, 512):
                    nn = min(512, ntok - t0)
                    a_, ba_ = aT[ci % 2], baT[ci % 2]
                    ci += 1
                    for f in range(4):
                        pa, bpa = self.psf[(2 * f) % 4]
                        pb, bpb = self.psf[(2 * f + 1) % 4]
                        for k in range(8):
                            self.mm(pa[:, 0:nn], w1s[sl_][:, k, f * 128:(f + 1) * 128], hf[:, k, t0:t0 + nn],
                                    k == 0, k == 7, [bw[sl_], bhf], [bpa])
                        for k in range(8):
                            self.mm(pb[:, 0:nn], w3s[sl_][:, k, f * 128:(f + 1) * 128], hf[:, k, t0:t0 + nn],
                                    k == 0, k == 7, [bw[sl_], bhf], [bpb])
                        s_, bs_ = sa[f % 2], bsa[f % 2]
                        self.act(s_[:, 0:nn], pa[:, 0:nn], AF.Silu, [bpa], [bs_])
                        self.tt("dve", a_[:, f, 0:nn], pb[:, 0:nn], s_[:, 0:nn], ALU.mult, [bpb, bs_], [ba_])
                    for tt_ in range(nn // 128):
                        j = (t0 + tt_ * 128) // 128
                        ti = part[j]
                        for hh in range(2):
                            oi += 1
                            po, bpo = pobanks[oi % 4]
                            sl = slice(hh * 512, (hh + 1) * 512)
                            for f in range(4):
                                self.mm(po[:, :], a_[:, f, tt_ * 128:(tt_ + 1) * 128], w2s[sl_][:, f, sl],
                                        f == 0, f == 3, [ba_, bw[sl_]], [bpo])
                            gcol = self.gates[:, ti, e_:e_ + 1]
                            if e_ == 0:
                                self.ts("dve", acc[:, j, sl], po[:, :], gcol, None, ALU.mult, None,
                                        [bpo, self.b_gates], [bacc])
                            else:
                                self.stt(acc[:, j, sl], po[:, :], gcol, acc[:, j, sl], ALU.mult, ALU.add,
                                         [bpo, self.b_gates, bacc], [bacc])
            for j, ti in enumerate(part):
                s = j % 2
                r_ = 1 if ti < 2 else 0
                tk = ti * 128
                self.dma("sp", xt[s][:], A["xres"][tk:tk + 128, :], [bxres], [bxt[s]])
                self.tt("pool", t1[:], acc[:, j, :], self.gbc[:, 2 + r_, :], ALU.mult, [bacc, self.b_gbc], [bt1])
                self.stt(t1[:], xt[s][:], ALPHA, t1[:], ALU.mult, ALU.add, [bxt[s], bt1], [bt1])
                self.ln_stats(t1, bt1, st, mv, rs, bs)
                self.ts("dve", t1[:], t1[:], mv[:, 0:1], rs[:, 0:1], ALU.subtract, ALU.mult, [bt1, bs], [bt1])
                self.tt("pool", t1[:], t1[:], self.lnbc[:, 2, :], ALU.mult, [bt1, self.b_lnbc], [bt1])
                self.tt("pool", xo[s][:], t1[:], self.lnbc[:, 3, :], ALU.add, [bt1, self.b_lnbc], [bxo[s]])
                if last:
                    self.dma("sp", A["out"][tk - LC:tk - LC + 128, :], xo[s][:], [bxo[s]], [self.dbuf["out"]])
                else:
                    self.dma("sp", A["xres"][tk:tk + 128, :], xo[s][:], [bxo[s]], [bxres])
        self.barrier()


K.phase_moe = phase_moe


def precast_experts(self, l):
    A = self.A
    for e_ in range(NE):
        self.dma("pool", A["ewb1"][e_ * D:(e_ + 1) * D, :], A["exp_w1"][l, e_, :, :], [], [self.dbuf["ewb1"]])
        self.dma("pool", A["ewb3"][e_ * D:(e_ + 1) * D, :], A["exp_w3"][l, e_, :, :], [], [self.dbuf["ewb3"]])
        self.dma("pool", A["ewb2"][e_ * DE:(e_ + 1) * DE, :], A["exp_w2"][l, e_, :, :], [], [self.dbuf["ewb2"]])


K.precast_experts = precast_experts


def phase_attn(self, l, ctx_out):
    A = self.A
    with ExitStack() as es:
        w, bw = self.load_w_in(es, l, "at_w", OFF_ATTN, 512)
        hts = [self.sb(es, "at_ht%d" % i, [128, 8, 128], BF16) for i in range(3)]
        bhts = [Buf() for _ in range(3)]
        qT = self.sb(es, "at_qT", [64, 4, NTOK], BF16); bqT = Buf()
        kT = self.sb(es, "at_kT", [64, 2, NTOK], BF16); bkT = Buf()
        va = self.sb(es, "at_va", [128, NTILE, 2, 66], BF16); bva = Buf()
        gqk = self.sb(es, "at_g", [128, 6, 64], F32); bg = Buf()
        cs = self.sb(es, "at_cs", [128, 32, 64], F32); bcs = Buf()
        for h in range(6):
            src = A["attn_q_norm"] if h < 4 else A["attn_k_norm"]
            self.dma("sp", gqk[:, h, :], src[l:l + 1, :].partition_broadcast(128), [], [bg])
        self.dma("sp", cs[:], A["rope_cs"].rearrange("(j p) c -> p j c", p=128), [], [bcs])
        self.memset("pool", va[:, :, :, 64:66], 1.0, [bva])
        NB = 2
        sq = [self.sb(es, "at_sq%d" % i, [128, 6, 64], F32) for i in range(NB)]
        qn = [self.sb(es, "at_qn%d" % i, [128, 6, 64], F32) for i in range(NB)]
        t_a = [self.sb(es, "at_ta%d" % i, [128, 6, 32], F32) for i in range(NB)]
        t_b = [self.sb(es, "at_tb%d" % i, [128, 6, 32], F32) for i in range(NB)]
        qr = [self.sb(es, "at_qr%d" % i, [128, 6, 64], BF16) for i in range(NB)]
        ss = [self.sb(es, "at_ss%d" % i, [128, 8], F32) for i in range(NB)]
        bt = [Buf() for _ in range(NB)]
        bqr = [Buf() for _ in range(NB)]
        tiles = list(range(NTILE))

        def tile_gen(n, ti):
            s = n % NB
            col = tok_col(ti * 128)
            tk = ti * 128
            ps, bp = self.psf[n % 2]
            ht_, bht_ = hts[n % 3], bhts[n % 3]
            self.dma("sp", ht_[:], self.hT3[:, :, col:col + 128], [self.dbuf["hT"]], [bht_])
            for k in range(8):
                self.mm(ps[:, :], ht_[:, k, :], w[:, k, :], k == 0, k == 7, [bht_, bw], [bp])
            yield
            B = [bt[s]]
            p3 = ps[:, 0:384].rearrange("p (h d) -> p h d", h=6)
            self.copy("dve", va[:, ti, :, 0:64], ps[:, 384:512].rearrange("p (g d) -> p g d", g=2), [bp], [bva])
            self.copy("act", qn[s][:].rearrange("p h d -> p (h d)"), ps[:, 0:384], [bp], B)
            self.tt("dve", sq[s][:], qn[s][:], qn[s][:], ALU.mult, B, B)
            yield
            self.op("dve", lambda e, s=s: e.tensor_reduce(out=ss[s][:, 0:6], in_=sq[s][:], axis=AX.X, op=ALU.add), B, B)
            self.act(ss[s][:, 0:6], ss[s][:, 0:6], AF.Sqrt, B, B, bias=1e-6, scale=1.0 / 64.0)
            self.op("dve", lambda e, s=s: e.reciprocal(out=ss[s][:, 0:6], in_=ss[s][:, 0:6]), B, B)
            self.tt("dve", qn[s][:], qn[s][:], ss[s][:, 0:6].unsqueeze(2).to_broadcast([128, 6, 64]), ALU.mult, B, B)
            self.tt("pool", qn[s][:], qn[s][:], gqk[:], ALU.mult, B + [bg], B)
            yield True
            if ti >= 2:
                j = ti - 2
                cosb = cs[:, j, 0:32].unsqueeze(1).to_broadcast([128, 6, 32])
                sinb = cs[:, j, 32:64].unsqueeze(1).to_broadcast([128, 6, 32])
                x1 = qn[s][:, :, 0:64:2]
                x2 = qn[s][:, :, 1:64:2]
                self.tt("dve", t_a[s][:], x1, cosb, ALU.mult, B + [bcs], B)
                self.tt("dve", t_b[s][:], x2, sinb, ALU.mult, B + [bcs], B)
                self.tt("dve", qr[s][:, :, 0:64:2], t_a[s][:], t_b[s][:], ALU.subtract, B + [bqr[s]], [bqr[s]])
                self.tt("dve", t_a[s][:], x1, sinb, ALU.mult, B + [bcs, bqr[s]], B)
                self.tt("dve", t_b[s][:], x2, cosb, ALU.mult, B + [bcs, bqr[s]], B)
                self.tt("dve", qr[s][:, :, 1:64:2], t_a[s][:], t_b[s][:], ALU.add, B + [bqr[s]], [bqr[s]])
            else:
                self.copy("dve", qr[s][:], qn[s][:], B, [bqr[s]])
            yield
            pt, bpt = self.psb[n % 2]
            for h in range(6):
                self.tr(pt[0:64, h * 128:(h + 1) * 128], qr[s][:, h, :], self.ident_b[:], [bqr[s], self.b_ident_b],
                        [bpt], inc=(h == 5))
            yield
            self.copy("act", qT[:, :, tk:tk + 128], pt[0:64, 0:512].rearrange("p (h t) -> p h t", h=4), [bpt], [bqT])
            self.copy("dve", kT[:, :, tk:tk + 128], pt[0:64, 512:768].rearrange("p (h t) -> p h t", h=2), [bpt], [bkT])
            yield

        run_pipeline((tile_gen(n, ti) for n, ti in enumerate(tiles)), depth=2)
        self.precast_experts(l)
        pT = [self.sb(es, "at_pT%d" % i, [128, 512], BF16) for i in range(3)]; bpT = [Buf() for _ in range(3)]
        osb = [self.sb(es, "at_o%d" % i, [65, 512], F32) for i in range(2)]; bosb = [Buf(), Buf()]
        yo = [self.sb(es, "at_y%d" % i, [64, 512], BF16) for i in range(2)]; byo = [Buf(), Buf()]
        qsets = [(LC + c * 512, 512, list(range(NTILE))) for c in range(8)]
        if ctx_out:
            qsets = [(0, LC, [0, 1])] + qsets
        it = 0
        ei = 0
        for (q0, nq, kts) in qsets:
            for h in range(4):
                g = h // 2
                po, bpo = self.psf[4 + it % 2]
                o_, bo_ = osb[it % 2], bosb[it % 2]
                y_, by_ = yo[it % 2], byo[it % 2]
                it += 1

                def S(i):
                    kt = kts[i]
                    ps_, bp_ = self.psf[i % 3]
                    self.mm(ps_[:, 0:nq], kT[0:64, g, kt * 128:(kt + 1) * 128], qT[0:64, h, q0:q0 + nq], True, True,
                            [bkT, bqT], [bp_])
                S(0)
                for i, kt in enumerate(kts):
                    if i + 1 < len(kts):
                        S(i + 1)
                    ps_, bp_ = self.psf[i % 3]
                    p_, bp2 = pT[ei % 3], bpT[ei % 3]
                    ei += 1
                    self.act(p_[:, 0:nq], ps_[:, 0:nq], AF.Exp, [bp_], [bp2], scale=0.125)
                    self.mm(po[0:65, 0:nq], va[:, kt, g, 0:65], p_[:, 0:nq], i == 0, i == len(kts) - 1, [bva, bp2], [bpo])
                self.copy("dve", o_[0:65, 0:nq], po[0:65, 0:nq], [bpo], [bo_])
                self.op("dve", lambda e, o_=o_, nq=nq: e.reciprocal(out=o_[64:65, 0:nq], in_=o_[64:65, 0:nq]), [bo_], [bo_])
                pb_, bpb_ = self.psf[3]
                self.mm(pb_[0:64, 0:nq], self.ones_f[64:65, 0:64], o_[64:65, 0:nq], True, True, [self.b_ones_f, bo_], [bpb_])
                self.tt("dve", y_[:, 0:nq], o_[0:64, 0:nq], pb_[0:64, 0:nq], ALU.mult, [bo_, bpb_], [by_])
                r0 = (h % 2) * 64
                self.dma("sp", self.ysT3[r0:r0 + 64, 6 + h // 2, q0:q0 + nq], y_[:, 0:nq], [by_], [self.dbuf["ysT"]])
        self.barrier()


K.phase_attn = phase_attn


def phase_hyfilt(self, l, Ls, zname, tname, Gname):
    A = self.A
    CS = min(512, Ls)
    with ExitStack() as es:
        w1 = self.sb(es, "hf_w1", [33, 64], F32); w2 = self.sb(es, "hf_w2", [64, 64], F32)
        w3 = self.sb(es, "hf_w3", [64, 512], F32); bw = Buf()
        cols = self.sb(es, "hf_cols", [64, 4], F32)
        dl = self.sb(es, "hf_dl", [128, 2], F32)
        self.dma("sp", w1[:], A["hyena_w1"][l, :, :], [], [bw])
        self.dma("sp", w2[:], A["hyena_w2"][l, :, :], [], [bw])
        self.dma("sp", w3[:], A["hyena_w3"][l, :, :], [], [bw])
        for i, nme in enumerate(("hyena_b1", "hyena_freq1", "hyena_b2", "hyena_freq2")):
            self.dma("sp", cols[:, i:i + 1], A[nme][l:l + 1, :].rearrange("o n -> n o"), [], [bw],
                     allow_slow_non_contiguous=True)
        self.dma("sp", dl[:], A["hy_delta"].rearrange("(c p) o -> p (c o)", p=128), [], [bw],
                 allow_slow_non_contiguous=True)
        self.ts("dve", dl[:], dl[:], -1.0, None, ALU.mult, None, [bw], [bw])
        z = [self.sb(es, "hf_z%d" % i, [33, CS], F32) for i in range(2)]; bz = [Buf(), Buf()]
        tb = [self.sb(es, "hf_t%d" % i, [128, CS], F32) for i in range(2)]; btb = [Buf(), Buf()]
        a1s = [self.sb(es, "hf_a1_%d" % i, [64, CS], F32) for i in range(2)]; ba1s = [Buf(), Buf()]
        a2s = [self.sb(es, "hf_a2_%d" % i, [64, CS], F32) for i in range(2)]; ba2s = [Buf(), Buf()]
        tmps = [self.sb(es, "hf_tmp%d" % i, [64, CS], F32) for i in range(2)]
        win = [self.sb(es, "hf_win%d" % i, [128, CS], F32) for i in range(2)]; bwin = [Buf(), Buf()]
        g = [self.sb(es, "hf_g%d" % i, [128, CS], BF16) for i in range(2)]; bg = [Buf(), Buf()]
        Gap = A[Gname]
        gi_ = [0]

        def chunk_gen(ci):
            n0 = ci * CS
            s = ci % 2
            a1, ba1, a2, ba2, tmp = a1s[s], ba1s[s], a2s[s], ba2s[s], tmps[s]
            self.dma("sp", z[s][:], A[zname][:, n0:n0 + CS], [], [bz[s]])
            self.dma("sp", tb[s][:], A[tname][0:1, n0:n0 + CS].partition_broadcast(128), [], [btb[s]])
            yield
            p1, bp1 = self.psf[0 + 3 * s]
            self.mm(p1[0:64, 0:CS], w1[:, :], z[s][:, :], True, True, [bw, bz[s]], [bp1])
            self.ts("dve", a1[:], p1[0:64, 0:CS], cols[:, 0:1], cols[:, 1:2], ALU.add, ALU.mult, [bp1, bw], [ba1])
            yield
            self.range_reduce(a1[:], tmp[:], [ba1], [ba1])
            yield
            self.act(a1[:], a1[:], AF.Sin, [ba1], [ba1])
            yield True
            p2, bp2 = self.psf[1 + 3 * s]
            self.mm(p2[0:64, 0:CS], w2[:, :], a1[:, :], True, True, [bw, ba1], [bp2])
            self.ts("dve", a2[:], p2[0:64, 0:CS], cols[:, 2:3], cols[:, 3:4], ALU.add, ALU.mult, [bp2, bw], [ba2])
            yield
            self.range_reduce(a2[:], tmp[:], [ba2, ba1], [ba2, ba1])
            yield
            self.act(a2[:], a2[:], AF.Sin, [ba2], [ba2])
            yield
            for cc in range(2):
                cb = (256 if n0 < Ls else 0) + cc * 128
                p3, bp3 = self.psf[2 + 3 * s] if cc == 0 else self.psb_f32[s]
                self.mm(p3[:, 0:CS], w3[:, cb:cb + 128], a2[:, :], True, True, [bw, ba2], [bp3])
                w_, bw_ = win[gi_[0] % 2], bwin[gi_[0] % 2]
                g_, bg_ = g[gi_[0] % 2], bg[gi_[0] % 2]
                gi_[0] += 1
                self.act(w_[:], tb[s][:], AF.Exp, [btb[s], bw], [bw_], scale=dl[:, cc:cc + 1])
                self.tt("dve", g_[:], p3[:, 0:CS], w_[:], ALU.mult, [bp3, bw_], [bg_])
                self.dma("sp", Gap[cc * 128:(cc + 1) * 128, n0:n0 + CS], g_[:], [bg_], [self.dbuf[Gname]])
                yield

        run_pipeline((chunk_gen(ci) for ci in range(2 * Ls // CS)), depth=2)
        self.barrier()


K.phase_hyfilt = phase_hyfilt


def phase_hyena(self, l, ctx_out):
    A = self.A
    seqs = [(LAT0, LC, L, "hyG")]
    if ctx_out:
        seqs = [(CTX0, 0, LC, "hyGc")] + seqs
    self.phase_hyfilt(l, L, "hy_z", "hy_t", "hyG")
    if ctx_out:
        self.phase_hyfilt(l, LC, "hy_zc", "hy_tc", "hyGc")
    with ExitStack() as es:
        w, bw = self.load_w_in(es, l, "hy_w", OFF_HYENA, 768)
        taps = self.sb(es, "hy_taps", [128, 6, 3], F32); btaps = Buf()
        for c6 in range(6):
            self.dma("sp", taps[:, c6, :], A["hyena_conv"][l, :, c6 * 128:(c6 + 1) * 128].rearrange("j p -> p j"),
                     [], [btaps], allow_slow_non_contiguous=True)
        skip = self.sb(es, "hy_skip", [128, 2], F32)
        self.dma("sp", skip[:], A["hyena_skip"][l:l + 1, :].rearrange("o (c p) -> p (o c)", p=128), [], [btaps],
                 allow_slow_non_contiguous=True)
        hch = [self.sb(es, "hy_h%d" % i, [128, 8, 512], BF16) for i in range(2)]; bhch = [Buf(), Buf()]
        praw = self.sb(es, "hy_praw", [128, L + 2], F32); bpraw = Buf()
        tmp = self.sb(es, "hy_tmp", [128, L], F32); btmp = Buf()
        u = self.sb(es, "hy_u", [128, L], F32); bu = Buf()
        x0c = self.sb(es, "hy_x0", [128, L], F32); bx0 = Buf()
        ub = self.sb(es, "hy_ub", [128, L], BF16); bub = Buf()
        utok = self.sb(es, "hy_utok", [128, 128, L // 128], BF16); butok = Buf()
        ut = self.sb(es, "hy_ut", [128, 512], BF16); but = Buf()
        S = [self.sb(es, "hy_S%d" % i, [128, 63 * 128], BF16) for i in range(2)]; bS = [Buf(), Buf()]
        hi = 0
        si = 0
        for (col0, tok0, n, Gname) in seqs:
            nb = n // 128
            W = (2 * nb - 1) * 128
            Gt = self.dram[Gname]
            ytok = praw[:, 0:n].rearrange("p (a c) -> p a c", c=128)
            for cc in range(2):
                for part, cb in (("x1", 256), ("v", 512), ("x0", 0)):
                    c6 = cb // 128 + cc
                    self.memset("pool", praw[:, 0:1], 0.0, [bpraw])
                    self.memset("pool", praw[:, n + 1:n + 2], 0.0, [bpraw])
                    for t0 in range(0, n, 512):
                        nn = min(512, n - t0)
                        h_, bh_ = hch[hi % 2], bhch[hi % 2]
                        hi += 1
                        self.dma("sp", h_[:, :, 0:nn], self.hT3[:, :, col0 + t0:col0 + t0 + nn], [self.dbuf["hT"]], [bh_])
                        ps, bp = self.psf[2 + hi % 2]
                        for k in range(8):
                            self.mm(ps[:, 0:nn], w[:, k, cb + cc * 128:cb + (cc + 1) * 128], h_[:, k, 0:nn],
                                    k == 0, k == 7, [bw, bh_], [bp])
                        self.copy("act", praw[:, 1 + t0:1 + t0 + nn], ps[:, 0:nn], [bp], [bpraw])
                    dst, bdst = {"x1": (u, bu), "v": (tmp, btmp), "x0": (x0c, bx0)}[part]
                    self.ts("dve", dst[:, 0:n], praw[:, 0:n], taps[:, c6, 0:1], None, ALU.mult, None, [bpraw, btaps], [bdst])
                    self.stt(dst[:, 0:n], praw[:, 1:n + 1], taps[:, c6, 1:2], dst[:, 0:n], ALU.mult, ALU.add,
                             [bpraw, btaps, bdst], [bdst])
                    self.stt(dst[:, 0:n], praw[:, 2:n + 2], taps[:, c6, 2:3], dst[:, 0:n], ALU.mult, ALU.add,
                             [bpraw, btaps, bdst], [bdst])
                    if part == "v":
                        self.tt("dve", u[:, 0:n], u[:, 0:n], tmp[:, 0:n], ALU.mult, [bu, btmp], [bu])
                        self.copy("act", ub[:, 0:n], u[:, 0:n], [bu], [bub])
                for b0 in range(0, nb, 4):
                    nbb = min(4, nb - b0)
                    pt, bpt = self.psb[(b0 // 4) % 2]
                    for bb in range(nbb):
                        b_ = b0 + bb
                        self.tr(pt[:, bb * 128:(bb + 1) * 128], ub[:, b_ * 128:(b_ + 1) * 128], self.ident_b[:],
                                [bub, self.b_ident_b], [bpt], inc=(bb == nbb - 1))
                    self.copy("dve", ut[:, 0:nbb * 128], pt[:, 0:nbb * 128], [bpt], [but])
                    pf, bpf = self.psf[4 + (b0 // 4) % 2]
                    self.mm(pf[:, 0:nbb * 128], self.flip_b[:], ut[:, 0:nbb * 128], True, True, [self.b_flip_b, but], [bpf])
                    self.copy("act", utok[:, :, b0:b0 + nbb].rearrange("p c b -> p b c"),
                              pf[:, 0:nbb * 128].rearrange("p (b c) -> p b c", c=128), [bpf], [butok])
                for c0 in range(0, 128, 16):
                    py, bpy = self.psf[(c0 // 16) % 2]
                    for cl in range(16):
                        c = c0 + cl
                        cg = cc * 128 + c
                        S_, bS_ = S[si % 2], bS[si % 2]
                        si += 1
                        src = bass.AP(Gt, cg * 2 * n + 1, [[1, 128], [1, W]])
                        self.dma("sp", S_[:, 0:W], src, [self.dbuf[Gname]], [bS_])
                        ds = [0] + [d for d in range(-(nb - 1), nb) if d != 0]
                        for ii, d in enumerate(ds):
                            a0 = max(0, d)
                            a1_ = min(nb - 1, nb - 1 + d)
                            off = (nb - 1 + d) * 128
                            self.mm(py[:, cl * nb + a0:cl * nb + a1_ + 1], S_[:, off:off + 128],
                                    utok[:, c, a0 - d:a1_ - d + 1], ii == 0, ii == len(ds) - 1, [bS_, butok], [bpy])
                    eng = "dve" if (c0 // 16) % 2 == 0 else "act"
                    self.copy(eng, ytok[:, :, c0:c0 + 16], py[:, 0:16 * nb].rearrange("p (c a) -> p a c", a=nb),
                              [bpy], [bpraw])
                for a0 in range(0, nb, 4):
                    na = min(4, nb - a0)
                    pf, bpf = self.psf[2 + (a0 // 4) % 2]
                    for aa in range(na):
                        self.tr(pf[:, aa * 128:(aa + 1) * 128], ytok[:, a0 + aa, :], self.ident_f[:],
                                [bpraw, self.b_ident_f], [bpf], inc=(aa == na - 1))
                    self.copy("act", tmp[:, a0 * 128:(a0 + na) * 128], pf[:, 0:na * 128], [bpf], [btmp])
                self.stt(tmp[:, 0:n], u[:, 0:n], skip[:, cc:cc + 1], tmp[:, 0:n], ALU.mult, ALU.add, [bu, btaps, btmp], [btmp])
                self.tt("dve", ub[:, 0:n], tmp[:, 0:n], x0c[:, 0:n], ALU.mult, [btmp, bx0], [bub])
                self.dma("sp", self.ysT3[:, 2 + cc, tok0:tok0 + n], ub[:, 0:n], [bub], [self.dbuf["ysT"]])
        self.barrier()


K.phase_hyena = phase_hyena


NCH = NTOK // CH
C0 = math.exp(-0.5)


def phase_rwkv_prep(self, l):
    A = self.A
    with ExitStack() as es:
        w, bw = self.load_w_in(es, l, "rk_w", 0, 1024)
        mu = self.sb(es, "rk_mu", [128, 8], F32); bmu = Buf()
        om = self.sb(es, "rk_om", [128, 8], F32)
        hm = self.sb(es, "rk_hm", [128, 8], F32)
        self.dma("sp", mu[:], A["rwkv_mu"][l:l + 1, :].rearrange("o (j p) -> p (o j)", p=128), [], [bmu],
                 allow_slow_non_contiguous=True)
        self.ts("dve", om[:], mu[:], -1.0, 1.0, ALU.mult, ALU.add, [bmu], [bmu])
        self.ts("dve", hm[:], mu[:], 0.5, None, ALU.mult, None, [bmu], [bmu])
        colv = self.sb(es, "rk_colv", [128, 16], F32); bcv = Buf()
        for i, nme in enumerate(("rwkv_k_k", "rwkv_k_a", "rwkv_r_k")):
            self.dma("sp", colv[:, 2 * i:2 * i + 2], A[nme][l:l + 1, :].rearrange("o (c p) -> p (o c)", p=128), [], [bcv],
                     allow_slow_non_contiguous=True)
        for d in range(2):
            self.dma("sp", colv[:, 8 + 2 * d:10 + 2 * d], A["rwkv_w0"][l, d:d + 1, :].rearrange("o (c p) -> p (o c)", p=128),
                     [], [bcv], allow_slow_non_contiguous=True)
            self.dma("sp", colv[:, 12 + 2 * d:14 + 2 * d], A["rwkv_a0"][l, d:d + 1, :].rearrange("o (c p) -> p (o c)", p=128),
                     [], [bcv], allow_slow_non_contiguous=True)
        self.ts("dve", colv[:, 6:8], colv[:, 2:4], -1.0, 1.0, ALU.mult, ALU.add, [bcv], [bcv])
        wup = self.sb(es, "rk_wup", [64, 2, 256], F32)
        aup = self.sb(es, "rk_aup", [128, 2, 256], F32)
        gup = self.sb(es, "rk_gup", [128, 256], F32); bwl = Buf()
        for d in range(2):
            self.dma("sp", wup[:, d, :], A["rwkv_w_up"][l, d, :, :], [], [bwl])
            self.dma("sp", aup[64:128, d, :], A["rwkv_a_up"][l, d, :, :], [], [bwl])
        self.dma("sp", gup[:], A["rwkv_g_up"][l, :, :], [], [bwl])
        ones512 = self.sb(es, "rk_ones", [128, CH], F32); bones = Buf()
        self.memset("pool", ones512[:], 1.0, [bones])
        hch = [self.sb(es, "rk_h%d" % i, [128, 8, 514], BF16) for i in range(2)]; bhch = [Buf(), Buf()]
        pj = [self.sb(es, "rk_p%d" % j, [128, 512], F32) for j in range(8)]; bpj = [Buf() for _ in range(8)]
        NWK = 22
        wk = [self.sb(es, "rk_wk%d" % i, [128, 512], F32) for i in range(NWK)]
        bwk = [Buf() for _ in range(NWK)]
        tmo = [self.sb(es, "rk_tmo%d" % i, [128, 4, 128], F32) for i in range(3)]; btmo = [Buf(), Buf(), Buf()]
        wk1 = {i_: self.sb(es, "rk_wkb%d" % i_, [128, 512], F32) for i_ in range(7, NWK)}
        bwk1 = {i_: Buf() for i_ in range(7, NWK)}
        gcs2 = [self.sb(es, "rk_gcs%d" % i, [128, 8], F32) for i in range(2)]; bgcs2 = [Buf(), Buf()]

        def run_rr(gens):
            gens = list(gens)
            while gens:
                for g_ in list(gens):
                    try:
                        next(g_)
                    except StopIteration:
                        gens.remove(g_)
        fmA = A["rk_fm"].rearrange("(d k c) n -> d k c n", d=2, k=4)
        tmA = A["rk_tm"].rearrange("(d n) (k c) -> d n k c", d=2, k=2)
        gcA = A["rk_gc"].rearrange("(d c) n -> d c n", d=2)
        hi = 0
        ti_ = [0]
        psi = [0]

        def nps():
            psi[0] += 1
            return self.psf[psi[0] % 6]

        def to_tm(src, bsrc, nn, dstfn):
            nt = nn // 128
            pf, bpf = nps()
            for a in range(nt):
                self.tr(pf[:, a * 128:(a + 1) * 128], src[:, a * 128:(a + 1) * 128], self.ident_f[:],
                        [bsrc, self.b_ident_f], [bpf], inc=(a == nt - 1))
            t_, bt_ = tmo[ti_[0] % 3], btmo[ti_[0] % 3]
            ti_[0] += 1
            self.copy("act", t_[:, 0:nt, :], pf[:, 0:nt * 128].rearrange("p (a c) -> p a c", c=128), [bpf], [bt_])
            return t_, bt_, nt

        for (col0, tok0, n) in [(CTX0, 0, LC), (LAT0, LC, L)]:
            for t0 in range(0, n, 512):
                nn = min(512, n - t0)
                tk = tok0 + t0
                nsub = nn // CH
                h_, bh_ = hch[hi % 2], bhch[hi % 2]
                hi += 1
                c0 = col0 + t0
                self.dma("sp", h_[:, :, 0:nn + 2], self.hT3[:, :, c0 - 1:c0 + nn + 1], [self.dbuf["hT"]], [bh_])
                for j in range(8):
                    pa, bpa = nps()
                    for k in range(8):
                        self.mm(pa[:, 0:nn], w[:, k, j * 128:(j + 1) * 128], h_[:, k, 1:nn + 1], k == 0, k == 7, [bw, bh_], [bpa])
                    pb, bpb = nps()
                    for k in range(8):
                        self.mm(pb[:, 0:nn], w[:, k, j * 128:(j + 1) * 128], h_[:, k, 0:nn], k == 0, False, [bw, bh_], [bpb], inc=False)
                    for k in range(8):
                        self.mm(pb[:, 0:nn], w[:, k, j * 128:(j + 1) * 128], h_[:, k, 2:nn + 2], False, k == 7, [bw, bh_], [bpb])
                    self.act(pj[j][:, 0:nn], pa[:, 0:nn], AF.Identity, [bpa, bmu], [bpj[j]], scale=om[:, j:j + 1])
                    self.stt(pj[j][:, 0:nn], pb[:, 0:nn], hm[:, j:j + 1], pj[j][:, 0:nn], ALU.mult, ALU.add,
                             [bpb, bmu, bpj[j]], [bpj[j]])
                tw, btw = wk[0], bwk[0]
                sg, bsg = wk[1], bwk[1]
                self.act(tw[0:64, 0:nn], pj[6][0:64, 0:nn], AF.Tanh, [bpj[6]], [btw])
                self.act(sg[:, 0:nn], pj[7][:, 0:nn], AF.Sigmoid, [bpj[7]], [bsg])
                for cc in range(2):
                    S = slice(0, nn)
                    r_, br_ = pj[cc], bpj[cc]
                    k_, bk_ = pj[2 + cc], bpj[2 + cc]
                    v_, bv_ = pj[4 + cc], bpj[4 + cc]
                    pg, bpg = nps()
                    self.mm(pg[:, S], gup[:, cc * 128:(cc + 1) * 128], sg[:, S], True, True, [bwl, bsg], [bpg])
                    gT, bgT = wk[2], bwk[2]
                    self.copy("act", gT[:, S], pg[:, S], [bpg], [bgT])
                    self.dma("sp", A["rk_g"][cc * 128:(cc + 1) * 128, tk:tk + nn], gT[:, S], [bgT], [self.dbuf["rk_g"]])
                    t_, bt_, nt = to_tm(v_, bv_, nn, None)
                    self.dma("sp", A["rk_v"][tk:tk + nn, cc * 128:(cc + 1) * 128].rearrange("(a p) c -> p a c", p=128),
                             t_[:, 0:nt, :], [bt_], [self.dbuf["rk_v"]])
                    kx, bkx = wk[3], bwk[3]
                    sq, bsq = wk[4], bwk[4]
                    kk, bkk = wk[5], bwk[5]
                    self.ts("dve", kx[:, S], k_[:, S], colv[:, 0 + cc:1 + cc], None, ALU.mult, None, [bk_, bcv], [bkx])
                    self.tt("pool", sq[:, S], kx[:, S], kx[:, S], ALU.mult, [bkx], [bsq])
                    pn, bpn = nps()
                    self.mm(pn[:, S], self.blk_f[:], sq[:, S], True, True, [self.b_blk_f, bsq], [bpn])
                    self.ts("dve", sq[:, S], pn[:, S], 1e-24, None, ALU.max, None, [bpn], [bsq])
                    self.act(sq[:, S], sq[:, S], AF.Sqrt, [bsq], [bsq])
                    self.op("dve", lambda e, sq=sq, S=S: e.reciprocal(out=sq[:, S], in_=sq[:, S]), [bsq], [bsq])
                    self.tt("dve", kk[:, S], kx[:, S], sq[:, S], ALU.mult, [bkx, bsq], [bkk])
                    ksum, bks = wk[6], bwk[6]
                    def dchain(d, cc=cc, S=S, nn=nn, nsub=nsub, tk=tk, r_=r_, br_=br_, k_=k_, bk_=bk_, kk=kk, bkk=bkk, ksum=ksum, bks=bks, tw=tw, btw=btw):
                        W = (lambda i_: (wk[i_], bwk[i_])) if d == 0 else (lambda i_: (wk1[i_], bwk1[i_]))
                        gcs, bgcs = gcs2[d], bgcs2[d]
                        pw, bpw = nps()
                        self.mm(pw[:, S], wup[:, d, cc * 128:(cc + 1) * 128], tw[0:64, S], True, True, [bwl, btw], [bpw])
                        sgm, bsgm = W(7)
                        self.act(sgm[:, S], pw[:, S], AF.Sigmoid, [bpw, bcv], [bsgm], bias=colv[:, 8 + 2 * d + cc:9 + 2 * d + cc])
                        yield
                        lw, blw = W(8)
                        self.ts("pool", lw[:, S], sgm[:, S], -C0, None, ALU.mult, None, [bsgm], [blw])
                        pa_, bpa_ = nps()
                        self.mm(pa_[:, S], aup[64:128, d, cc * 128:(cc + 1) * 128], pj[6][64:128, S], True, True, [bwl, bpj[6]], [bpa_])
                        a_, ba_ = W(9)
                        self.act(a_[:, S], pa_[:, S], AF.Sigmoid, [bpa_, bcv], [ba_], bias=colv[:, 12 + 2 * d + cc:13 + 2 * d + cc])
                        yield
                        kd, bkd = W(10)
                        self.ts("dve", kd[:, S], a_[:, S], colv[:, 2 + cc:3 + cc], colv[:, 6 + cc:7 + cc], ALU.mult, ALU.add,
                                [ba_, bcv], [bkd])
                        self.tt("dve", kd[:, S], kd[:, S], k_[:, S], ALU.mult, [bkd, bk_], [bkd])
                        b_, bb_ = W(11)
                        self.tt("pool", b_[:, S], kk[:, S], a_[:, S], ALU.mult, [bkk, ba_], [bb_])
                        if d == 0:
                            self.copy("pool", ksum[:, S], kd[:, S], [bkd], [bks])
                        else:
                            self.tt("pool", ksum[:, S], ksum[:, S], kd[:, S], ALU.add, [bks, bkd], [bks])
                        yield
                        ci_, bci = W(12)
                        for sb_ in range(nsub):
                            sl = slice(sb_ * CH, (sb_ + 1) * CH)
                            self.op("dve", lambda e, ci_=ci_, lw=lw, sl=sl: e.tensor_tensor_scan(
                                out=ci_[:, sl], data0=ones512[:, 0:CH], data1=lw[:, sl], initial=0.0,
                                op0=ALU.mult, op1=ALU.add), [blw, bones], [bci])
                        yield
                        self.copy("dve", gcs[:, 0:nsub], ci_[:, CH - 1:nn:CH], [bci], [bgcs])
                        if d == 1:
                            for sb_ in range(nsub):
                                sl = slice(sb_ * CH, (sb_ + 1) * CH)
                                self.ts("dve", ci_[:, sl], ci_[:, sl], -1.0, gcs[:, sb_:sb_ + 1], ALU.mult, ALU.add, [bci, bgcs], [bci])
                            self.tt("dve", ci_[:, S], ci_[:, S], lw[:, S], ALU.add, [bci, blw], [bci])
                        yield
                        ce, bce = W(13)
                        self.tt("pool", ce[:, S], ci_[:, S], lw[:, S], ALU.subtract, [bci, blw], [bce])
                        yield
                        e1, be1 = W(14)
                        e2, be2 = W(15)
                        e3, be3 = W(16)
                        e4, be4 = W(17)
                        self.act(e1[:, S], ce[:, S], AF.Exp, [bce], [be1])
                        self.act(e2[:, S], ci_[:, S], AF.Exp, [bci], [be2])
                        self.act(e3[:, S], ci_[:, S], AF.Exp, [bci], [be3], scale=-1.0)
                        for sb_ in range(nsub):
                            sl = slice(sb_ * CH, (sb_ + 1) * CH)
                            self.act(e4[:, sl], ci_[:, sl], AF.Exp, [bci, bgcs], [be4], scale=-1.0, bias=gcs[:, sb_:sb_ + 1])
                        yield
                        gce, bgce = W(18)
                        self.act(gce[:, 0:nsub], gcs[:, 0:nsub], AF.Exp, [bgcs], [bgce])
                        self.dma("sp", gcA[d, cc * 128:(cc + 1) * 128, tk // CH:tk // CH + nsub], gce[:, 0:nsub], [bgce],
                                 [self.dbuf["rk_gc"]], allow_slow_non_contiguous=True)
                        yield
                        o_, bo_ = W(19)
                        for kind, (x_, bx_, e_, be_) in enumerate(((kk, bkk, e1, be1), (r_, br_, e2, be2),
                                                                    (b_, bb_, e3, be3), (kd, bkd, e3, be3))):
                            o_, bo_ = W(19 + kind % 2)
                            self.tt("dve" if kind % 2 == 0 else "pool", o_[:, S], x_[:, S], e_[:, S], ALU.mult, [bx_, be_], [bo_])
                            self.dma("sp", fmA[d, kind, cc * 128:(cc + 1) * 128, tk:tk + nn], o_[:, S], [bo_], [self.dbuf["rk_fm"]])
                        yield
                        for kind, (x_, bx_) in enumerate(((b_, bb_), (kd, bkd))):
                            o_, bo_ = W(21)
                            self.tt("dve", o_[:, S], x_[:, S], e4[:, S], ALU.mult, [bx_, be4], [bo_])
                            t_, bt_, nt = to_tm(o_, bo_, nn, None)
                            self.dma("sp", tmA[d, tk:tk + nn, kind, cc * 128:(cc + 1) * 128].rearrange("(a p) c -> p a c", p=128),
                                     t_[:, 0:nt, :], [bt_], [self.dbuf["rk_tm"]])

                    def _delay(g_):
                        yield
                        for _ in g_:
                            yield
                    run_rr([dchain(0), _delay(dchain(1))])
                    self.stt(ksum[:, S], r_[:, S], colv[:, 4 + cc:5 + cc], ksum[:, S], ALU.mult, ALU.mult, [br_, bcv, bks], [bks])
                    pbn, bpbn = nps()
                    self.mm(pbn[:, S], self.blk_f[:], ksum[:, S], True, True, [self.b_blk_f, bks], [bpbn])
                    bo2, bbo2 = wk[2], bwk[2]
                    self.tt("dve", bo2[:, S], pbn[:, S], v_[:, S], ALU.mult, [bpbn, bv_], [bbo2])
                    self.dma("sp", A["rk_bonus"][cc * 128:(cc + 1) * 128, tk:tk + nn], bo2[:, S], [bbo2], [self.dbuf["rk_bonus"]])
        self.barrier()


K.phase_rwkv_prep = phase_rwkv_prep


def phase_rwkv_scan(self, l, ctx_out):
    A = self.A
    fmA = A["rk_fm"].rearrange("(d k c) n -> d k c n", d=2, k=4)
    tmA = A["rk_tm"].rearrange("(d n) (k c) -> d n k c", d=2, k=2)
    gcA = A["rk_gc"].rearrange("(d c) n -> d c n", d=2)
    yA = A["rk_y"].rearrange("(d n) c -> d n c", d=2)
    F32R = mybir.dt.float32r

    def R_(ap):
        return ap.bitcast(F32R)
    with ExitStack() as es:
        base = self.sb(es, "sc_mbase", [64, 4, 64], F32); bmk = Buf()
        for i, (pat, cm, cmp_) in enumerate((([[1, 64]], -1, ALU.is_gt), ([[1, 64]], -1, ALU.is_ge),
                                             ([[-1, 64]], 1, ALU.is_gt), ([[-1, 64]], 1, ALU.is_ge))):
            self.memset("pool", base[:, i, :], 1.0, [bmk])
            self.op("pool", lambda e, i=i, pat=pat, cm=cm, cmp_=cmp_: e.affine_select(
                out=base[:, i, :], in_=base[:, i, :], pattern=pat, compare_op=cmp_, fill=0.0, base=0,
                channel_multiplier=cm), [bmk], [bmk])
        amask = self.sb(es, "sc_amask", [64, 2, 128], F32)
        mmask = self.sb(es, "sc_mmask", [64, 2, 128], F32)
        ntmask = self.sb(es, "sc_ntmask", [64, 2, 64], F32)
        for d in range(2):
            st_, in_ = (0, 1) if d == 0 else (2, 3)
            self.ts("dve", amask[:, d, 0:64], base[:, st_, :], -1.0, None, ALU.mult, None, [bmk], [bmk])
            self.copy("dve", amask[:, d, 64:128], base[:, st_, :], [bmk], [bmk])
            self.copy("dve", mmask[:, d, 0:64], base[:, in_, :], [bmk], [bmk])
            self.copy("dve", mmask[:, d, 64:128], base[:, in_, :], [bmk], [bmk])
            self.ts("dve", ntmask[:, d, :], base[:, 2 if d == 0 else 0, :], -1.0, None, ALU.mult, None, [bmk], [bmk])
        idb = self.ident_f[0:64, 0:64].unsqueeze(1).to_broadcast([64, 4, 64])
        R = []
        for d in range(2):
            r = {}
            r["gcs"] = self.sb(es, "sc_gcs%d" % d, [64, 4, NCH], F32); r["bgcs"] = Buf()
            self.dma("sp", r["gcs"][:], gcA[d].rearrange("(h k) n -> k h n", k=64), [self.dbuf["rk_gc"]], [r["bgcs"]])
            for nm, shp in (("fm", [64, 4, 4, 64]), ("tm", [64, 3, 256]), ("fmr", [64, 4, 4, 64]), ("tmr", [64, 3, 256]), ("AMa", [64, 4, 128]), ("AMm", [64, 4, 128]),
                            ("NT", [64, 4, 64]), ("AkV", [64, 4, 64]), ("X", [64, 4, 64]), ("XT", [64, 4, 64]),
                            ("P0", [64, 4, 64]), ("P1", [64, 4, 64]), ("Q0", [64, 4, 64]), ("Q1", [64, 4, 64])):
                r[nm] = [self.sb(es, "sc_%s%d_%d" % (nm, d, s_), shp, F32) for s_ in range(2)]
                r["b" + nm] = [Buf(), Buf()]
            for nm in ("RHS", "Zs", "Ys", "ST2", "STr"):
                r[nm] = self.sb(es, "sc_%s%d" % (nm, d), [64, 4, 64], F32); r["b" + nm] = Buf()
            r["ST"] = [self.sb(es, "sc_ST%d_%d" % (d, s_), [64, 4, 64], F32) for s_ in range(2)]
            r["bST"] = [Buf(), Buf()]
            self.memset("pool", r["ST"][0][:], 0.0, [r["bST"][0]])
            self.copy("dve", R_(r["STr"][:]), r["ST"][0][:], [r["bST"][0]], [r["bSTr"]])
            r["B0"] = self.psf[2 * d]
            r["B1"] = self.psf[2 * d + 1]
            r["B3"] = self.psf[4 + d]
            r["B2"] = (self.psb[d][0][:].bitcast(F32), self.psb[d][1])
            r["order"] = ([0, 1, 2, 3] + list(range(4, NCH))) if d == 0 else ([3, 2, 1, 0] + list(range(NCH - 1, 3, -1)))
            R.append(r)

        def v4(ap):
            return ap.rearrange("p (h t) -> p h t", h=4)

        def pre(d, g, s_):
            r = R[d]
            fm, bfm = r["fm"][s_], r["bfm"][s_]
            tm, btm = r["tm"][s_], r["btm"][s_]
            tsl = slice(g * CH, (g + 1) * CH)
            for kind in range(4):
                self.dma("sp", fm[:, :, kind, :], fmA[d, kind].rearrange("(h k) n -> k h n", k=64)[:, :, tsl],
                         [self.dbuf["rk_fm"]], [bfm])
            self.dma("sp", tm[:, 0:2, :], tmA[d, tsl, :, :], [self.dbuf["rk_tm"]], [btm])
            self.dma("sp", tm[:, 2, :], A["rk_v"][tsl, :], [self.dbuf["rk_v"]], [btm])
            yield
            fmr, bfmr = r["fmr"][s_], r["bfmr"][s_]
            tmr, btmr = r["tmr"][s_], r["btmr"][s_]
            self.copy("pool", R_(fmr[:]), fm[:], [bfm], [bfmr])
            self.copy("act", R_(tmr[:]), tm[:], [btm], [btmr])
            yield
            fm, bfm, tm, btm = fmr, bfmr, tmr, btmr
            b0, bb0 = r["B0"]
            b1, bb1 = r["B1"]
            b2, bb2 = r["B2"]
            AMa, bAMa = r["AMa"][s_], r["bAMa"][s_]
            AMm, bAMm = r["AMm"][s_], r["bAMm"][s_]
            NT, bNT = r["NT"][s_], r["bNT"][s_]
            AkV, bAkV = r["AkV"][s_], r["bAkV"][s_]
            X, bX = r["X"][s_], r["bX"][s_]
            for h in range(4):
                self.mm(b0[0:64, h * 128:h * 128 + 64], R_(fm[:, h, 2, :]), R_(fm[:, h, 0, :]), True, True, [bfm], [bb0], inc=False)
                self.mm(b0[0:64, h * 128 + 64:h * 128 + 128], R_(fm[:, h, 3, :]), R_(fm[:, h, 0, :]), True, True, [bfm], [bb0], inc=(h == 3))
            for h in range(4):
                self.mm(b1[0:64, h * 64:(h + 1) * 64], R_(fm[:, h, 0, :]), R_(fm[:, h, 2, :]), True, True, [bfm], [bb1], inc=(h == 3))
            for h in range(4):
                self.mm(b2[0:64, h * 128:h * 128 + 64], R_(fm[:, h, 2, :]), R_(fm[:, h, 1, :]), True, True, [bfm], [bb2], inc=False)
                self.mm(b2[0:64, h * 128 + 64:h * 128 + 128], R_(fm[:, h, 3, :]), R_(fm[:, h, 1, :]), True, True, [bfm], [bb2], inc=(h == 3))
            yield
            self.tt("dve", R_(AMa[:]), v4(b0[0:64, :]), amask[:, d, :].unsqueeze(1).to_broadcast([64, 4, 128]), ALU.mult, [bb0, bmk], [bAMa])
            self.tt("dve", R_(NT[:]), v4(b1[0:64, 0:256]), ntmask[:, d, :].unsqueeze(1).to_broadcast([64, 4, 64]), ALU.mult, [bb1, bmk], [bNT])
            self.tt("dve", R_(AMm[:]), v4(b2[0:64, :]), mmask[:, d, :].unsqueeze(1).to_broadcast([64, 4, 128]), ALU.mult, [bb2, bmk], [bAMm])
            yield
            self.tt("dve", R_(X[:]), AMa[:, :, 0:64], idb, ALU.add, [bAMa, self.b_ident_f], [bX])
            Pl = [(AMa[:, :, 0:64], bAMa)]
            Ql = [(NT[:], bNT)]
            for j in range(1, 6):
                Pl.append((r["P%d" % (j % 2)][s_][:], r["bP%d" % (j % 2)][s_]))
                Ql.append((r["Q%d" % (j % 2)][s_][:], r["bQ%d" % (j % 2)][s_]))
            for stg in range(1, 7):
                if stg == 1:
                    for h in range(4):
                        self.mm(b1[0:64, 256 + h * 64:256 + (h + 1) * 64], R_(AMa[:, h, 64:128]), R_(tm[:, 2, h * 64:(h + 1) * 64]), True, True,
                                [bAMa, btm], [bb1], inc=(h == 3))
                if stg <= 5:
                    (Pp, bPp), (Qp, bQp) = Pl[stg - 1], Ql[stg - 1]
                    if stg < 5:
                        for h in range(4):
                            self.mm(b0[0:64, h * 64:(h + 1) * 64], R_(Qp[:, h, :]), R_(Pp[:, h, :]), True, True, [bPp, bQp], [bb0], inc=False)
                    for h in range(4):
                        self.mm(b0[0:64, 256 + h * 64:256 + (h + 1) * 64], R_(Pp[:, h, :]), R_(Qp[:, h, :]), True, True, [bPp, bQp], [bb0],
                                inc=(h == 3))
                if stg >= 2:
                    Qa, bQa = Ql[stg - 1]
                    for h in range(4):
                        self.mm(b1[0:64, h * 64:(h + 1) * 64], R_(Qa[:, h, :]), R_(X[:, h, :]), True, True, [bQa, bX], [bb1], inc=(h == 3))
                yield
                if stg == 1:
                    self.copy("dve", AkV[:], v4(b1[0:64, 256:512]), [bb1], [bAkV])
                if stg >= 2:
                    self.tt("dve", R_(X[:]), v4(b1[0:64, 0:256]), X[:], ALU.add, [bb1, bX], [bX])
                if stg <= 5:
                    if stg < 5:
                        Pn, bPn = Pl[stg]
                        self.copy("act", R_(Pn), v4(b0[0:64, 0:256]), [bb0], [bPn])
                    Qn, bQn = Ql[stg]
                    self.copy("act", R_(Qn), v4(b0[0:64, 256:512]), [bb0], [bQn])
                yield

        def seq(d, g, s_, i):
            r = R[d]
            fm, bfm = r["fmr"][s_], r["bfmr"][s_]
            tm, btm = r["tmr"][s_], r["btmr"][s_]
            b2, bb2 = r["B2"]
            b3, bb3 = r["B3"]
            STr, bSTr = r["STr"], r["bSTr"]
            STc, bSTc = r["ST"][i % 2], r["bST"][i % 2]
            STn, bSTn = r["ST"][(i + 1) % 2], r["bST"][(i + 1) % 2]
            X, bX = r["X"][s_], r["bX"][s_]
            AMm, bAMm = r["AMm"][s_], r["bAMm"][s_]
            AkV, bAkV = r["AkV"][s_], r["bAkV"][s_]
            RHS, bRHS = r["RHS"], r["bRHS"]
            Zs, bZs = r["Zs"], r["bZs"]
            Ys, bYs = r["Ys"], r["bYs"]
            ST2, bST2 = r["ST2"], r["bST2"]
            for h in range(4):
                self.mm(b3[0:64, h * 64:(h + 1) * 64], R_(fm[:, h, 0, :]), R_(STr[:, h, :]), True, True, [bfm, bSTr], [bb3], inc=(h == 3))
            self.tt("pool", ST2[:], STc[:], r["gcs"][:, :, g:g + 1].to_broadcast([64, 4, 64]), ALU.mult, [bSTc, r["bgcs"]], [bST2])
            yield
            self.stt(R_(RHS[:]), v4(b3[0:64, 0:256]), -1.0, AkV[:], ALU.mult, ALU.subtract, [bb3, bAkV], [bRHS])
            yield
            for h in range(4):
                self.mm(b3[0:64, 256 + h * 64:256 + (h + 1) * 64], R_(X[:, h, :]), R_(RHS[:, h, :]), True, True, [bX, bRHS], [bb3], inc=(h == 3))
            yield
            self.copy("act", R_(Zs[:]), v4(b3[0:64, 256:512]), [bb3], [bZs])
            yield
            emit = ctx_out or g >= 4
            for h in range(4):
                self.mm(b2[0:64, h * 64:(h + 1) * 64], R_(tm[:, 0, h * 64:(h + 1) * 64]), R_(Zs[:, h, :]), True, False, [btm, bZs], [bb2], inc=False)
                self.mm(b2[0:64, h * 64:(h + 1) * 64], R_(tm[:, 1, h * 64:(h + 1) * 64]), R_(tm[:, 2, h * 64:(h + 1) * 64]), False, True,
                        [btm], [bb2], inc=(h == 3 and not emit))
            if emit:
                for h in range(4):
                    o = b2[0:64, 256 + h * 64:256 + (h + 1) * 64]
                    self.mm(o, R_(fm[:, h, 1, :]), R_(STr[:, h, :]), True, False, [bfm, bSTr], [bb2], inc=False)
                    self.mm(o, R_(AMm[:, h, 0:64]), R_(Zs[:, h, :]), False, False, [bAMm, bZs], [bb2], inc=False)
                    self.mm(o, R_(AMm[:, h, 64:128]), R_(tm[:, 2, h * 64:(h + 1) * 64]), False, True, [bAMm, btm], [bb2], inc=(h == 3))
            yield
            self.tt("dve", STn[:], v4(b2[0:64, 0:256]), ST2[:], ALU.add, [bb2, bST2], [bSTn])
            self.copy("act", R_(STr[:]), STn[:], [bSTn], [bSTr])
            if emit:
                self.copy("dve", Ys[:], v4(b2[0:64, 256:512]), [bb2], [bYs])
                self.dma("sp", yA[d, g * CH:(g + 1) * CH, :], Ys[:].rearrange("p h t -> p (h t)"), [bYs], [self.dbuf["rk_y"]])
            yield

        def run_rr(gens):
            gens = list(gens)
            while gens:
                for g_ in list(gens):
                    try:
                        next(g_)
                    except StopIteration:
                        gens.remove(g_)

        run_rr([pre(d, R[d]["order"][0], 0) for d in range(2)])
        def delayed(g_, n_=1):
            for _ in range(n_):
                yield
            for _ in g_:
                yield

        for i in range(NCH):
            gl = []
            for d in range(2):
                if i + 1 < NCH:
                    g1 = pre(d, R[d]["order"][i + 1], (i + 1) % 2)
                    gl.append(g1 if d == 0 else delayed(g1))
                g2 = seq(d, R[d]["order"][i], i % 2, i)
                gl.append(g2 if d == 0 else delayed(g2))
            run_rr(gl)
        self.barrier()


K.phase_rwkv_scan = phase_rwkv_scan


def phase_rwkv_out(self, l, tiles):
    A = self.A
    yA = A["rk_y"].rearrange("(d n) c -> d n c", d=2)
    with ExitStack() as es:
        gb = self.sb(es, "ro_gb", [128, 2, 256], F32); bgb = Buf()
        self.dma("sp", gb[:, 0, :], A["rwkv_lnx_g"][l:l + 1, :].partition_broadcast(128), [], [bgb])
        self.dma("sp", gb[:, 1, :], A["rwkv_lnx_b"][l:l + 1, :].partition_broadcast(128), [], [bgb])
        NB = 2
        yf = [self.sb(es, "ro_yf%d" % i, [128, 256], F32) for i in range(NB)]
        yb = [self.sb(es, "ro_yb%d" % i, [128, 256], F32) for i in range(NB)]
        bo = [self.sb(es, "ro_bo%d" % i, [128, 2, 128], F32) for i in range(NB)]
        gt = [self.sb(es, "ro_gt%d" % i, [128, 2, 128], F32) for i in range(NB)]
        bin_ = [Buf() for _ in range(NB)]
        st = self.sb(es, "ro_st", [128, 4, 6], F32); mv = self.sb(es, "ro_mv", [128, 4, 2], F32)
        rs = self.sb(es, "ro_rs", [128, 4], F32); bs = Buf()
        yn = self.sb(es, "ro_yn", [128, 256], F32); byn = Buf()
        res = [self.sb(es, "ro_res%d" % i, [128, 2, 128], BF16) for i in range(NB)]; bres = [Buf() for _ in range(NB)]
        tmp = self.sb(es, "ro_tmp", [128, 2, 128], F32); btmp = Buf()
        def tile_gen(n, ti):
            s = n % NB
            tk = ti * 128
            self.dma("sp", yf[s][:], yA[0, tk:tk + 128, :], [self.dbuf["rk_y"]], [bin_[s]])
            self.dma("sp", yb[s][:], yA[1, tk:tk + 128, :], [self.dbuf["rk_y"]], [bin_[s]])
            self.dma("sp", bo[s][:], A["rk_bonus"].rearrange("(c p) n -> p c n", p=128)[:, :, tk:tk + 128], [self.dbuf["rk_bonus"]], [bin_[s]])
            self.dma("sp", gt[s][:], A["rk_g"].rearrange("(c p) n -> p c n", p=128)[:, :, tk:tk + 128], [self.dbuf["rk_g"]], [bin_[s]])
            yield
            self.tt("dve", yf[s][:], yf[s][:], yb[s][:], ALU.add, [bin_[s]], [bin_[s]])
            for h in range(4):
                self.op("dve", lambda e, h=h, s=s: e.bn_stats(out=st[:, h, :], in_=yf[s][:, h * 64:(h + 1) * 64]), [bin_[s]], [bs])
                self.op("dve", lambda e, h=h: e.bn_aggr(out=mv[:, h, :], in_=st[:, h, :]), [bs], [bs])
            yield True
            self.act(rs[:], mv[:, :, 1], AF.Sqrt, [bs], [bs], bias=64e-5, scale=1.0)
            self.op("dve", lambda e: e.reciprocal(out=rs[:], in_=rs[:]), [bs], [bs])
            for h in range(4):
                self.ts("dve", yn[:, h * 64:(h + 1) * 64], yf[s][:, h * 64:(h + 1) * 64], mv[:, h, 0:1], rs[:, h:h + 1],
                        ALU.subtract, ALU.mult, [bin_[s], bs], [byn])
            self.tt("pool", yn[:], yn[:], gb[:, 0, :], ALU.mult, [byn, bgb], [byn])
            self.tt("pool", yn[:], yn[:], gb[:, 1, :], ALU.add, [byn, bgb], [byn])
            yield
            pf, bpf = self.psf[n % 2]
            for cc in range(2):
                self.tr(pf[:, cc * 128:(cc + 1) * 128], yn[:, cc * 128:(cc + 1) * 128], self.ident_f[:], [byn, self.b_ident_f], [bpf],
                        inc=(cc == 1))
            yield
            self.tt("dve", tmp[:], pf[:, 0:256].rearrange("p (c t) -> p c t", c=2), bo[s][:], ALU.add, [bpf, bin_[s]], [btmp])
            self.tt("dve", res[s][:], tmp[:], gt[s][:], ALU.mult, [btmp, bin_[s]], [bres[s]])
            self.dma("sp", self.ysT3[:, 0:2, tk:tk + 128], res[s][:], [bres[s]], [self.dbuf["ysT"]])
            yield

        run_pipeline((tile_gen(n, ti) for n, ti in enumerate(tiles)), depth=2)
        self.barrier()


K.phase_rwkv_out = phase_rwkv_out


def phase_rwkv(self, l, ctx_out):
    self.phase_rwkv_prep(l)
    self.phase_rwkv_scan(l, ctx_out)
    self.phase_rwkv_out(l, list(range(NTILE)) if ctx_out else list(range(2, NTILE)))


def phase_rwkv_out_merge(self, l, ctx_out, tiles_merge):
    with ExitStack() as es:
        pre = self.merge_load(es, l)
        self.phase_rwkv_out(l, list(range(NTILE)) if ctx_out else list(range(2, NTILE)))
        self.phase_merge(l, tiles_merge, pre=pre)


K.phase_rwkv_out_merge = phase_rwkv_out_merge


K.phase_rwkv = phase_rwkv


def build_program(dbg=False):
    nc = bass.Bass("TRN2", target_bir_lowering=False)
    k = K(nc, dbg=dbg)
    k.setup()
    alltiles = list(range(NTILE))
    lat = list(range(2, NTILE))
    for l in range(DEPTH):
        ctx_out = l < DEPTH - 1
        k.phase_mod(l)
        k.phase_h(l, alltiles)
        k.phase_sconv(l, ctx_out)
        k.phase_attn(l, ctx_out)
        k.phase_hyena(l, ctx_out)
        k.phase_rwkv_prep(l)
        k.phase_rwkv_scan(l, ctx_out)
        tl = alltiles if ctx_out else lat
        k.phase_rwkv_out_merge(l, ctx_out, tl)
        k.phase_moe(l, tl, not ctx_out)
    k.P.finish([k.dbuf["out"]])
    return nc, k


_CACHE = {}


def kernel(**inputs):
    n = 8
    if "nc" not in _CACHE:
        _CACHE["nc"] = build_program()[0]
    nc = _CACHE["nc"]
    maps = make_in_maps(inputs, list(range(n)))
    res = run_bass_kernel_spmd(nc, maps, core_ids=list(range(n)))
    out = np.stack([np.asarray(res.results[i]["out"], dtype=np.float32) for i in range(n)], 0)
    return out
```
